# Optimizing a Trainium2 kernel written in Bass

```python
import math
import jax, jax.numpy as jnp
from jax import lax
import numpy as np


D_MODEL = 1024
BATCH = 1
SEQ = 16384
DEPTH = 4

N_META = 16
SSM_WIDTH = D_MODEL // 2
SSM_GROUP = 16
SSM_GROUPS = SSM_WIDTH // SSM_GROUP
SSM_STATE = 64
MLSTM_WIDTH = D_MODEL // 2
MLSTM_HEADS = 4
MLSTM_HEAD_DIM = MLSTM_WIDTH // MLSTM_HEADS
MLSTM_CHUNK = 64
QK_CONV = 4
FFN_DIM = 2816
FFN_CONV = 3
NORM_EPS = 1e-6
PAD_LOG_INPUT_GATE = -1e4
IN_WIDTHS = (SSM_WIDTH, MLSTM_WIDTH, MLSTM_WIDTH, MLSTM_WIDTH, MLSTM_WIDTH, MLSTM_HEADS, MLSTM_HEADS, D_MODEL, D_MODEL)
IN_SPLITS = tuple(int(s) for s in np.cumsum(IN_WIDTHS)[:-1])
N_IN = int(sum(IN_WIDTHS))

kernel_name = 'hybrid_s5_mlstm_gated_trunk'


def rmsnorm(x, g):
    xf = x.astype(jnp.float32)
    y = xf * lax.rsqrt(jnp.mean(xf * xf, axis=-1, keepdims=True) + NORM_EPS)
    return (y * g.astype(jnp.float32)).astype(x.dtype)


def causal_dwconv(x, w, b):
    k = w.shape[0]
    y = lax.conv_general_dilated(x, w[:, None, :].astype(x.dtype), (1,), [(k - 1, 0)],
                                 dimension_numbers=('NWC', 'WIO', 'NWC'),
                                 feature_group_count=x.shape[-1])
    return y + b.astype(x.dtype)


def _complex_affine_combine(e1, e2):
    a1r, a1i, b1r, b1i = e1
    a2r, a2i, b2r, b2i = e2
    return (a2r * a1r - a2i * a1i,
            a2r * a1i + a2i * a1r,
            a2r * b1r - a2i * b1i + b2r,
            a2r * b1i + a2i * b1r + b2i)


def s5_ssm(u, lam_re, lam_im, b_re, b_im, c_re, c_im, d, log_dt):
    bsz, L, _ = u.shape
    u = u.astype(jnp.float32).reshape(bsz, L, SSM_GROUPS, SSM_GROUP)
    lam_re = lam_re.astype(jnp.float32)
    lam_im = lam_im.astype(jnp.float32)
    dt = jnp.exp(log_dt.astype(jnp.float32))[:, None]
    mag = jnp.exp(lam_re * dt)
    ab_re = mag * jnp.cos(lam_im * dt)
    ab_im = mag * jnp.sin(lam_im * dt)
    nr, ni = ab_re - 1.0, ab_im
    den = lam_re * lam_re + lam_im * lam_im
    z_re = (nr * lam_re + ni * lam_im) / den
    z_im = (ni * lam_re - nr * lam_im) / den
    b_re = b_re.astype(jnp.float32)
    b_im = b_im.astype(jnp.float32)
    bb_re = z_re[..., None] * b_re - z_im[..., None] * b_im
    bb_im = z_re[..., None] * b_im + z_im[..., None] * b_re
    bu_re = jnp.einsum('blgh,gph->blgp', u, bb_re)
    bu_im = jnp.einsum('blgh,gph->blgp', u, bb_im)
    a_re = jnp.broadcast_to(ab_re, bu_re.shape)
    a_im = jnp.broadcast_to(ab_im, bu_im.shape)
    _, _, x_re, x_im = lax.associative_scan(_complex_affine_combine, (a_re, a_im, bu_re, bu_im), axis=1)
    y = (jnp.einsum('blgp,ghp->blgh', x_re, c_re.astype(jnp.float32))
         - jnp.einsum('blgp,ghp->blgh', x_im, c_im.astype(jnp.float32))
         + d.astype(jnp.float32) * u)
    return y.reshape(bsz, L, SSM_WIDTH)


def mlstm_chunkwise(q, k, v, log_i, log_f):
    bsz, lp, nh, dh = q.shape
    nc = lp // MLSTM_CHUNK

    def to_chunks(t):
        return jnp.moveaxis(t.reshape(bsz, nc, MLSTM_CHUNK, nh, -1), 3, 1)

    q, k, v = to_chunks(q), to_chunks(k), to_chunks(v)
    log_i = to_chunks(log_i[..., None])[..., 0]
    log_f = to_chunks(log_f[..., None])[..., 0]
    b = jnp.cumsum(log_f, axis=-1)
    g = b[..., -1]
    causal = jnp.tril(jnp.ones((MLSTM_CHUNK, MLSTM_CHUNK), dtype=bool))
    log_d = jnp.where(causal, b[..., :, None] - b[..., None, :] + log_i[..., None, :], -jnp.inf)
    a = g[..., None] - b + log_i

    def step(carry, xs):
        c_st, n_st, m_st = carry
        k_c, v_c, a_c, g_c = xs
        m_new = jnp.maximum(g_c + m_st, jnp.max(a_c, axis=-1))
        decay = jnp.exp(g_c + m_st - m_new)
        w = jnp.exp(a_c - m_new[..., None])
        c_new = decay[..., None, None] * c_st + jnp.einsum('bhs,bhsd,bhse->bhde', w, v_c, k_c)
        n_new = decay[..., None] * n_st + jnp.einsum('bhs,bhse->bhe', w, k_c)
        return (c_new, n_new, m_new), (c_st, n_st, m_st)

    init = (jnp.zeros((bsz, nh, dh, dh), jnp.float32),
            jnp.zeros((bsz, nh, dh), jnp.float32),
            jnp.zeros((bsz, nh), jnp.float32))
    xs = (jnp.moveaxis(k, 2, 0), jnp.moveaxis(v, 2, 0), jnp.moveaxis(a, 2, 0), jnp.moveaxis(g, 2, 0))
    _, (c_prev, n_prev, m_prev) = lax.scan(step, init, xs)
    c_prev = jnp.moveaxis(c_prev, 0, 2)
    n_prev = jnp.moveaxis(n_prev, 0, 2)
    m_prev = jnp.moveaxis(m_prev, 0, 2)
    log_inter = b + m_prev[..., None]
    m_t = jnp.maximum(log_inter, jnp.max(log_d, axis=-1))
    s = jnp.einsum('bhnte,bhnse->bhnts', q, k) * jnp.exp(log_d - m_t[..., None])
    w_inter = jnp.exp(log_inter - m_t)
    num = (jnp.einsum('bhnts,bhnsd->bhntd', s, v)
           + w_inter[..., None] * jnp.einsum('bhnde,bhnte->bhntd', c_prev, q))
    den = jnp.sum(s, axis=-1) + w_inter * jnp.einsum('bhne,bhnte->bhnt', n_prev, q)
    h = num / jnp.maximum(jnp.abs(den), jnp.exp(-m_t))[..., None]
    return jnp.moveaxis(h, 1, 3).reshape(bsz, lp, nh, dh)


def token_mixer(x, g_pre, g_post, w_in, b_gates, lam_re, lam_im, b_re, b_im, c_re, c_im, d, log_dt,
                w_glu, w_qk, b_qk, g_head, w_a, w_b, w_out):
    bsz, L, _ = x.shape
    dtype = x.dtype
    h = rmsnorm(x, g_pre)
    proj = h @ w_in
    u, q, k, v, o, gi, gf, ga, gb = jnp.split(proj, IN_SPLITS, axis=-1)

    y_a = jax.nn.gelu(s5_ssm(u, lam_re, lam_im, b_re, b_im, c_re, c_im, d, log_dt))
    y_a = y_a * jax.nn.sigmoid(y_a @ w_glu.astype(jnp.float32))

    qk = jax.nn.silu(causal_dwconv(jnp.concatenate([q, k], axis=-1), w_qk, b_qk)).astype(jnp.float32)
    q, k = jnp.split(qk, 2, axis=-1)
    heads = lambda t: t.reshape(bsz, L, MLSTM_HEADS, MLSTM_HEAD_DIM)
    q = heads(q)
    k = heads(k) * (MLSTM_HEAD_DIM ** -0.5)
    v = heads(v.astype(jnp.float32))
    gates = (jnp.concatenate([gi, gf], axis=-1) + b_gates.astype(dtype)).astype(jnp.float32)
    log_i, f_pre = jnp.split(gates, 2, axis=-1)
    log_f = jax.nn.log_sigmoid(f_pre)
    pad = MLSTM_CHUNK - N_META
    padseq = lambda t, val: jnp.pad(t, [(0, 0), (pad, 0)] + [(0, 0)] * (t.ndim - 2), constant_values=val)
    hb = mlstm_chunkwise(padseq(q, 0.0), padseq(k, 0.0), padseq(v, 0.0),
                         padseq(log_i, PAD_LOG_INPUT_GATE), padseq(log_f, 0.0))[:, pad:]
    hb = hb * lax.rsqrt(jnp.mean(hb * hb, axis=-1, keepdims=True) + NORM_EPS)
    hb = hb * g_head.astype(jnp.float32).reshape(MLSTM_HEADS, MLSTM_HEAD_DIM)
    y_b = hb.reshape(bsz, L, MLSTM_WIDTH) * jax.nn.sigmoid(o.astype(jnp.float32))

    merged = (jax.nn.sigmoid(ga) * (y_a.astype(dtype) @ w_a)
              + jax.nn.sigmoid(gb) * (y_b.astype(dtype) @ w_b))
    return x + rmsnorm(merged @ w_out, g_post)


def channel_mixer(x, g_pre, g_post, w_gate, w_up, w_conv, b_conv, w_down):
    h = rmsnorm(x, g_pre)
    a = causal_dwconv(h @ w_gate, w_conv, b_conv)
    y = (jax.nn.gelu(a, approximate=True) * (h @ w_up)) @ w_down
    return x + rmsnorm(y, g_post)


def setup_inputs(seed: int = 0) -> dict:
    key = jax.random.key(seed)
    ks = iter(jax.random.split(key, 40))
    f32 = jnp.float32
    nrm = lambda shape, scale: scale * jax.random.normal(next(ks), shape, f32)
    gain = lambda: 1.0 + nrm((DEPTH, D_MODEL), 0.02)
    G, P, GH, H = SSM_GROUPS, SSM_STATE, SSM_GROUP, MLSTM_HEADS
    x = nrm((BATCH, SEQ, D_MODEL), 1.0)
    meta_tokens = nrm((N_META, D_MODEL), 1.0)
    g_mix_pre = gain()
    g_mix_post = gain()
    w_in = nrm((DEPTH, D_MODEL, N_IN), D_MODEL ** -0.5)
    b_gates = jnp.concatenate([nrm((DEPTH, H), 0.1),
                               jnp.linspace(3.0, 6.0, H, dtype=f32)[None, :] + nrm((DEPTH, H), 0.1)], axis=-1)
    n_idx = jnp.arange(P, dtype=f32)
    ssm_lambda_re = -0.5 + nrm((DEPTH, G, P), 0.01)
    ssm_lambda_im = jnp.pi * n_idx[None, None, :] + nrm((DEPTH, G, P), 0.01)
    ssm_b_re = nrm((DEPTH, G, P, GH), (2 * GH) ** -0.5)
    ssm_b_im = nrm((DEPTH, G, P, GH), (2 * GH) ** -0.5)
    ssm_c_re = nrm((DEPTH, G, GH, P), P ** -0.5)
    ssm_c_im = nrm((DEPTH, G, GH, P), P ** -0.5)
    ssm_d = nrm((DEPTH, G, GH), 1.0)
    ssm_log_dt = jax.random.uniform(next(ks), (DEPTH, G), f32, math.log(1e-3), math.log(1e-1))
    w_ssm_glu = nrm((DEPTH, SSM_WIDTH, SSM_WIDTH), SSM_WIDTH ** -0.5)
    w_qk_conv = nrm((DEPTH, QK_CONV, 2 * MLSTM_WIDTH), QK_CONV ** -0.5)
    b_qk_conv = nrm((DEPTH, 2 * MLSTM_WIDTH), 0.02)
    g_head_norm = 1.0 + nrm((DEPTH, MLSTM_WIDTH), 0.02)
    w_branch_ssm = nrm((DEPTH, SSM_WIDTH, D_MODEL), SSM_WIDTH ** -0.5)
    w_branch_mlstm = nrm((DEPTH, MLSTM_WIDTH, D_MODEL), MLSTM_WIDTH ** -0.5)
    w_out = nrm((DEPTH, D_MODEL, D_MODEL), D_MODEL ** -0.5)
    g_ffn_pre = gain()
    g_ffn_post = gain()
    w_ffn_gate = nrm((DEPTH, D_MODEL, FFN_DIM), D_MODEL ** -0.5)
    w_ffn_up = nrm((DEPTH, D_MODEL, FFN_DIM), D_MODEL ** -0.5)
    w_ffn_conv = nrm((DEPTH, FFN_CONV, FFN_DIM), FFN_CONV ** -0.5)
    b_ffn_conv = nrm((DEPTH, FFN_DIM), 0.02)
    w_ffn_down = nrm((DEPTH, FFN_DIM, D_MODEL), FFN_DIM ** -0.5)
    return {'x': x, 'meta_tokens': meta_tokens, 'g_mix_pre': g_mix_pre, 'g_mix_post': g_mix_post,
            'w_in': w_in, 'b_gates': b_gates, 'ssm_lambda_re': ssm_lambda_re, 'ssm_lambda_im': ssm_lambda_im,
            'ssm_b_re': ssm_b_re, 'ssm_b_im': ssm_b_im, 'ssm_c_re': ssm_c_re, 'ssm_c_im': ssm_c_im,
            'ssm_d': ssm_d, 'ssm_log_dt': ssm_log_dt, 'w_ssm_glu': w_ssm_glu, 'w_qk_conv': w_qk_conv,
            'b_qk_conv': b_qk_conv, 'g_head_norm': g_head_norm, 'w_branch_ssm': w_branch_ssm,
            'w_branch_mlstm': w_branch_mlstm, 'w_out': w_out, 'g_ffn_pre': g_ffn_pre, 'g_ffn_post': g_ffn_post,
            'w_ffn_gate': w_ffn_gate, 'w_ffn_up': w_ffn_up, 'w_ffn_conv': w_ffn_conv, 'b_ffn_conv': b_ffn_conv,
            'w_ffn_down': w_ffn_down}


def reference(x, meta_tokens, g_mix_pre, g_mix_post, w_in, b_gates, ssm_lambda_re, ssm_lambda_im,
              ssm_b_re, ssm_b_im, ssm_c_re, ssm_c_im, ssm_d, ssm_log_dt, w_ssm_glu, w_qk_conv,
              b_qk_conv, g_head_norm, w_branch_ssm, w_branch_mlstm, w_out, g_ffn_pre, g_ffn_post,
              w_ffn_gate, w_ffn_up, w_ffn_conv, b_ffn_conv, w_ffn_down):
    bsz = x.shape[0]
    meta = jnp.broadcast_to(meta_tokens.astype(x.dtype)[None], (bsz, N_META, D_MODEL))
    h = jnp.concatenate([meta, x], axis=1)
    for l in range(DEPTH):
        h = token_mixer(h, g_mix_pre[l], g_mix_post[l], w_in[l], b_gates[l],
                        ssm_lambda_re[l], ssm_lambda_im[l], ssm_b_re[l], ssm_b_im[l],
                        ssm_c_re[l], ssm_c_im[l], ssm_d[l], ssm_log_dt[l], w_ssm_glu[l],
                        w_qk_conv[l], b_qk_conv[l], g_head_norm[l],
                        w_branch_ssm[l], w_branch_mlstm[l], w_out[l])
        h = channel_mixer(h, g_ffn_pre[l], g_ffn_post[l], w_ffn_gate[l], w_ffn_up[l],
                          w_ffn_conv[l], b_ffn_conv[l], w_ffn_down[l])
    return h[:, N_META:]
```

```python
import math
from contextlib import ExitStack
import numpy as np
import concourse.bass as bass
import concourse.mybir as mybir
from concourse.bass_utils import run_bass_kernel_spmd

F32 = mybir.dt.float32
BF16 = mybir.dt.bfloat16
I32 = mybir.dt.int32
AF = mybir.ActivationFunctionType
ALU = mybir.AluOpType

D = 1024
KD = 8
TC = 512
NSUB = 4
DEPTH = 4
NMETA = 16
SEQ = 16384
NCH = 33
PADF = NCH * TC - SEQ - NMETA
FFN = 2816
NFT = 22
NBLK = 36
NBUF = 4
EPS = 1e-6
LN_SCALE = math.log(128.0 ** -0.5)
TWO_PI = 2.0 * math.pi
CW1 = 6.28125
CW2 = TWO_PI - CW1
SEM_LIMIT = 30000

_off = {}
_o = 0
for _n, _w in [("g_pre", 8), ("g_post", 8), ("gf_pre", 8), ("gf_post", 8), ("wgate", 64), ("bgate", 8),
               ("wqk", 32), ("bqk", 8), ("ghead", 4), ("wfc", 66), ("bfc", 22), ("lre", 16), ("lim", 16),
               ("ldt", 16), ("dvec", 4)]:
    _off[_n] = (_o, _w)
    _o += _w
NSMALL = _o


class Reg:
    __slots__ = ("w", "r", "name")

    def __init__(self, name=""):
        self.w = None
        self.r = []
        self.name = name


class Prog:
    ENGS = ("pe", "act", "dve", "pool", "sp")

    def __init__(self, nc, stack):
        self.nc = nc
        self.stack = stack
        self.q = {e: [] for e in self.ENGS}
        self.cnt = {e: 0 for e in self.ENGS}
        self.sems = {e: [stack.enter_context(nc.semaphore(f"s_{e}_0"))] for e in self.ENGS}
        self.waited = {e: {} for e in self.ENGS}
        self.dma_sems = [stack.enter_context(nc.semaphore(f"s_dma_{i}")) for i in range(8 + NBUF)]
        self.dma_cnt = [0] * (8 + NBUF)
        self.dma_rr = 0
        self.regs = {}
        self.nops = 0

    def R(self, x):
        if isinstance(x, Reg):
            return x
        if x not in self.regs:
            self.regs[x] = Reg(x)
        return self.regs[x]

    def _collect(self, eng, R, W, extra=()):
        evs = list(extra)
        for r in R:
            if r.w is not None:
                evs.append(r.w)
        for w in W:
            if w.w is not None:
                evs.append(w.w)
            evs.extend(w.r)
        need = {}
        for (s, v) in evs:
            k = id(s)
            if self.waited[eng].get(k, 0) >= v:
                continue
            if k not in need or need[k][1] < v:
                need[k] = (s, v)
        for k, (s, v) in need.items():
            self.waited[eng][k] = v
        return list(need.values())

    def _mark(self, ev, R, W):
        for r in R:
            r.r.append(ev)
        for w in W:
            w.w = ev
            w.r = []

    def op(self, eng, fn, R=(), W=()):
        R = [self.R(x) for x in R]
        W = [self.R(x) for x in W]
        waits = self._collect(eng, R, W)
        if self.cnt[eng] >= SEM_LIMIT:
            self.sems[eng].append(self.stack.enter_context(self.nc.semaphore(f"s_{eng}_{len(self.sems[eng])}")))
            self.cnt[eng] = 0
        self.cnt[eng] += 1
        sem = self.sems[eng][-1]
        ev = (sem, self.cnt[eng])
        self.q[eng].append((waits, fn, (sem, 1)))
        self._mark(ev, R, W)
        self.nops += 1
        return ev

    def dma(self, eng, out, in_, R=(), W=(), sem_idx=None):
        R = [self.R(x) for x in R]
        W = [self.R(x) for x in W]
        if sem_idx is None:
            sem_idx = self.dma_rr
            self.dma_rr = (self.dma_rr + 1) % 8
        s = self.dma_sems[sem_idx]
        extra = [(s, self.dma_cnt[sem_idx])] if self.dma_cnt[sem_idx] else []
        waits = self._collect(eng, R, W, extra)
        self.dma_cnt[sem_idx] += 16
        ev = (s, self.dma_cnt[sem_idx])
        self.q[eng].append((waits, (lambda e, o=out, i=in_: e.dma_start(out=o, in_=i)), (s, 16)))
        self._mark(ev, R, W)
        return ev

    def final_wait(self, eng, evs):
        self.q[eng].append((list(evs), None, None))

    def emit(self):
        nc = self.nc
        with nc.Block() as block:
            def mk(name):
                def body(e):
                    for waits, fn, inc in self.q[name]:
                        for (s, v) in waits:
                            e.wait_ge(s, v)
                        if fn is not None:
                            ins = fn(e)
                            ins.then_inc(inc[0], inc[1])
                return body
            block.tensor(mk("pe"))
            block.scalar(mk("act"))
            block.vector(mk("dve"))
            block.gpsimd(mk("pool"))
            block.sync(mk("sp"))


def build_program(nch_run, dumps=()):
    nc = bass.Bass("TRN2", target_bir_lowering=False)
    xT = nc.dram_tensor("xT", [nch_run, 128, KD, TC], F32, kind="ExternalInput").ap()
    yT = nc.dram_tensor("yT", [nch_run, 128, KD, TC], F32, kind="ExternalOutput").ap()
    wst = nc.dram_tensor("wst", [DEPTH, NBLK, 128, 4096], F32, kind="ExternalInput").ap()
    small = nc.dram_tensor("small", [128, DEPTH, NSMALL], F32, kind="ExternalInput").ap()
    craw = nc.dram_tensor("craw", [DEPTH, 128, 16, 256], F32, kind="ExternalInput").ap()
    consts = nc.dram_tensor("consts", [128, 128 * 3 + TC], F32, kind="ExternalInput").ap()
    padb = nc.dram_tensor("padb", [128, nch_run * NSUB], F32, kind="ExternalInput").ap()
    tabs = nc.dram_tensor("tabs", [DEPTH, 16, 128, 2 * TC], F32).ap()
    cz = nc.dram_tensor("cz", [DEPTH, 128, 4096], F32).ap()
    dump_out = {}
    dump_c = 1 if nch_run > 1 else 0

    st = ExitStack()
    P = Prog(nc, st)

    def sb(name, shape, dt=F32):
        return st.enter_context(nc.sbuf_tensor(name, shape, dt))

    X = sb("X", [128, KD, TC])
    HT = sb("HT", [128, KD, TC], BF16)
    SQ = sb("SQ", [128, 2, TC], BF16)
    RSTD = sb("RSTD", [128, TC])
    OT = sb("OT", [128, KD, TC])
    WB = [sb(f"WB{i}", [128, 4096], BF16) for i in range(NBUF)]
    SM = sb("SM", [128, DEPTH, NSMALL])
    CONS = sb("CONS", [128, 128 * 3 + TC])
    PADB = sb("PADB", [128, nch_run * NSUB])
    IDB = sb("IDB", [128, 128], BF16)
    ONESB = sb("ONESB", [128, 128], BF16)
    WGB = sb("WGB", [128, DEPTH, 64], BF16)
    ONE1 = sb("ONE1", [128, 1])
    LNS = sb("LNS", [128, 1])
    UT = sb("UT", [128, 4, TC], BF16)
    QKPH = sb("QKPH", [128, DEPTH, 8, 3])
    QKP1 = sb("QKP1", [128, 2, 3 + TC])
    QKA = sb("QKA", [128, 2, TC])
    QKT = sb("QKT", [128, 8, TC], BF16)
    V = sb("V", [128, NSUB, 4, 129], BF16)
    SIGO = sb("SIGO", [128, 4, TC], BF16)
    BIG = sb("BIG", [128, 3 * KD, TC], BF16)
    G = sb("G", [128, NSUB, 8])
    YA = sb("YA", [128, 4, TC], BF16)
    YG = sb("YG", [128, 4, TC], BF16)
    YB = sb("YB", [128, 4, TC], BF16)
    S5DEC = sb("S5DEC", [128, DEPTH, 16])
    S5ROT = sb("S5ROT", [128, DEPTH, 2, 16])
    S5ST = sb("S5ST", [128, DEPTH, 2, 16])
    S5E = sb("S5E", [128, 2, 16])
    TAB = [sb(f"TAB{i}", [128, 2 * TC]) for i in range(2)]
    BB = sb("BB", [128, 2, TC])
    SS = sb("SS", [128, 2, TC])
    T = [sb(f"T{i}", [128, TC]) for i in range(4)]
    XR = [sb(f"XR{i}", [128, 2, TC], BF16) for i in range(2)]
    YS = sb("YS", [128, TC])
    CT = sb("CT", [128, DEPTH, 4, 129])
    CTB = sb("CTB", [128, 4, 128], BF16)
    NB = sb("NB", [128, 4, 128], BF16)
    SP_ = sb("SP_", [128, 4])
    LI = sb("LI", [128, 4])
    SPB = sb("SPB", [128, 4, 128])
    BD = sb("BD", [128, 4])
    TW = sb("TW", [128, 4])
    WEX = sb("WEX", [128, 4])
    DEC = sb("DEC", [128, 4])
    DT_ = sb("DT_", [128, TC])
    DTM = sb("DTM", [128, TC])
    EF = sb("EF", [128, TC])
    QP = sb("QP", [128, TC], BF16)
    WT = sb("WT", [128, TC], BF16)
    KW = sb("KW", [128, TC], BF16)
    SQH = sb("SQH", [128, TC], BF16)
    FH = sb("FH", [128, DEPTH, NFT, 2])
    GE = sb("GE", [128, TC])
    PRK = GE[:].bitcast(I32)
    SCN = ("dt", "th", "ar", "c", "s", "abr", "abi", "nr", "den", "t1", "t2", "zre", "zim", "a16", "c2", "s2", "nzim")
    SCT = sb("SCT", [128, len(SCN), 16])
    SC = {n: SCT[:, i, :] for i, n in enumerate(SCN)}
    SIGA = [BIG[:, t, :] for t in range(KD)]
    SIGB = [BIG[:, KD + t, :] for t in range(KD)]
    MRG = [BIG[:, 2 * KD + t, :] for t in range(KD)]
    ACTT = [BIG[:, j, :] for j in range(NFT)]
    AD, HH, RS = T[0], T[1], T[2]
    ACC = T[3]
    GP = BB[:].rearrange("p a t -> p (a t)")

    PS = [st.enter_context(nc.psum_tensor(f"PS{i}", [128, TC], F32)) for i in range(7)]
    PST = st.enter_context(nc.psum_tensor("PST", [128, TC], BF16))
    PSR = [Reg(f"ps{i}") for i in range(7)]
    bank_rr = [0]

    def bank():
        i = bank_rr[0]
        bank_rr[0] = (i + 1) % 7
        return PS[i], PSR[i]

    def ACT(out, in_, func, R, W, **kw):
        P.op("act", lambda e: e.activation(out=out, in_=in_, func=func, **kw), R, W)

    def TT(eng, out, in0, in1, op, R, W):
        P.op(eng, lambda e: e.tensor_tensor(out=out, in0=in0, in1=in1, op=op), R, W)

    def TS(eng, out, in0, s1, s2, op0, op1, R, W):
        if s2 is None:
            P.op(eng, lambda e: e.tensor_scalar(out=out, in0=in0, scalar1=s1, scalar2=None, op0=op0), R, W)
        else:
            P.op(eng, lambda e: e.tensor_scalar(out=out, in0=in0, scalar1=s1, scalar2=s2, op0=op0, op1=op1), R, W)

    def STT(eng, out, in0, scalar, in1, op0, op1, R, W):
        P.op(eng, lambda e: e.scalar_tensor_tensor(out=out, in0=in0, scalar=scalar, in1=in1, op0=op0, op1=op1), R, W)

    def CP(eng, out, in_, R, W):
        P.op(eng, lambda e: e.tensor_copy(out=out, in_=in_), R, W)

    def RECIP(out, in_, R, W):
        P.op("dve", lambda e: e.reciprocal(out=out, in_=in_), R, W)

    def MM(out, lhsT, rhs, R, W, start=True, stop=True):
        P.op("pe", lambda e: e.matmul(out, lhsT=lhsT, rhs=rhs, start=start, stop=stop), R, W)

    def MMG(out, pairs, R, W):
        pairs = list(pairs)

        def fn(e):
            ins = None
            n = len(pairs)
            for i, (a, b) in enumerate(pairs):
                ins = e.matmul(out, lhsT=a, rhs=b, start=(i == 0), stop=(i == n - 1))
            return ins
        P.op("pe", fn, R, W)

    def dump(name, ap, shape, R):
        if name in dumps:
            t = nc.dram_tensor("dbg_" + name, list(shape), F32 if ap.dtype == F32 else BF16, kind="ExternalOutput").ap()
            dump_out[name] = P.dma("sp", t, ap, R=R, W=["dbg_" + name])

    P.dma("sp", SM[:], small[:, :, :], W=["SM"])
    P.dma("sp", CONS[:], consts[:, :], W=["CONS"])
    P.dma("sp", PADB[:], padb[:, :], W=["PADB"])
    IDF = CONS[:, 0:128]
    TRIU = CONS[:, 128:256]
    ONESF = CONS[:, 256:384]
    IOTA = CONS[:, 384:384 + TC]
    CP("dve", IDB[:], IDF, ["CONS"], ["IDB"])
    CP("dve", ONESB[:], ONESF, ["CONS"], ["ONESB"])
    for l in range(DEPTH):
        o = _off["wgate"][0]
        CP("dve", WGB[:, l, :], SM[:, l, o:o + 64], ["SM"], ["WGB"])
    P.op("pool", lambda e: e.memset(V[:], 1.0), W=["V0", "V1", "V2", "V3"])
    P.op("pool", lambda e: e.memset(CT[:], 0.0), W=["CT"])
    P.op("pool", lambda e: e.memset(S5ST[:], 0.0), W=["S5ST"])
    P.op("pool", lambda e: e.memset(QKPH[:], 0.0), W=["QKPH"])
    P.op("pool", lambda e: e.memset(FH[:], 0.0), W=["FH"])
    P.op("pool", lambda e: e.memset(ONE1[:], 1.0), W=["ONE1"])
    P.op("pool", lambda e: e.memset(LNS[:], LN_SCALE), W=["LNS"])

    def sm(l, name, j=None):
        o, w = _off[name]
        if j is None:
            return SM[:, l, o:o + w]
        return SM[:, l, o + j:o + j + 1]

    PRC = OT[:].rearrange("p a t -> p (a t)")
    PRZ = X[:].rearrange("p a t -> p (a t)")
    PRA, PRF, PRT0, PRT1 = T[0], T[1], T[2], T[3]

    OTR = [f"OT{t}" for t in range(KD)]

    def prologue_layer(l):
        s = SC
        lre, lim, ldt = sm(l, "lre"), sm(l, "lim"), sm(l, "ldt")
        dec = S5DEC[:, l, :]
        def exp_taylor(dst, dreg, src, sreg):
            TS("dve", dst, src, 1.0 / 5040, 1.0 / 720, ALU.mult, ALU.add, [sreg], [dreg])
            for cf in (1.0 / 120, 1.0 / 24, 1.0 / 6, 0.5, 1.0, 1.0):
                TT("dve", dst, dst, src, ALU.mult, [sreg], [dreg])
                TS("dve", dst, dst, cf, None, ALU.add, None, [], [dreg])

        def sin_reduced(dst, dreg, ang, areg, shift):
            TS("dve", s["t1"], ang, shift, 1.0 / TWO_PI, ALU.add, ALU.mult, [areg], ["sc_t1"])
            CP("dve", PRK[:, 0:16], s["t1"], ["sc_t1"], ["PRK"])
            CP("dve", s["t1"], PRK[:, 0:16], ["PRK"], ["sc_t1"])
            STT("dve", s["t2"], s["t1"], -CW1, ang, ALU.mult, ALU.add, ["sc_t1", areg], ["sc_t2"])
            STT("dve", s["t2"], s["t1"], -CW2, s["t2"], ALU.mult, ALU.add, ["sc_t1"], ["sc_t2"])
            TS("dve", s["t2"], s["t2"], shift, -math.pi, ALU.add, ALU.max, [], ["sc_t2"])
            TS("dve", s["t2"], s["t2"], math.pi, None, ALU.min, None, [], ["sc_t2"])
            ACT(dst, s["t2"], AF.Sin, ["sc_t2"], [dreg])

        TS("dve", s["a16"], ldt, 1.0 / 16, None, ALU.mult, None, ["SM"], ["sc_a16"])
        exp_taylor(s["dt"], "sc_dt", s["a16"], "sc_a16")
        for _ in range(4):
            TT("dve", s["dt"], s["dt"], s["dt"], ALU.mult, [], ["sc_dt"])
        TT("dve", s["ar"], lre, s["dt"], ALU.mult, ["SM", "sc_dt"], ["sc_ar"])
        TT("dve", s["th"], lim, s["dt"], ALU.mult, ["SM", "sc_dt"], ["sc_th"])
        exp_taylor(dec, "S5DEC", s["ar"], "sc_ar")
        sin_reduced(s["s"], "sc_s", s["th"], "sc_th", 0.0)
        sin_reduced(s["c"], "sc_c", s["th"], "sc_th", math.pi / 2)
        TT("dve", s["abr"], dec, s["c"], ALU.mult, ["S5DEC", "sc_c"], ["sc_abr"])
        TT("dve", s["abi"], dec, s["s"], ALU.mult, ["S5DEC", "sc_s"], ["sc_abi"])
        TS("dve", s["nr"], s["abr"], -1.0, None, ALU.add, None, ["sc_abr"], ["sc_nr"])
        TT("dve", s["t1"], lre, lre, ALU.mult, ["SM"], ["sc_t1"])
        TT("dve", s["t2"], lim, lim, ALU.mult, ["SM"], ["sc_t2"])
        TT("dve", s["den"], s["t1"], s["t2"], ALU.add, ["sc_t1", "sc_t2"], ["sc_den"])
        RECIP(s["den"], s["den"], ["sc_den"], ["sc_den"])
        TT("dve", s["t1"], s["nr"], lre, ALU.mult, ["sc_nr", "SM"], ["sc_t1"])
        TT("dve", s["t2"], s["abi"], lim, ALU.mult, ["sc_abi", "SM"], ["sc_t2"])
        TT("dve", s["t1"], s["t1"], s["t2"], ALU.add, ["sc_t1", "sc_t2"], ["sc_t1"])
        TT("dve", s["zre"], s["t1"], s["den"], ALU.mult, ["sc_t1", "sc_den"], ["sc_zre"])
        TT("dve", s["t1"], s["abi"], lre, ALU.mult, ["sc_abi", "SM"], ["sc_t1"])
        TT("dve", s["t2"], s["nr"], lim, ALU.mult, ["sc_nr", "SM"], ["sc_t2"])
        TT("dve", s["t1"], s["t1"], s["t2"], ALU.subtract, ["sc_t1", "sc_t2"], ["sc_t1"])
        TT("dve", s["zim"], s["t1"], s["den"], ALU.mult, ["sc_t1", "sc_den"], ["sc_zim"])
        TS("dve", s["nzim"], s["zim"], -1.0, None, ALU.mult, None, ["sc_zim"], ["sc_nzim"])
        P.dma("sp", PRC, craw[l].rearrange("p a t -> p (a t)"), W=OTR)
        for k in range(16):
            cre, cim = PRC[:, k * 256:k * 256 + 128], PRC[:, k * 256 + 128:k * 256 + 256]
            o1, o2 = PRZ[:, k * 256:k * 256 + 128], PRZ[:, k * 256 + 128:k * 256 + 256]
            TS("dve", o1, cim, s["nzim"][:, k:k + 1], None, ALU.mult, None, OTR + ["sc_nzim"], ["X"])
            STT("dve", o1, cre, s["zre"][:, k:k + 1], o1, ALU.mult, ALU.add, OTR + ["sc_zre"], ["X"])
            TS("dve", o2, cim, s["zre"][:, k:k + 1], -1.0, ALU.mult, ALU.mult, OTR + ["sc_zre"], ["X"])
            STT("dve", o2, cre, s["nzim"][:, k:k + 1], o2, ALU.mult, ALU.add, OTR + ["sc_nzim"], ["X"])
        P.dma("sp", cz[l], PRZ, R=["X"], W=[f"cz{l}"])
        for k in range(16):
            th = s["th"][:, k:k + 1]
            TS("dve", PRA[:], IOTA, th, None, ALU.mult, None, ["CONS", "sc_th"], ["T0"])
            tb = TAB[k % 2]
            for half, shift in ((1, 0.0), (0, math.pi / 2)):
                TS("dve", PRF[:], PRA[:], shift, 1.0 / TWO_PI, ALU.add, ALU.mult, ["T0"], ["T1"])
                CP("dve", PRK[:], PRF[:], ["T1"], ["PRK"])
                CP("dve", PRF[:], PRK[:], ["PRK"], ["T1"])
                STT("dve", PRT0[:], PRF[:], -CW1, PRA[:], ALU.mult, ALU.add, ["T1", "T0"], ["T2"])
                STT("dve", PRT0[:], PRF[:], -CW2, PRT0[:], ALU.mult, ALU.add, ["T1", "T2"], ["T2"])
                TS("dve", PRT0[:], PRT0[:], shift, -math.pi, ALU.add, ALU.max, ["T2"], ["T2"])
                TS("dve", PRT0[:], PRT0[:], math.pi, None, ALU.min, None, ["T2"], ["T2"])
                ACT(tb[:, half * TC:(half + 1) * TC], PRT0[:], AF.Sin, ["T2"], [f"TAB{k % 2}"])
            c1, s1 = s["c"][:, k:k + 1], s["s"][:, k:k + 1]
            c511, s511 = tb[:, TC - 1:TC], tb[:, 2 * TC - 1:2 * TC]
            tr = f"TAB{k % 2}"
            TT("dve", s["c2"][:, k:k + 1], c511, c1, ALU.mult, [tr, "sc_c"], ["sc_c2"])
            TT("dve", s["s2"][:, k:k + 1], s511, s1, ALU.mult, [tr, "sc_s"], ["sc_s2"])
            TT("dve", S5ROT[:, l, 0, k:k + 1], s["c2"][:, k:k + 1], s["s2"][:, k:k + 1], ALU.subtract, ["sc_c2", "sc_s2"], ["S5ROT"])
            TT("dve", s["c2"][:, k:k + 1], s511, c1, ALU.mult, [tr, "sc_c"], ["sc_c2"])
            TT("dve", s["s2"][:, k:k + 1], c511, s1, ALU.mult, [tr, "sc_s"], ["sc_s2"])
            TT("dve", S5ROT[:, l, 1, k:k + 1], s["c2"][:, k:k + 1], s["s2"][:, k:k + 1], ALU.add, ["sc_c2", "sc_s2"], ["S5ROT"])
            P.dma("sp", tabs[l, k], tb[:], R=[tr], W=[f"tab{l}_{k}", tr])

    for l in range(DEPTH):
        prologue_layer(l)

    wq = [(l, b) for c in range(nch_run) for l in range(DEPTH) for b in range(NBLK) if b != -1]
    wstate = {"next": 0, "use": 0}
    WBR = [Reg(f"wb{i}") for i in range(NBUF)]

    def w_issue():
        i = wstate["next"]
        l, b = wq[i]
        buf = i % NBUF
        if b == 10:
            P.dma("pool", WB[buf][:], cz[l], R=[f"cz{l}"], W=[WBR[buf]], sem_idx=8 + buf)
        else:
            P.dma("pool", WB[buf][:], wst[l, b], W=[WBR[buf]], sem_idx=8 + buf)
        wstate["next"] += 1

    def w_get(l, b):
        i = wstate["use"]
        assert wq[i] == (l, b), (wq[i], l, b)
        wstate["use"] += 1
        while wstate["next"] < min(len(wq), i + NBUF - 1):
            w_issue()
        return WB[i % NBUF], WBR[i % NBUF]

    HTR = [f"HT{kt}" for kt in range(KD)]

    def rmsnorm_stats(src_tile, src_reg):
        ps, pr = bank()
        for kt in range(KD):
            ACT(SQ[:, kt % 2, :], src_tile(kt), AF.Square, [src_reg(kt)], [f"SQ{kt % 2}"])
            MM(ps[:, :], ONESB[:], SQ[:, kt % 2, :], ["ONESB", f"SQ{kt % 2}"], [pr], start=(kt == 0), stop=(kt == KD - 1))
        TS("dve", RSTD[:], ps[:, :], 1.0 / D, EPS, ALU.mult, ALU.add, [pr], ["RSTD"])
        ACT(RSTD[:], RSTD[:], AF.Sqrt, ["RSTD"], ["RSTD"])
        RECIP(RSTD[:], RSTD[:], ["RSTD"], ["RSTD"])

    def pre_norm(l, gname):
        rmsnorm_stats(lambda kt: X[:, kt, :], lambda kt: "X")
        for kt in range(KD):
            STT("dve", HT[:, kt, :], X[:, kt, :], sm(l, gname, kt), RSTD[:], ALU.mult, ALU.mult, ["X", "RSTD", "SM"], [f"HT{kt}"])

    def post_norm_residual(l, gname):
        rmsnorm_stats(lambda kt: OT[:, kt, :], lambda kt: f"OT{kt}")
        for kt in range(KD):
            STT("dve", OT[:, kt, :], OT[:, kt, :], sm(l, gname, kt), RSTD[:], ALU.mult, ALU.mult, ["RSTD", "SM"], [f"OT{kt}"])
            TT("pool", X[:, kt, :], X[:, kt, :], OT[:, kt, :], ALU.add, [f"OT{kt}"], ["X"])

    def proj_tile(wb, wr, mm, ncols=512):
        ps, pr = bank()
        MMG(ps[:, :], [(wb[:, kc * ncols + mm * 128: kc * ncols + (mm + 1) * 128], HT[:, kc, :]) for kc in range(KD)], [wr] + HTR, [pr])
        return ps, pr

    def refresh_state(l):
        ACT(CTB[:], CT[:, l, :, 0:128], AF.Copy, ["CT"], ["CTB"])
        for h in range(4):
            TS("pool", NB[:, h, :], ONESB[:], CT[:, l, h, 128:129], None, ALU.mult, None, ["ONESB", "CT"], ["NB"])

    def mixer(c, l):
        pre_norm(l, "g_pre")
        if c == dump_c and l == 0:
            dump("ht", HT[:], [128, KD, TC], HTR)
        wb, wr = w_get(l, 0)
        for mm in range(4):
            ps, pr = proj_tile(wb, wr, mm)
            ACT(UT[:, mm, :], ps[:, :], AF.Copy, [pr], [f"UT{mm}"])
        for blk, base in ((1, 0), (2, 4)):
            wb, wr = w_get(l, blk)
            for mm in range(4):
                t = base + mm
                pb = t % 2
                ps, pr = proj_tile(wb, wr, mm)
                rq = f"QKP1_{pb}"
                ACT(QKP1[:, pb, 3:3 + TC], ps[:, :], AF.Copy, [pr], [rq])
                CP("pool", QKP1[:, pb, 0:3], QKPH[:, l, t, :], ["QKPH"], [rq])
                o = _off["wqk"][0] + t * 4
                ACT(QKA[:, pb, :], QKP1[:, pb, 0:TC], AF.Identity, [rq, "SM"], [f"QKA{pb}"], scale=SM[:, l, o:o + 1], bias=sm(l, "bqk", t))
                for j in (1, 2, 3):
                    STT("dve", QKA[:, pb, :], QKP1[:, pb, j:j + TC], SM[:, l, o + j:o + j + 1], QKA[:, pb, :], ALU.mult, ALU.add, [rq, "SM"], [f"QKA{pb}"])
                ACT(QKT[:, t, :], QKA[:, pb, :], AF.Silu, [f"QKA{pb}"], [f"QKT{t}"])
                CP("pool", QKPH[:, l, t, :], QKP1[:, pb, TC:TC + 3], [rq], ["QKPH"])
        wb, wr = w_get(l, 3)
        for sc in range(NSUB):
            ps, pr = bank()
            MMG(ps[:, :], [(HT[:, kc, sc * 128:(sc + 1) * 128], wb[:, kc * 512:(kc + 1) * 512]) for kc in range(KD)], [wr] + HTR, [pr])
            for h in range(4):
                ACT(V[:, sc, h, 0:128], ps[:, h * 128:(h + 1) * 128], AF.Copy, [pr], [f"V{sc}"])
        for sc in range(NSUB):
            ps, pr = bank()
            MMG(ps[:, 0:8], [(HT[:, kc, sc * 128:(sc + 1) * 128], WGB[:, l, kc * 8:kc * 8 + 8]) for kc in range(KD)], ["WGB"] + HTR, [pr])
            TT("dve", G[:, sc, :], ps[:, 0:8], sm(l, "bgate"), ALU.add, [pr, "SM"], [f"G{sc}"])
        wb, wr = w_get(l, 4)
        for mm in range(4):
            ps, pr = proj_tile(wb, wr, mm)
            ACT(SIGO[:, mm, :], ps[:, :], AF.Sigmoid, [pr], [f"SIGO{mm}"])
        for dst, nm, blks in ((SIGA, "SIGA", (5, 6)), (SIGB, "SIGB", (7, 8))):
            for bi, blk in enumerate(blks):
                wb, wr = w_get(l, blk)
                for mm in range(4):
                    t = bi * 4 + mm
                    ps, pr = proj_tile(wb, wr, mm)
                    ACT(dst[t], ps[:, :], AF.Sigmoid, [pr], [f"{nm}{t}"])
        if c == dump_c and l == 0:
            dump("ut", UT[:], [128, 4, TC], [f"UT{m}" for m in range(4)])
            dump("qkt", QKT[:], [128, 8, TC], [f"QKT{m}" for m in range(8)])
        wbB, wrB = w_get(l, 9)
        wbC, wrC = w_get(l, 10)
        ys_ps = ys_pr = None
        for k in range(16):
            q4 = k // 4
            tb, tbr = TAB[k % 2], f"TAB{k % 2}"
            P.dma("sp", tb[:], tabs[l, k], R=[f"tab{l}_{k}"], W=[tbr])
            cs, sn = tb[:, 0:TC], tb[:, TC:2 * TC]
            psr_, prr = bank()
            psi_, pri = bank()
            MM(psr_[:, :], wbB[:, k * 256:k * 256 + 128], UT[:, q4, :], [wrB, f"UT{q4}"], [prr])
            MM(psi_[:, :], wbB[:, k * 256 + 128:k * 256 + 256], UT[:, q4, :], [wrB, f"UT{q4}"], [pri])
            TT("dve", T[0][:], psr_[:, :], cs, ALU.mult, [prr, tbr], ["T0"])
            TT("dve", T[1][:], psi_[:, :], sn, ALU.mult, [pri, tbr], ["T1"])
            TT("dve", BB[:, 0, :], T[0][:], T[1][:], ALU.add, [], ["BB0", "T0", "T1"])
            TT("dve", T[0][:], psi_[:, :], cs, ALU.mult, [pri, tbr], ["T0"])
            TT("dve", T[1][:], psr_[:, :], sn, ALU.mult, [prr, tbr], ["T1"])
            TT("dve", BB[:, 1, :], T[0][:], T[1][:], ALU.subtract, [], ["BB1", "T0", "T1"])
            ssb = SS if k % 2 == 0 else QKA
            ssr = ("SS0", "SS1") if k % 2 == 0 else ("QKA0", "QKA1")
            for ri in range(2):
                dec_b = S5DEC[:, l, k:k + 1].to_broadcast([128, TC])
                P.op("dve", (lambda e, ri=ri, dec_b=dec_b, init=S5ST[:, l, ri, k:k + 1], o=ssb[:, ri, :]:
                             e.tensor_tensor_scan(out=o, data0=dec_b, data1=BB[:, ri, :], initial=init, op0=ALU.mult, op1=ALU.add)),
                     ["S5DEC", "S5ST", f"BB{ri}"], [ssr[ri]])
                ACT(S5E[:, ri, k:k + 1], ssb[:, ri, TC - 1:TC], AF.Copy, [ssr[ri]], ["S5E"])
            xr, xrr = XR[k % 2], f"XR{k % 2}"
            TT("pool", T[2][:], ssb[:, 0, :], cs, ALU.mult, [ssr[0], tbr], ["T2"])
            TT("pool", T[3][:], ssb[:, 1, :], sn, ALU.mult, [ssr[1], tbr], ["T3"])
            TT("pool", xr[:, 0, :], T[2][:], T[3][:], ALU.subtract, [], [xrr, "T2", "T3"])
            TT("pool", T[2][:], ssb[:, 0, :], sn, ALU.mult, [ssr[0], tbr], ["T2"])
            TT("pool", T[3][:], ssb[:, 1, :], cs, ALU.mult, [ssr[1], tbr], ["T3"])
            TT("pool", xr[:, 1, :], T[2][:], T[3][:], ALU.add, [], [xrr, "T2", "T3"])
            if k % 4 == 0:
                ys_ps, ys_pr = bank()
            MM(ys_ps[:, :], wbC[:, k * 256:k * 256 + 128], xr[:, 0, :], [wrC, xrr], [ys_pr], start=(k % 4 == 0), stop=False)
            MM(ys_ps[:, :], wbC[:, k * 256 + 128:k * 256 + 256], xr[:, 1, :], [wrC, xrr], [ys_pr], start=False, stop=(k % 4 == 3))
            if k % 4 == 3:
                STT("dve", YS[:], UT[:, q4, :], sm(l, "dvec", q4), ys_ps[:, :], ALU.mult, ALU.add, [f"UT{q4}", "SM", ys_pr], ["YS"])
                if c == dump_c and l == 0 and q4 == 0:
                    dump("ys", YS[:], [128, TC], ["YS"])
                ACT(YG[:, q4, :], YS[:], AF.Gelu_apprx_tanh, ["YS"], [f"YG{q4}"])
        c5, s5 = S5ROT[:, l, 0, :], S5ROT[:, l, 1, :]
        TT("dve", SC["t1"], S5E[:, 0, :], c5, ALU.mult, ["S5E", "S5ROT"], ["sc_t1"])
        TT("dve", SC["t2"], S5E[:, 1, :], s5, ALU.mult, ["S5E", "S5ROT"], ["sc_t2"])
        TT("dve", S5ST[:, l, 0, :], SC["t1"], SC["t2"], ALU.subtract, ["sc_t1", "sc_t2"], ["S5ST"])
        TT("dve", SC["t1"], S5E[:, 0, :], s5, ALU.mult, ["S5E", "S5ROT"], ["sc_t1"])
        TT("dve", SC["t2"], S5E[:, 1, :], c5, ALU.mult, ["S5E", "S5ROT"], ["sc_t2"])
        TT("dve", S5ST[:, l, 1, :], SC["t1"], SC["t2"], ALU.add, ["sc_t1", "sc_t2"], ["S5ST"])
        wb, wr = w_get(l, 11)
        YGR = [f"YG{q}" for q in range(4)]
        for mm in range(4):
            ps, pr = bank()
            MMG(ps[:, :], [(wb[:, kc * 512 + mm * 128:kc * 512 + (mm + 1) * 128], YG[:, kc, :]) for kc in range(4)], [wr] + YGR, [pr])
            ACT(GE[:], ps[:, :], AF.Sigmoid, [pr], ["GE"])
            TT("dve", YA[:, mm, :], YG[:, mm, :], GE[:], ALU.mult, ["GE", f"YG{mm}"], [f"YA{mm}"])
        if c == dump_c and l == 0:
            dump("ya", YA[:], [128, 4, TC], [f"YA{m}" for m in range(4)])
        refresh_state(l)
        for sc in range(NSUB):
            tsl = slice(sc * 128, (sc + 1) * 128)
            gcol = c * NSUB + sc
            ACT(SP_[:], G[:, sc, 4:8], AF.Exp, [f"G{sc}"], ["SP_"], scale=-1.0)
            ACT(SP_[:], SP_[:], AF.Ln, ["ONE1"], ["SP_"], bias=ONE1[:, 0:1])
            TS("dve", LI[:], G[:, sc, 0:4], PADB[:, gcol:gcol + 1], None, ALU.add, None, [f"G{sc}", "PADB"], ["LI"])
            for h in range(4):
                TS("dve", SPB[:, h, :], ONESF, SP_[:, h:h + 1], None, ALU.mult, None, ["CONS", "SP_"], ["SPB"])
            psF, prF = bank()
            MM(psF[:, 0:4], TRIU, SP_[:], ["CONS", "SP_"], [prF])
            MM(psF[:, 4:8], ONESF, SP_[:], ["CONS", "SP_"], [prF])
            psFr, prFr = bank()
            for h in range(4):
                MM(psFr[:, h * 128:(h + 1) * 128], SPB[:, h, :], TRIU, ["SPB", "CONS"], [prFr])
            TT("dve", BD[:], psF[:, 0:4], LI[:], ALU.add, [prF, "LI"], ["BD"])
            TT("dve", TW[:], BD[:], psF[:, 4:8], ALU.subtract, [prF, "BD"], ["TW"])
            ACT(WEX[:], TW[:], AF.Exp, ["TW"], ["WEX"])
            ACT(DEC[:], psF[:, 4:8], AF.Exp, [prF], ["DEC"], scale=-1.0)
            TS("dve", BD[:], BD[:], LN_SCALE, None, ALU.add, None, ["TW"], ["BD"])
            for h in range(4):
                hs = slice(h * 128, (h + 1) * 128)
                ACT(DT_[:, hs], psFr[:, hs], AF.Exp, [prFr, "BD"], ["DT_"], scale=-1.0, bias=BD[:, h:h + 1])
                TT("pool", DTM[:, hs], DT_[:, hs], TRIU, ALU.mult, ["DT_", "CONS"], ["DTM"])
            ACT(EF[:], psFr[:, :], AF.Exp, [prFr, "LNS"], ["EF"], scale=-1.0, bias=LNS[:, 0:1])
            for h in range(4):
                hs = slice(h * 128, (h + 1) * 128)
                TT("dve", QP[:, hs], QKT[:, h, tsl], EF[:, hs], ALU.mult, [f"QKT{h}", "EF"], ["QP"])
            psS, prS = bank()
            for h in range(4):
                MM(psS[:, h * 128:(h + 1) * 128], QKT[:, 4 + h, tsl], QKT[:, h, tsl], [f"QKT{4 + h}", f"QKT{h}"], [prS])
            TT("dve", WT[:], psS[:, :], DTM[:], ALU.mult, [prS, "DTM"], ["WT"])
            for h in range(4):
                P.op("pe", (lambda e, o=PST[:, h * 128:(h + 1) * 128], i=QKT[:, 4 + h, tsl]: e.transpose(o, i, IDB[:])),
                     [f"QKT{4 + h}", "IDB"], ["PST"])
            for h in range(4):
                hs = slice(h * 128, (h + 1) * 128)
                TS("dve", KW[:, hs], PST[:, hs], WEX[:, h:h + 1], None, ALU.mult, None, ["PST", "WEX"], ["KW"])
            psN, prN = bank()
            psD, prD = bank()
            for h in range(4):
                hs = slice(h * 128, (h + 1) * 128)
                MM(psN[:, hs], V[:, sc, h, 0:128], WT[:, hs], [f"V{sc}", "WT"], [prN], start=True, stop=False)
                MM(psN[:, hs], CTB[:, h, :], QP[:, hs], ["CTB", "QP"], [prN], start=False, stop=True)
                MM(psD[:, hs], ONESB[:], WT[:, hs], ["ONESB", "WT"], [prD], start=True, stop=False)
                MM(psD[:, hs], NB[:, h, :], QP[:, hs], ["NB", "QP"], [prD], start=False, stop=True)
            TS("dve", AD[:], psD[:, :], -1.0, 1.0, ALU.mult, ALU.max, [prD], ["T0"])
            TT("dve", AD[:], AD[:], psD[:, :], ALU.max, [prD], ["T0"])
            RECIP(AD[:], AD[:], [], ["T0"])
            TT("dve", HH[:], psN[:, :], AD[:], ALU.mult, [prN, "T0"], ["T1"])
            for half in range(2):
                psU, prU = bank()
                for hh in range(2):
                    h = half * 2 + hh
                    MM(psU[:, hh * 129:(hh + 1) * 129], KW[:, h * 128:(h + 1) * 128], V[:, sc, h, :], ["KW", f"V{sc}"], [prU])
                for hh in range(2):
                    h = half * 2 + hh
                    STT("dve", CT[:, l, h, :], CT[:, l, h, :], DEC[:, h:h + 1], psU[:, hh * 129:(hh + 1) * 129], ALU.mult, ALU.add, [prU, "DEC"], ["CT"])
            ACT(SQH[:], HH[:], AF.Square, ["T1"], ["SQH"])
            psH, prH = bank()
            for h in range(4):
                hs = slice(h * 128, (h + 1) * 128)
                MM(psH[:, hs], ONESB[:], SQH[:, hs], ["ONESB", "SQH"], [prH])
            TS("dve", RS[:], psH[:, :], 1.0 / 128, EPS, ALU.mult, ALU.add, [prH], ["T2"])
            ACT(RS[:], RS[:], AF.Sqrt, [], ["T2"])
            RECIP(RS[:], RS[:], [], ["T2"])
            for h in range(4):
                hs = slice(h * 128, (h + 1) * 128)
                STT("dve", HH[:, hs], HH[:, hs], sm(l, "ghead", h), RS[:, hs], ALU.mult, ALU.mult, ["T2", "SM"], ["T1"])
                TT("pool", YB[:, h, tsl], HH[:, hs], SIGO[:, h, tsl], ALU.mult, ["T1", f"SIGO{h}"], [f"YB{h}"])
            if sc < NSUB - 1:
                refresh_state(l)
        if c == dump_c and l == 0:
            dump("yb", YB[:], [128, 4, TC], [f"YB{m}" for m in range(4)])
        wbA, wrA = w_get(l, 12)
        wbBm, wrBm = w_get(l, 13)
        YAR = [f"YA{q}" for q in range(4)]
        YBR = [f"YB{q}" for q in range(4)]
        for mm in range(KD):
            psa, pra = bank()
            psb, prb = bank()
            MMG(psa[:, :], [(wbA[:, kc * 1024 + mm * 128:kc * 1024 + (mm + 1) * 128], YA[:, kc, :]) for kc in range(4)], [wrA] + YAR, [pra])
            MMG(psb[:, :], [(wbBm[:, kc * 1024 + mm * 128:kc * 1024 + (mm + 1) * 128], YB[:, kc, :]) for kc in range(4)], [wrBm] + YBR, [prb])
            TT("dve", T[0][:], psa[:, :], SIGA[mm], ALU.mult, [pra, f"SIGA{mm}"], ["T0"])
            TT("dve", T[1][:], psb[:, :], SIGB[mm], ALU.mult, [prb, f"SIGB{mm}"], ["T1"])
            TT("pool", MRG[mm], T[0][:], T[1][:], ALU.add, ["T0", "T1"], [f"MRG{mm}"])
        MR = [f"MRG{q}" for q in range(KD)]
        if c == dump_c and l == 0:
            dump("mrg", BIG[:, 2 * KD:3 * KD, :], [128, KD, TC], MR)
        for bi in range(2):
            wb, wr = w_get(l, 14 + bi)
            for mm in range(4):
                t = bi * 4 + mm
                ps, pr = bank()
                MMG(ps[:, :], [(wb[:, kc * 512 + mm * 128:kc * 512 + (mm + 1) * 128], MRG[kc]) for kc in range(KD)], [wr] + MR, [pr])
                ACT(OT[:, t, :], ps[:, :], AF.Copy, [pr], [f"OT{t}"])
        if c == dump_c and l == 0:
            dump("ot", OT[:], [128, KD, TC], [f"OT{t}" for t in range(KD)])
        post_norm_residual(l, "g_post")
        if c == dump_c and l == 0:
            dump("xmid", X[:], [128, KD, TC], ["X"])

    def ffn(c, l):
        pre_norm(l, "gf_pre")
        alias = [f"SIGA{t}" for t in range(KD)] + [f"SIGB{t}" for t in range(KD)] + [f"MRG{t}" for t in range(KD)]
        for bi in range(6):
            wbg, wrg = w_get(l, 16 + 2 * bi)
            wbu, wru = w_get(l, 17 + 2 * bi)
            ncols = 512 if bi < 5 else 256
            for mm in range(ncols // 128):
                j = bi * 4 + mm
                psg, prg = proj_tile(wbg, wrg, mm, ncols)
                psu, pru = proj_tile(wbu, wru, mm, ncols)
                o = _off["wfc"][0] + j * 3
                ACT(GP[:, 2:2 + TC], psg[:, :], AF.Copy, [prg], ["BB0", "BB1"])
                CP("pool", GP[:, 0:2], FH[:, l, j, :], ["FH"], ["BB0", "BB1"])
                ACT(ACC[:], GP[:, 0:TC], AF.Identity, ["BB0", "BB1", "SM"], ["T3"], scale=SM[:, l, o:o + 1], bias=sm(l, "bfc", j))
                STT("dve", ACC[:], GP[:, 1:1 + TC], SM[:, l, o + 1:o + 2], ACC[:], ALU.mult, ALU.add, ["BB0", "BB1", "SM"], ["T3"])
                STT("dve", ACC[:], GP[:, 2:2 + TC], SM[:, l, o + 2:o + 3], ACC[:], ALU.mult, ALU.add, ["BB0", "BB1", "SM"], ["T3"])
                CP("pool", FH[:, l, j, :], GP[:, TC:TC + 2], ["BB0", "BB1"], ["FH"])
                ACT(GE[:], ACC[:], AF.Gelu_apprx_tanh, ["T3"], ["GE"])
                TT("dve", ACTT[j], psu[:, :], GE[:], ALU.mult, [pru, "GE"], [f"ACTT{j}"] + alias)
        AR = [f"ACTT{j}" for j in range(NFT)]
        for mm in range(KD):
            wb, wr = w_get(l, 28 + mm)
            ps, pr = bank()
            MMG(ps[:, :], [(wb[:, j * 128:(j + 1) * 128], ACTT[j]) for j in range(NFT)], [wr] + AR, [pr])
            ACT(OT[:, mm, :], ps[:, :], AF.Copy, [pr], [f"OT{mm}"])
        post_norm_residual(l, "gf_post")
        P.op("act", lambda e: e.activation(out=GE[:, 0:1], in_=GE[:, 0:1], func=AF.Copy), [], AR + alias + ["GE"])

    for c in range(nch_run):
        P.dma("sp", X[:], xT[c], W=["X"])
        for l in range(DEPTH):
            mixer(c, l)
            ffn(c, l)
        P.dma("sp", yT[c], X[:], R=["X"], W=[f"yT{c}"])
    evs = [P.R(f"yT{c}").w for c in range(nch_run)] + list(dump_out.values())
    P.final_wait("sp", evs)
    P.emit()
    st.close()
    return nc


def _kblock(W, n):
    K = W.shape[0]
    kc = K // 128
    a = W.reshape(kc, 128, n).transpose(1, 0, 2).reshape(128, kc * n)
    out = np.zeros((128, 4096), np.float32)
    out[:, :kc * n] = a
    return out


def pack_weights(inp):
    wst = np.zeros((DEPTH, NBLK, 128, 4096), np.float32)
    small = np.zeros((128, DEPTH, NSMALL), np.float32)
    craw = np.zeros((DEPTH, 128, 16, 256), np.float32)

    def put(l, name, arr):
        o, w = _off[name]
        small[:, l, o:o + w] = arr

    for l in range(DEPTH):
        w_in = np.asarray(inp["w_in"][l], np.float32)
        for b, c0 in enumerate((0, 512, 1024, 1536, 2048)):
            wst[l, b] = _kblock(w_in[:, c0:c0 + 512], 512)
        for b, c0 in ((5, 2568), (6, 3080), (7, 3592), (8, 4104)):
            wst[l, b] = _kblock(w_in[:, c0:c0 + 512], 512)
        bre = np.asarray(inp["ssm_b_re"][l], np.float32)
        bim = np.asarray(inp["ssm_b_im"][l], np.float32)
        cre = np.asarray(inp["ssm_c_re"][l], np.float32)
        cim = np.asarray(inp["ssm_c_im"][l], np.float32)
        blkB = np.zeros((128, 16, 256), np.float32)
        for k in range(16):
            for gi in range(2):
                g = 2 * k + gi
                gg = g % 8
                blkB[gg * 16:(gg + 1) * 16, k, gi * 64:(gi + 1) * 64] = bre[g].T
                blkB[gg * 16:(gg + 1) * 16, k, 128 + gi * 64:128 + (gi + 1) * 64] = bim[g].T
                craw[l, gi * 64:(gi + 1) * 64, k, gg * 16:(gg + 1) * 16] = cre[g].T
                craw[l, gi * 64:(gi + 1) * 64, k, 128 + gg * 16:128 + (gg + 1) * 16] = cim[g].T
        wst[l, 9] = blkB.reshape(128, 4096)
        wst[l, 11] = _kblock(np.asarray(inp["w_ssm_glu"][l], np.float32), 512)
        wst[l, 12] = _kblock(np.asarray(inp["w_branch_ssm"][l], np.float32), 1024)
        wst[l, 13] = _kblock(np.asarray(inp["w_branch_mlstm"][l], np.float32), 1024)
        w_out = np.asarray(inp["w_out"][l], np.float32)
        wst[l, 14] = _kblock(w_out[:, 0:512], 512)
        wst[l, 15] = _kblock(w_out[:, 512:1024], 512)
        wg = np.asarray(inp["w_ffn_gate"][l], np.float32)
        wu = np.asarray(inp["w_ffn_up"][l], np.float32)
        for bi in range(6):
            n = 512 if bi < 5 else 256
            wst[l, 16 + 2 * bi] = _kblock(wg[:, bi * 512:bi * 512 + n], n)
            wst[l, 17 + 2 * bi] = _kblock(wu[:, bi * 512:bi * 512 + n], n)
        wd = np.asarray(inp["w_ffn_down"][l], np.float32)
        for m in range(8):
            wst[l, 28 + m] = _kblock(wd[:, m * 128:(m + 1) * 128], 128)
        put(l, "g_pre", np.asarray(inp["g_mix_pre"][l]).reshape(8, 128).T)
        put(l, "g_post", np.asarray(inp["g_mix_post"][l]).reshape(8, 128).T)
        put(l, "gf_pre", np.asarray(inp["g_ffn_pre"][l]).reshape(8, 128).T)
        put(l, "gf_post", np.asarray(inp["g_ffn_post"][l]).reshape(8, 128).T)
        put(l, "wgate", w_in[:, 2560:2568].reshape(8, 128, 8).transpose(1, 0, 2).reshape(128, 64))
        put(l, "bgate", np.broadcast_to(np.asarray(inp["b_gates"][l], np.float32)[None, :], (128, 8)))
        wqk = np.asarray(inp["w_qk_conv"][l], np.float32)
        put(l, "wqk", wqk.reshape(4, 8, 128).transpose(2, 1, 0).reshape(128, 32))
        put(l, "bqk", np.asarray(inp["b_qk_conv"][l]).reshape(8, 128).T)
        put(l, "ghead", np.asarray(inp["g_head_norm"][l]).reshape(4, 128).T)
        wfc = np.asarray(inp["w_ffn_conv"][l], np.float32)
        put(l, "wfc", wfc.reshape(3, 22, 128).transpose(2, 1, 0).reshape(128, 66))
        put(l, "bfc", np.asarray(inp["b_ffn_conv"][l]).reshape(22, 128).T)
        lre = np.asarray(inp["ssm_lambda_re"][l], np.float32)
        lim = np.asarray(inp["ssm_lambda_im"][l], np.float32)
        ldt = np.asarray(inp["ssm_log_dt"][l], np.float32)
        put(l, "lre", lre.reshape(16, 128).T)
        put(l, "lim", lim.reshape(16, 128).T)
        put(l, "ldt", np.repeat(ldt, 64).reshape(16, 128).T)
        put(l, "dvec", np.asarray(inp["ssm_d"][l], np.float32).reshape(4, 128).T)
    return wst, small, craw


def make_consts():
    cst = np.zeros((128, 128 * 3 + TC), np.float32)
    cst[:, 0:128] = np.eye(128, dtype=np.float32)
    cst[:, 128:256] = np.triu(np.ones((128, 128), np.float32))
    cst[:, 256:384] = 1.0
    cst[:, 384:] = np.arange(TC, dtype=np.float32)[None, :]
    return cst


def pack_tokens(x, meta, nch):
    seq = np.zeros((NCH * TC, D), np.float32)
    seq[PADF:PADF + NMETA] = meta
    seq[PADF + NMETA:] = x
    seq = seq[:nch * TC]
    xT = np.ascontiguousarray(seq.reshape(nch, TC, KD, 128).transpose(0, 3, 2, 1))
    tok = np.arange(nch * TC).reshape(nch * NSUB, 128).T
    padb = np.where(tok < PADF, np.float32(-30000.0), np.float32(0.0)).astype(np.float32)
    return xT, np.ascontiguousarray(padb)


_CACHE = {}


def run(inputs, nch, dumps=()):
    key = (nch, tuple(dumps))
    if key not in _CACHE:
        _CACHE[key] = build_program(nch, dumps)
    nc = _CACHE[key]
    wst, small, craw = pack_weights(inputs)
    xT, padb = pack_tokens(np.asarray(inputs["x"], np.float32)[0], np.asarray(inputs["meta_tokens"], np.float32), nch)
    in_map = {"xT": xT, "wst": wst, "small": small, "craw": craw, "consts": make_consts(), "padb": padb}
    res = run_bass_kernel_spmd(nc, [in_map], core_ids=[0])
    return res.results[0]


def kernel(**inputs):
    r = run(inputs, NCH)
    yT = r["yT"]
    seq = yT.transpose(0, 3, 2, 1).reshape(NCH * TC, D)
    out = seq[PADF + NMETA:].reshape(1, SEQ, D)
    return np.ascontiguousarray(out.astype(np.float32))
```

```python
import math
from contextlib import ExitStack
import numpy as np
import concourse.bass as bass
import concourse.mybir as mybir
from concourse.bass_utils import run_bass_kernel_spmd

F32 = mybir.dt.float32
BF16 = mybir.dt.bfloat16
I32 = mybir.dt.int32
AF = mybir.ActivationFunctionType
ALU = mybir.AluOpType

D = 1024
KD = 8
TC = 512
NSUB = 4
DEPTH = 4
NMETA = 16
SEQ = 16384
NCH = 33
PADF = NCH * TC - SEQ - NMETA
FFN = 2816
NFT = 22
NBLK = 36
NBUF = 4
EPS = 1e-6
LN_SCALE = math.log(128.0 ** -0.5)
TWO_PI = 2.0 * math.pi
CW1 = 6.28125
CW2 = TWO_PI - CW1
SEM_LIMIT = 30000
NO_SELF_WAIT = ("pe",)

_off = {}
_o = 0
for _n, _w in [("g_pre", 8), ("g_post", 8), ("gf_pre", 8), ("gf_post", 8), ("wgate", 64), ("bgate", 8),
               ("wqk", 32), ("bqk", 8), ("ghead", 4), ("wfc", 66), ("bfc", 22), ("lre", 16), ("lim", 16),
               ("ldt", 16), ("dvec", 4)]:
    _off[_n] = (_o, _w)
    _o += _w
NSMALL = _o


class Reg:
    __slots__ = ("w", "r", "name")

    def __init__(self, name=""):
        self.w = None
        self.r = []
        self.name = name


class Prog:
    ENGS = ("pe", "act", "dve", "pool", "sp")

    def __init__(self, nc, stack):
        self.nc = nc
        self.stack = stack
        self.q = {e: [] for e in self.ENGS}
        self.cnt = {e: 0 for e in self.ENGS}
        self.sems = {e: [stack.enter_context(nc.semaphore(f"s_{e}_0"))] for e in self.ENGS}
        self.waited = {e: {} for e in self.ENGS}
        self.dma_sems = [stack.enter_context(nc.semaphore(f"s_dma_{i}")) for i in range(8 + NBUF)]
        self.dma_cnt = [0] * (8 + NBUF)
        self.dma_rr = 0
        self.regs = {}
        self.nops = 0
        self.no_self_wait = set(NO_SELF_WAIT)
        self.own_sem_ids = {e: {id(self.sems[e][0])} for e in self.ENGS}

    def R(self, x):
        if isinstance(x, Reg):
            return x
        if x not in self.regs:
            self.regs[x] = Reg(x)
        return self.regs[x]

    def _collect(self, eng, R, W, extra=()):
        evs = list(extra)
        for r in R:
            if r.w is not None:
                evs.append(r.w)
        for w in W:
            if w.w is not None:
                evs.append(w.w)
            evs.extend(w.r)
        need = {}
        own = self.own_sem_ids[eng] if eng in self.no_self_wait else ()
        for (s, v) in evs:
            k = id(s)
            if k in own:
                continue
            if self.waited[eng].get(k, 0) >= v:
                continue
            if k not in need or need[k][1] < v:
                need[k] = (s, v)
        for k, (s, v) in need.items():
            self.waited[eng][k] = v
        return list(need.values())

    def _mark(self, ev, R, W):
        for r in R:
            r.r.append(ev)
        for w in W:
            w.w = ev
            w.r = []

    def op(self, eng, fn, R=(), W=()):
        R = [self.R(x) for x in R]
        W = [self.R(x) for x in W]
        waits = self._collect(eng, R, W)
        if self.cnt[eng] >= SEM_LIMIT:
            self.sems[eng].append(self.stack.enter_context(self.nc.semaphore(f"s_{eng}_{len(self.sems[eng])}")))
            self.own_sem_ids[eng].add(id(self.sems[eng][-1]))
            self.cnt[eng] = 0
        self.cnt[eng] += 1
        sem = self.sems[eng][-1]
        ev = (sem, self.cnt[eng])
        self.q[eng].append((waits, fn, (sem, 1)))
        self._mark(ev, R, W)
        self.nops += 1
        return ev

    def dma(self, eng, out, in_, R=(), W=(), sem_idx=None):
        R = [self.R(x) for x in R]
        W = [self.R(x) for x in W]
        if sem_idx is None:
            sem_idx = self.dma_rr
            self.dma_rr = (self.dma_rr + 1) % 8
        s = self.dma_sems[sem_idx]
        extra = [(s, self.dma_cnt[sem_idx])] if self.dma_cnt[sem_idx] else []
        waits = self._collect(eng, R, W, extra)
        self.dma_cnt[sem_idx] += 16
        ev = (s, self.dma_cnt[sem_idx])
        self.q[eng].append((waits, (lambda e, o=out, i=in_: e.dma_start(out=o, in_=i)), (s, 16)))
        self._mark(ev, R, W)
        return ev

    def final_wait(self, eng, evs):
        self.q[eng].append((list(evs), None, None))

    def emit(self):
        nc = self.nc
        with nc.Block() as block:
            def mk(name):
                def body(e):
                    for waits, fn, inc in self.q[name]:
                        for (s, v) in waits:
                            e.wait_ge(s, v)
                        if fn is not None:
                            ins = fn(e)
                            ins.then_inc(inc[0], inc[1])
                return body
            block.tensor(mk("pe"))
            block.scalar(mk("act"))
            block.vector(mk("dve"))
            block.gpsimd(mk("pool"))
            block.sync(mk("sp"))


def build_program(nch_run, dumps=()):
    nc = bass.Bass("TRN2", target_bir_lowering=False)
    xT = nc.dram_tensor("xT", [nch_run, 128, KD, TC], F32, kind="ExternalInput").ap()
    yT = nc.dram_tensor("yT", [nch_run, 128, KD, TC], F32, kind="ExternalOutput").ap()
    wst = nc.dram_tensor("wst", [DEPTH, NBLK, 128, 4096], F32, kind="ExternalInput").ap()
    small = nc.dram_tensor("small", [128, DEPTH, NSMALL], F32, kind="ExternalInput").ap()
    craw = nc.dram_tensor("craw", [DEPTH, 128, 16, 256], F32, kind="ExternalInput").ap()
    consts = nc.dram_tensor("consts", [128, 128 * 3 + TC], F32, kind="ExternalInput").ap()
    padb = nc.dram_tensor("padb", [128, nch_run * NSUB], F32, kind="ExternalInput").ap()
    tabs = nc.dram_tensor("tabs", [DEPTH, 16, 128, 2 * TC], F32).ap()
    cz = nc.dram_tensor("cz", [DEPTH, 128, 4096], F32).ap()
    dump_out = {}
    dump_c = 1 if nch_run > 1 else 0

    st = ExitStack()
    P = Prog(nc, st)

    def sb(name, shape, dt=F32):
        return st.enter_context(nc.sbuf_tensor(name, shape, dt))

    X = sb("X", [128, KD, TC])
    HT = sb("HT", [128, KD, TC], BF16)
    SQ = sb("SQ", [128, 2, TC], BF16)
    RSTD = sb("RSTD", [128, TC])
    OT = sb("OT", [128, KD, TC])
    WB = [sb(f"WB{i}", [128, 4096], BF16) for i in range(NBUF)]
    SM = sb("SM", [128, DEPTH, NSMALL])
    CONS = sb("CONS", [128, 128 * 3 + TC])
    PADB = sb("PADB", [128, nch_run * NSUB])
    IDB = sb("IDB", [128, 128], BF16)
    ONESB = sb("ONESB", [128, 128], BF16)
    WGB = sb("WGB", [128, DEPTH, 64], BF16)
    ONE1 = sb("ONE1", [128, 1])
    LNS = sb("LNS", [128, 1])
    UT = sb("UT", [128, 4, TC], BF16)
    QKPH = sb("QKPH", [128, DEPTH, 8, 3])
    QKP1 = sb("QKP1", [128, 2, 3 + TC])
    QKA = sb("QKA", [128, 2, TC])
    QKT = sb("QKT", [128, 8, TC], BF16)
    V = sb("V", [128, NSUB, 4, 129], BF16)
    SIGO = sb("SIGO", [128, 4, TC], BF16)
    BIG = sb("BIG", [128, 3 * KD, TC], BF16)
    G = sb("G", [128, NSUB, 8])
    YA = sb("YA", [128, 4, TC], BF16)
    YG = sb("YG", [128, 4, TC], BF16)
    YB = sb("YB", [128, 4, TC], BF16)
    S5DEC = sb("S5DEC", [128, DEPTH, 16])
    S5ROT = sb("S5ROT", [128, DEPTH, 2, 16])
    S5ST = sb("S5ST", [128, DEPTH, 2, 16])
    S5E = sb("S5E", [128, 2, 16])
    TAB = [sb(f"TAB{i}", [128, 2 * TC]) for i in range(2)]
    BB = sb("BB", [128, 2, TC])
    SS = sb("SS", [128, 2, TC])
    T = [sb(f"T{i}", [128, TC]) for i in range(4)]
    XR = [sb(f"XR{i}", [128, 2, TC], BF16) for i in range(2)]
    YS = sb("YS", [128, TC])
    CT = sb("CT", [128, DEPTH, 4, 129])
    CTB = sb("CTB", [128, 4, 128], BF16)
    NB = sb("NB", [128, 4, 128], BF16)
    SP_ = sb("SP_", [128, 4])
    LI = sb("LI", [128, 4])
    SPB = sb("SPB", [128, 4, 128])
    BD = sb("BD", [128, 4])
    TW = sb("TW", [128, 4])
    WEX = sb("WEX", [128, 4])
    DEC = sb("DEC", [128, 4])
    DT_ = sb("DT_", [128, TC])
    DTM = sb("DTM", [128, TC])
    EF = sb("EF", [128, TC])
    QP = sb("QP", [128, TC], BF16)
    WT = sb("WT", [128, TC], BF16)
    KW = sb("KW", [128, TC], BF16)
    SQH = sb("SQH", [128, TC], BF16)
    FH = sb("FH", [128, DEPTH, NFT, 2])
    GE = sb("GE", [128, TC])
    PRK = GE[:].bitcast(I32)
    SCN = ("dt", "th", "ar", "c", "s", "abr", "abi", "nr", "den", "t1", "t2", "zre", "zim", "a16", "c2", "s2", "nzim")
    SCT = sb("SCT", [128, len(SCN), 16])
    SC = {n: SCT[:, i, :] for i, n in enumerate(SCN)}
    SIGA = [BIG[:, t, :] for t in range(KD)]
    SIGB = [BIG[:, KD + t, :] for t in range(KD)]
    MRG = [BIG[:, 2 * KD + t, :] for t in range(KD)]
    ACTT = [BIG[:, j, :] for j in range(NFT)]
    HTF = HT[:].rearrange("p a t -> p (a t)").bitcast(F32)
    AD, HH, RS = HTF[:, 0:TC], HTF[:, TC:2 * TC], HTF[:, 2 * TC:3 * TC]
    ADR, HHR, RSR = ["HT0", "HT1"], ["HT2", "HT3"], ["HT4", "HT5"]
    ACC = T[3]
    GP = BB[:].rearrange("p a t -> p (a t)")

    PS = [st.enter_context(nc.psum_tensor(f"PS{i}", [128, TC], F32)) for i in range(7)]
    PST = st.enter_context(nc.psum_tensor("PST", [128, TC], BF16))
    PSR = [Reg(f"ps{i}") for i in range(7)]
    bank_rr = [0]

    def bank():
        i = bank_rr[0]
        bank_rr[0] = (i + 1) % 6
        return PS[i], PSR[i]

    def ACT(out, in_, func, R, W, **kw):
        P.op("act", lambda e: e.activation(out=out, in_=in_, func=func, **kw), R, W)

    def TT(eng, out, in0, in1, op, R, W):
        P.op(eng, lambda e: e.tensor_tensor(out=out, in0=in0, in1=in1, op=op), R, W)

    def TS(eng, out, in0, s1, s2, op0, op1, R, W):
        if s2 is None:
            P.op(eng, lambda e: e.tensor_scalar(out=out, in0=in0, scalar1=s1, scalar2=None, op0=op0), R, W)
        else:
            P.op(eng, lambda e: e.tensor_scalar(out=out, in0=in0, scalar1=s1, scalar2=s2, op0=op0, op1=op1), R, W)

    def STT(eng, out, in0, scalar, in1, op0, op1, R, W):
        P.op(eng, lambda e: e.scalar_tensor_tensor(out=out, in0=in0, scalar=scalar, in1=in1, op0=op0, op1=op1), R, W)

    def CP(eng, out, in_, R, W):
        P.op(eng, lambda e: e.tensor_copy(out=out, in_=in_), R, W)

    def RECIP(out, in_, R, W):
        P.op("dve", lambda e: e.reciprocal(out=out, in_=in_), R, W)

    def MM(out, lhsT, rhs, R, W, start=True, stop=True):
        P.op("pe", lambda e: e.matmul(out, lhsT=lhsT, rhs=rhs, start=start, stop=stop), R, W)

    def MMG(out, pairs, R, W):
        pairs = list(pairs)

        def fn(e):
            ins = None
            n = len(pairs)
            for i, (a, b) in enumerate(pairs):
                ins = e.matmul(out, lhsT=a, rhs=b, start=(i == 0), stop=(i == n - 1))
            return ins
        P.op("pe", fn, R, W)

    def dump(name, ap, shape, R):
        if name in dumps:
            t = nc.dram_tensor("dbg_" + name, list(shape), F32 if ap.dtype == F32 else BF16, kind="ExternalOutput").ap()
            dump_out[name] = P.dma("sp", t, ap, R=R, W=["dbg_" + name])

    P.dma("sp", SM[:], small[:, :, :], W=["SM"])
    P.dma("sp", CONS[:], consts[:, :], W=["CONS"])
    P.dma("sp", PADB[:], padb[:, :], W=["PADB"])
    IDF = CONS[:, 0:128]
    TRIU = CONS[:, 128:256]
    ONESF = CONS[:, 256:384]
    IOTA = CONS[:, 384:384 + TC]
    CP("dve", IDB[:], IDF, ["CONS"], ["IDB"])
    CP("dve", ONESB[:], ONESF, ["CONS"], ["ONESB"])
    for l in range(DEPTH):
        o = _off["wgate"][0]
        CP("dve", WGB[:, l, :], SM[:, l, o:o + 64], ["SM"], ["WGB"])
    P.op("pool", lambda e: e.memset(V[:], 1.0), W=["V0", "V1", "V2", "V3"])
    P.op("pool", lambda e: e.memset(CT[:], 0.0), W=["CT"])
    P.op("pool", lambda e: e.memset(S5ST[:], 0.0), W=["S5ST"])
    P.op("pool", lambda e: e.memset(QKPH[:], 0.0), W=["QKPH"])
    P.op("pool", lambda e: e.memset(FH[:], 0.0), W=["FH"])
    P.op("pool", lambda e: e.memset(ONE1[:], 1.0), W=["ONE1"])
    P.op("pool", lambda e: e.memset(LNS[:], LN_SCALE), W=["LNS"])

    def sm(l, name, j=None):
        o, w = _off[name]
        if j is None:
            return SM[:, l, o:o + w]
        return SM[:, l, o + j:o + j + 1]

    PRC = OT[:].rearrange("p a t -> p (a t)")
    PRZ = X[:].rearrange("p a t -> p (a t)")
    PRA, PRF, PRT0, PRT1 = T[0], T[1], T[2], T[3]

    OTR = [f"OT{t}" for t in range(KD)]

    def prologue_layer(l):
        s = SC
        lre, lim, ldt = sm(l, "lre"), sm(l, "lim"), sm(l, "ldt")
        dec = S5DEC[:, l, :]
        def exp_taylor(dst, dreg, src, sreg):
            TS("dve", dst, src, 1.0 / 5040, 1.0 / 720, ALU.mult, ALU.add, [sreg], [dreg])
            for cf in (1.0 / 120, 1.0 / 24, 1.0 / 6, 0.5, 1.0, 1.0):
                TT("dve", dst, dst, src, ALU.mult, [sreg], [dreg])
                TS("dve", dst, dst, cf, None, ALU.add, None, [], [dreg])

        def sin_reduced(dst, dreg, ang, areg, shift):
            TS("dve", s["t1"], ang, shift, 1.0 / TWO_PI, ALU.add, ALU.mult, [areg], ["sc_t1"])
            CP("dve", PRK[:, 0:16], s["t1"], ["sc_t1"], ["PRK"])
            CP("dve", s["t1"], PRK[:, 0:16], ["PRK"], ["sc_t1"])
            STT("dve", s["t2"], s["t1"], -CW1, ang, ALU.mult, ALU.add, ["sc_t1", areg], ["sc_t2"])
            STT("dve", s["t2"], s["t1"], -CW2, s["t2"], ALU.mult, ALU.add, ["sc_t1"], ["sc_t2"])
            TS("dve", s["t2"], s["t2"], shift, -math.pi, ALU.add, ALU.max, [], ["sc_t2"])
            TS("dve", s["t2"], s["t2"], math.pi, None, ALU.min, None, [], ["sc_t2"])
            ACT(dst, s["t2"], AF.Sin, ["sc_t2"], [dreg])

        TS("dve", s["a16"], ldt, 1.0 / 16, None, ALU.mult, None, ["SM"], ["sc_a16"])
        exp_taylor(s["dt"], "sc_dt", s["a16"], "sc_a16")
        for _ in range(4):
            TT("dve", s["dt"], s["dt"], s["dt"], ALU.mult, [], ["sc_dt"])
        TT("dve", s["ar"], lre, s["dt"], ALU.mult, ["SM", "sc_dt"], ["sc_ar"])
        TT("dve", s["th"], lim, s["dt"], ALU.mult, ["SM", "sc_dt"], ["sc_th"])
        exp_taylor(dec, "S5DEC", s["ar"], "sc_ar")
        sin_reduced(s["s"], "sc_s", s["th"], "sc_th", 0.0)
        sin_reduced(s["c"], "sc_c", s["th"], "sc_th", math.pi / 2)
        TT("dve", s["abr"], dec, s["c"], ALU.mult, ["S5DEC", "sc_c"], ["sc_abr"])
        TT("dve", s["abi"], dec, s["s"], ALU.mult, ["S5DEC", "sc_s"], ["sc_abi"])
        TS("dve", s["nr"], s["abr"], -1.0, None, ALU.add, None, ["sc_abr"], ["sc_nr"])
        TT("dve", s["t1"], lre, lre, ALU.mult, ["SM"], ["sc_t1"])
        TT("dve", s["t2"], lim, lim, ALU.mult, ["SM"], ["sc_t2"])
        TT("dve", s["den"], s["t1"], s["t2"], ALU.add, ["sc_t1", "sc_t2"], ["sc_den"])
        RECIP(s["den"], s["den"], ["sc_den"], ["sc_den"])
        TT("dve", s["t1"], s["nr"], lre, ALU.mult, ["sc_nr", "SM"], ["sc_t1"])
        TT("dve", s["t2"], s["abi"], lim, ALU.mult, ["sc_abi", "SM"], ["sc_t2"])
        TT("dve", s["t1"], s["t1"], s["t2"], ALU.add, ["sc_t1", "sc_t2"], ["sc_t1"])
        TT("dve", s["zre"], s["t1"], s["den"], ALU.mult, ["sc_t1", "sc_den"], ["sc_zre"])
        TT("dve", s["t1"], s["abi"], lre, ALU.mult, ["sc_abi", "SM"], ["sc_t1"])
        TT("dve", s["t2"], s["nr"], lim, ALU.mult, ["sc_nr", "SM"], ["sc_t2"])
        TT("dve", s["t1"], s["t1"], s["t2"], ALU.subtract, ["sc_t1", "sc_t2"], ["sc_t1"])
        TT("dve", s["zim"], s["t1"], s["den"], ALU.mult, ["sc_t1", "sc_den"], ["sc_zim"])
        TS("dve", s["nzim"], s["zim"], -1.0, None, ALU.mult, None, ["sc_zim"], ["sc_nzim"])
        P.dma("sp", PRC, craw[l].rearrange("p a t -> p (a t)"), W=OTR)
        for k in range(16):
            cre, cim = PRC[:, k * 256:k * 256 + 128], PRC[:, k * 256 + 128:k * 256 + 256]
            o1, o2 = PRZ[:, k * 256:k * 256 + 128], PRZ[:, k * 256 + 128:k * 256 + 256]
            TS("dve", o1, cim, s["nzim"][:, k:k + 1], None, ALU.mult, None, OTR + ["sc_nzim"], ["X"])
            STT("dve", o1, cre, s["zre"][:, k:k + 1], o1, ALU.mult, ALU.add, OTR + ["sc_zre"], ["X"])
            TS("dve", o2, cim, s["zre"][:, k:k + 1], -1.0, ALU.mult, ALU.mult, OTR + ["sc_zre"], ["X"])
            STT("dve", o2, cre, s["nzim"][:, k:k + 1], o2, ALU.mult, ALU.add, OTR + ["sc_nzim"], ["X"])
        P.dma("sp", cz[l], PRZ, R=["X"], W=[f"cz{l}"])
        for k in range(16):
            th = s["th"][:, k:k + 1]
            TS("dve", PRA[:], IOTA, th, None, ALU.mult, None, ["CONS", "sc_th"], ["T0"])
            tb = TAB[k % 2]
            for half, shift in ((1, 0.0), (0, math.pi / 2)):
                TS("dve", PRF[:], PRA[:], shift, 1.0 / TWO_PI, ALU.add, ALU.mult, ["T0"], ["T1"])
                CP("dve", PRK[:], PRF[:], ["T1"], ["PRK"])
                CP("dve", PRF[:], PRK[:], ["PRK"], ["T1"])
                STT("dve", PRT0[:], PRF[:], -CW1, PRA[:], ALU.mult, ALU.add, ["T1", "T0"], ["T2"])
                STT("dve", PRT0[:], PRF[:], -CW2, PRT0[:], ALU.mult, ALU.add, ["T1", "T2"], ["T2"])
                TS("dve", PRT0[:], PRT0[:], shift, -math.pi, ALU.add, ALU.max, ["T2"], ["T2"])
                TS("dve", PRT0[:], PRT0[:], math.pi, None, ALU.min, None, ["T2"], ["T2"])
                ACT(tb[:, half * TC:(half + 1) * TC], PRT0[:], AF.Sin, ["T2"], [f"TAB{k % 2}"])
            c1, s1 = s["c"][:, k:k + 1], s["s"][:, k:k + 1]
            c511, s511 = tb[:, TC - 1:TC], tb[:, 2 * TC - 1:2 * TC]
            tr = f"TAB{k % 2}"
            TT("dve", s["c2"][:, k:k + 1], c511, c1, ALU.mult, [tr, "sc_c"], ["sc_c2"])
            TT("dve", s["s2"][:, k:k + 1], s511, s1, ALU.mult, [tr, "sc_s"], ["sc_s2"])
            TT("dve", S5ROT[:, l, 0, k:k + 1], s["c2"][:, k:k + 1], s["s2"][:, k:k + 1], ALU.subtract, ["sc_c2", "sc_s2"], ["S5ROT"])
            TT("dve", s["c2"][:, k:k + 1], s511, c1, ALU.mult, [tr, "sc_c"], ["sc_c2"])
            TT("dve", s["s2"][:, k:k + 1], c511, s1, ALU.mult, [tr, "sc_s"], ["sc_s2"])
            TT("dve", S5ROT[:, l, 1, k:k + 1], s["c2"][:, k:k + 1], s["s2"][:, k:k + 1], ALU.add, ["sc_c2", "sc_s2"], ["S5ROT"])
            P.dma("sp", tabs[l, k], tb[:], R=[tr], W=[f"tab{l}_{k}", tr])

    for l in range(DEPTH):
        prologue_layer(l)

    wq = [(l, b) for c in range(nch_run) for l in range(DEPTH) for b in range(NBLK) if b != -1]
    wstate = {"next": 0, "use": 0}
    WBR = [Reg(f"wb{i}") for i in range(NBUF)]

    def w_issue():
        i = wstate["next"]
        l, b = wq[i]
        buf = i % NBUF
        if b == 10:
            P.dma("pool", WB[buf][:], cz[l], R=[f"cz{l}"], W=[WBR[buf]], sem_idx=8 + buf)
        else:
            P.dma("pool", WB[buf][:], wst[l, b], W=[WBR[buf]], sem_idx=8 + buf)
        wstate["next"] += 1

    def w_get(l, b):
        i = wstate["use"]
        assert wq[i] == (l, b), (wq[i], l, b)
        wstate["use"] += 1
        while wstate["next"] < min(len(wq), i + NBUF - 1):
            w_issue()
        return WB[i % NBUF], WBR[i % NBUF]

    HTR = [f"HT{kt}" for kt in range(KD)]

    def rmsnorm_stats(src_tile, src_reg):
        ps, pr = bank()
        for kt in range(KD):
            ACT(SQ[:, kt % 2, :], src_tile(kt), AF.Square, [src_reg(kt)], [f"SQ{kt % 2}"])
            MM(ps[:, :], ONESB[:], SQ[:, kt % 2, :], ["ONESB", f"SQ{kt % 2}"], [pr], start=(kt == 0), stop=(kt == KD - 1))
        TS("dve", RSTD[:], ps[:, :], 1.0 / D, EPS, ALU.mult, ALU.add, [pr], ["RSTD"])
        ACT(RSTD[:], RSTD[:], AF.Sqrt, ["RSTD"], ["RSTD"])
        RECIP(RSTD[:], RSTD[:], ["RSTD"], ["RSTD"])

    def pre_norm(l, gname):
        rmsnorm_stats(lambda kt: X[:, kt, :], lambda kt: "X")
        for kt in range(KD):
            STT("dve", HT[:, kt, :], X[:, kt, :], sm(l, gname, kt), RSTD[:], ALU.mult, ALU.mult, ["X", "RSTD", "SM"], [f"HT{kt}"])

    def post_norm_residual(l, gname):
        rmsnorm_stats(lambda kt: OT[:, kt, :], lambda kt: f"OT{kt}")
        for kt in range(KD):
            STT("dve", OT[:, kt, :], OT[:, kt, :], sm(l, gname, kt), RSTD[:], ALU.mult, ALU.mult, ["RSTD", "SM"], [f"OT{kt}"])
            TT("pool", X[:, kt, :], X[:, kt, :], OT[:, kt, :], ALU.add, [f"OT{kt}"], ["X"])

    def proj_tile(wb, wr, mm, ncols=512):
        ps, pr = bank()
        MMG(ps[:, :], [(wb[:, kc * ncols + mm * 128: kc * ncols + (mm + 1) * 128], HT[:, kc, :]) for kc in range(KD)], [wr] + HTR, [pr])
        return ps, pr

    def refresh_state(l):
        ACT(CTB[:], CT[:, l, :, 0:128], AF.Copy, ["CT"], ["CTB"])
        for h in range(4):
            TS("pool", NB[:, h, :], ONESB[:], CT[:, l, h, 128:129], None, ALU.mult, None, ["ONESB", "CT"], ["NB"])

    def mixer(c, l):
        pre_norm(l, "g_pre")
        if c == dump_c and l == 0:
            dump("ht", HT[:], [128, KD, TC], HTR)
        wb, wr = w_get(l, 0)
        for mm in range(4):
            ps, pr = proj_tile(wb, wr, mm)
            ACT(UT[:, mm, :], ps[:, :], AF.Copy, [pr], [f"UT{mm}"])
        for blk, base in ((1, 0), (2, 4)):
            wb, wr = w_get(l, blk)
            for mm in range(4):
                t = base + mm
                pb = t % 2
                ps, pr = proj_tile(wb, wr, mm)
                rq = f"QKP1_{pb}"
                ACT(QKP1[:, pb, 3:3 + TC], ps[:, :], AF.Copy, [pr], [rq])
                CP("pool", QKP1[:, pb, 0:3], QKPH[:, l, t, :], ["QKPH"], [rq])
                o = _off["wqk"][0] + t * 4
                ACT(QKA[:, pb, :], QKP1[:, pb, 0:TC], AF.Identity, [rq, "SM"], [f"QKA{pb}"], scale=SM[:, l, o:o + 1], bias=sm(l, "bqk", t))
                for j in (1, 2, 3):
                    STT("dve", QKA[:, pb, :], QKP1[:, pb, j:j + TC], SM[:, l, o + j:o + j + 1], QKA[:, pb, :], ALU.mult, ALU.add, [rq, "SM"], [f"QKA{pb}"])
                ACT(QKT[:, t, :], QKA[:, pb, :], AF.Silu, [f"QKA{pb}"], [f"QKT{t}"])
                CP("pool", QKPH[:, l, t, :], QKP1[:, pb, TC:TC + 3], [rq], ["QKPH"])
        wb, wr = w_get(l, 3)
        for sc in range(NSUB):
            ps, pr = bank()
            MMG(ps[:, :], [(HT[:, kc, sc * 128:(sc + 1) * 128], wb[:, kc * 512:(kc + 1) * 512]) for kc in range(KD)], [wr] + HTR, [pr])
            for h in range(4):
                ACT(V[:, sc, h, 0:128], ps[:, h * 128:(h + 1) * 128], AF.Copy, [pr], [f"V{sc}"])
        for sc in range(NSUB):
            ps, pr = bank()
            MMG(ps[:, 0:8], [(HT[:, kc, sc * 128:(sc + 1) * 128], WGB[:, l, kc * 8:kc * 8 + 8]) for kc in range(KD)], ["WGB"] + HTR, [pr])
            TT("dve", G[:, sc, :], ps[:, 0:8], sm(l, "bgate"), ALU.add, [pr, "SM"], [f"G{sc}"])
        wb, wr = w_get(l, 4)
        for mm in range(4):
            ps, pr = proj_tile(wb, wr, mm)
            ACT(SIGO[:, mm, :], ps[:, :], AF.Sigmoid, [pr], [f"SIGO{mm}"])
        for dst, nm, blks in ((SIGA, "SIGA", (5, 6)), (SIGB, "SIGB", (7, 8))):
            for bi, blk in enumerate(blks):
                wb, wr = w_get(l, blk)
                for mm in range(4):
                    t = bi * 4 + mm
                    ps, pr = proj_tile(wb, wr, mm)
                    ACT(dst[t], ps[:, :], AF.Sigmoid, [pr], [f"{nm}{t}"])
        if c == dump_c and l == 0:
            dump("ut", UT[:], [128, 4, TC], [f"UT{m}" for m in range(4)])
            dump("qkt", QKT[:], [128, 8, TC], [f"QKT{m}" for m in range(8)])
        wbB, wrB = w_get(l, 9)
        wbC, wrC = w_get(l, 10)
        ys_ps, ys_pr = PS[6], PSR[6]

        def s5_front(k):
            q4 = k // 4
            tb, tbr = TAB[k % 2], f"TAB{k % 2}"
            P.dma("sp", tb[:], tabs[l, k], R=[f"tab{l}_{k}"], W=[tbr])
            cs, sn = tb[:, 0:TC], tb[:, TC:2 * TC]
            psr_, prr = bank()
            psi_, pri = bank()
            MM(psr_[:, :], wbB[:, k * 256:k * 256 + 128], UT[:, q4, :], [wrB, f"UT{q4}"], [prr])
            MM(psi_[:, :], wbB[:, k * 256 + 128:k * 256 + 256], UT[:, q4, :], [wrB, f"UT{q4}"], [pri])
            TT("dve", T[0][:], psr_[:, :], cs, ALU.mult, [prr, tbr], ["T0"])
            TT("dve", T[1][:], psi_[:, :], sn, ALU.mult, [pri, tbr], ["T1"])
            TT("dve", BB[:, 0, :], T[0][:], T[1][:], ALU.add, [], ["BB0", "T0", "T1"])
            TT("dve", T[0][:], psi_[:, :], cs, ALU.mult, [pri, tbr], ["T0"])
            TT("dve", T[1][:], psr_[:, :], sn, ALU.mult, [prr, tbr], ["T1"])
            TT("dve", BB[:, 1, :], T[0][:], T[1][:], ALU.subtract, [], ["BB1", "T0", "T1"])
            ssb = SS if k % 2 == 0 else QKA
            ssr = ("SS0", "SS1") if k % 2 == 0 else ("QKA0", "QKA1")
            for ri in range(2):
                dec_b = S5DEC[:, l, k:k + 1].to_broadcast([128, TC])
                P.op("dve", (lambda e, ri=ri, dec_b=dec_b, init=S5ST[:, l, ri, k:k + 1], o=ssb[:, ri, :]:
                             e.tensor_tensor_scan(out=o, data0=dec_b, data1=BB[:, ri, :], initial=init, op0=ALU.mult, op1=ALU.add)),
                     ["S5DEC", "S5ST", f"BB{ri}"], [ssr[ri]])
                ACT(S5E[:, ri, k:k + 1], ssb[:, ri, TC - 1:TC], AF.Copy, [ssr[ri]], ["S5E"])

        def s5_back(k):
            q4 = k // 4
            tb, tbr = TAB[k % 2], f"TAB{k % 2}"
            cs, sn = tb[:, 0:TC], tb[:, TC:2 * TC]
            ssb = SS if k % 2 == 0 else QKA
            ssr = ("SS0", "SS1") if k % 2 == 0 else ("QKA0", "QKA1")
            xr, xrr = XR[k % 2], f"XR{k % 2}"
            TT("pool", T[2][:], ssb[:, 0, :], cs, ALU.mult, [ssr[0], tbr], ["T2"])
            TT("pool", T[3][:], ssb[:, 1, :], sn, ALU.mult, [ssr[1], tbr], ["T3"])
            TT("pool", xr[:, 0, :], T[2][:], T[3][:], ALU.subtract, [], [xrr, "T2", "T3"])
            TT("pool", T[2][:], ssb[:, 0, :], sn, ALU.mult, [ssr[0], tbr], ["T2"])
            TT("pool", T[3][:], ssb[:, 1, :], cs, ALU.mult, [ssr[1], tbr], ["T3"])
            TT("pool", xr[:, 1, :], T[2][:], T[3][:], ALU.add, [], [xrr, "T2", "T3"])
            MM(ys_ps[:, :], wbC[:, k * 256:k * 256 + 128], xr[:, 0, :], [wrC, xrr], [ys_pr], start=(k % 4 == 0), stop=False)
            MM(ys_ps[:, :], wbC[:, k * 256 + 128:k * 256 + 256], xr[:, 1, :], [wrC, xrr], [ys_pr], start=False, stop=(k % 4 == 3))
            if k % 4 == 3:
                STT("dve", YS[:], UT[:, q4, :], sm(l, "dvec", q4), ys_ps[:, :], ALU.mult, ALU.add, [f"UT{q4}", "SM", ys_pr], ["YS"])
                if c == dump_c and l == 0 and q4 == 0:
                    dump("ys", YS[:], [128, TC], ["YS"])
                ACT(YG[:, q4, :], YS[:], AF.Gelu_apprx_tanh, ["YS"], [f"YG{q4}"])

        def mlstm_gen():
            refresh_state(l)
            yield
            for sc in range(NSUB):
                tsl = slice(sc * 128, (sc + 1) * 128)
                gcol = c * NSUB + sc
                ACT(SP_[:], G[:, sc, 4:8], AF.Exp, [f"G{sc}"], ["SP_"], scale=-1.0)
                ACT(SP_[:], SP_[:], AF.Ln, ["ONE1"], ["SP_"], bias=ONE1[:, 0:1])
                TS("dve", LI[:], G[:, sc, 0:4], PADB[:, gcol:gcol + 1], None, ALU.add, None, [f"G{sc}", "PADB"], ["LI"])
                yield
                for h in range(4):
                    TS("dve", SPB[:, h, :], ONESF, SP_[:, h:h + 1], None, ALU.mult, None, ["CONS", "SP_"], ["SPB"])
                psF, prF = bank()
                MM(psF[:, 0:4], TRIU, SP_[:], ["CONS", "SP_"], [prF])
                MM(psF[:, 4:8], ONESF, SP_[:], ["CONS", "SP_"], [prF])
                yield
                psFr, prFr = bank()
                for h in range(4):
                    MM(psFr[:, h * 128:(h + 1) * 128], SPB[:, h, :], TRIU, ["SPB", "CONS"], [prFr])
                TT("dve", BD[:], psF[:, 0:4], LI[:], ALU.add, [prF, "LI"], ["BD"])
                TT("dve", TW[:], BD[:], psF[:, 4:8], ALU.subtract, [prF, "BD"], ["TW"])
                ACT(WEX[:], TW[:], AF.Exp, ["TW"], ["WEX"])
                ACT(DEC[:], psF[:, 4:8], AF.Exp, [prF], ["DEC"], scale=-1.0)
                TS("dve", BD[:], BD[:], LN_SCALE, None, ALU.add, None, ["TW"], ["BD"])
                yield
                for h in range(4):
                    hs = slice(h * 128, (h + 1) * 128)
                    ACT(DT_[:, hs], psFr[:, hs], AF.Exp, [prFr, "BD"], ["DT_"], scale=-1.0, bias=BD[:, h:h + 1])
                    TT("pool", DTM[:, hs], DT_[:, hs], TRIU, ALU.mult, ["DT_", "CONS"], ["DTM"])
                ACT(EF[:], psFr[:, :], AF.Exp, [prFr, "LNS"], ["EF"], scale=-1.0, bias=LNS[:, 0:1])
                psS, prS = bank()
                for h in range(4):
                    MM(psS[:, h * 128:(h + 1) * 128], QKT[:, 4 + h, tsl], QKT[:, h, tsl], [f"QKT{4 + h}", f"QKT{h}"], [prS])
                for h in range(4):
                    P.op("pe", (lambda e, o=PST[:, h * 128:(h + 1) * 128], i=QKT[:, 4 + h, tsl]: e.transpose(o, i, IDB[:])),
                         [f"QKT{4 + h}", "IDB"], ["PST"])
                yield
                TT("dve", QP[:].rearrange("p (h t) -> p h t", h=4), QKT[:, 0:4, tsl], EF[:].rearrange("p (h t) -> p h t", h=4), ALU.mult,
                   [f"QKT{h}" for h in range(4)] + ["EF"], ["QP"])
                TT("dve", WT[:], psS[:, :], DTM[:], ALU.mult, [prS, "DTM"], ["WT"])
                for h in range(4):
                    hs = slice(h * 128, (h + 1) * 128)
                    TS("dve", KW[:, hs], PST[:, hs], WEX[:, h:h + 1], None, ALU.mult, None, ["PST", "WEX"], ["KW"])
                yield
                psN, prN = bank()
                psD, prD = bank()
                for h in range(4):
                    hs = slice(h * 128, (h + 1) * 128)
                    MM(psN[:, hs], V[:, sc, h, 0:128], WT[:, hs], [f"V{sc}", "WT"], [prN], start=True, stop=False)
                    MM(psN[:, hs], CTB[:, h, :], QP[:, hs], ["CTB", "QP"], [prN], start=False, stop=True)
                    MM(psD[:, hs], ONESB[:], WT[:, hs], ["ONESB", "WT"], [prD], start=True, stop=False)
                    MM(psD[:, hs], NB[:, h, :], QP[:, hs], ["NB", "QP"], [prD], start=False, stop=True)
                psU = []
                for half in range(2):
                    pu, pru = bank()
                    psU.append((pu, pru))
                    for hh in range(2):
                        h = half * 2 + hh
                        MM(pu[:, hh * 129:(hh + 1) * 129], KW[:, h * 128:(h + 1) * 128], V[:, sc, h, :], ["KW", f"V{sc}"], [pru])
                yield
                TS("dve", AD, psD[:, :], -1.0, 1.0, ALU.mult, ALU.max, [prD], ADR)
                TT("dve", AD, AD, psD[:, :], ALU.max, [prD], ADR)
                RECIP(AD, AD, [], ADR)
                TT("dve", HH, psN[:, :], AD, ALU.mult, [prN] + ADR, HHR)
                for half in range(2):
                    pu, pru = psU[half]
                    for hh in range(2):
                        h = half * 2 + hh
                        STT("dve", CT[:, l, h, :], CT[:, l, h, :], DEC[:, h:h + 1], pu[:, hh * 129:(hh + 1) * 129], ALU.mult, ALU.add, [pru, "DEC"], ["CT"])
                yield
                ACT(SQH[:], HH, AF.Square, HHR, ["SQH"])
                if sc < NSUB - 1:
                    refresh_state(l)
                psH, prH = bank()
                for h in range(4):
                    hs = slice(h * 128, (h + 1) * 128)
                    MM(psH[:, hs], ONESB[:], SQH[:, hs], ["ONESB", "SQH"], [prH])
                yield
                TS("dve", RS, psH[:, :], 1.0 / 128, EPS, ALU.mult, ALU.add, [prH], RSR)
                ACT(RS, RS, AF.Sqrt, [], RSR)
                RECIP(RS, RS, [], RSR)
                yield
                for h in range(4):
                    hs = slice(h * 128, (h + 1) * 128)
                    STT("dve", HH[:, hs], HH[:, hs], sm(l, "ghead", h), RS[:, hs], ALU.mult, ALU.mult, RSR + ["SM"], HHR)
                TT("pool", YB[:, :, tsl], HH.rearrange("p (h t) -> p h t", h=4), SIGO[:, :, tsl], ALU.mult,
                   HHR + [f"SIGO{h}" for h in range(4)], [f"YB{h}" for h in range(4)])
                yield

        mg = mlstm_gen()

        def pull(n):
            for _ in range(n):
                try:
                    next(mg)
                except StopIteration:
                    return
        s5_front(0)
        pull(3)
        for k in range(1, 16):
            s5_front(k)
            s5_back(k - 1)
            pull(3)
        s5_back(15)
        for _ in mg:
            pass
        c5, s5 = S5ROT[:, l, 0, :], S5ROT[:, l, 1, :]
        TT("dve", SC["t1"], S5E[:, 0, :], c5, ALU.mult, ["S5E", "S5ROT"], ["sc_t1"])
        TT("dve", SC["t2"], S5E[:, 1, :], s5, ALU.mult, ["S5E", "S5ROT"], ["sc_t2"])
        TT("dve", S5ST[:, l, 0, :], SC["t1"], SC["t2"], ALU.subtract, ["sc_t1", "sc_t2"], ["S5ST"])
        TT("dve", SC["t1"], S5E[:, 0, :], s5, ALU.mult, ["S5E", "S5ROT"], ["sc_t1"])
        TT("dve", SC["t2"], S5E[:, 1, :], c5, ALU.mult, ["S5E", "S5ROT"], ["sc_t2"])
        TT("dve", S5ST[:, l, 1, :], SC["t1"], SC["t2"], ALU.add, ["sc_t1", "sc_t2"], ["S5ST"])
        wb, wr = w_get(l, 11)
        YGR = [f"YG{q}" for q in range(4)]
        for mm in range(4):
            ps, pr = bank()
            MMG(ps[:, :], [(wb[:, kc * 512 + mm * 128:kc * 512 + (mm + 1) * 128], YG[:, kc, :]) for kc in range(4)], [wr] + YGR, [pr])
            ACT(GE[:], ps[:, :], AF.Sigmoid, [pr], ["GE"])
            TT("dve", YA[:, mm, :], YG[:, mm, :], GE[:], ALU.mult, ["GE", f"YG{mm}"], [f"YA{mm}"])
        if c == dump_c and l == 0:
            dump("ya", YA[:], [128, 4, TC], [f"YA{m}" for m in range(4)])
        if c == dump_c and l == 0:
            dump("yb", YB[:], [128, 4, TC], [f"YB{m}" for m in range(4)])
        wbA, wrA = w_get(l, 12)
        wbBm, wrBm = w_get(l, 13)
        YAR = [f"YA{q}" for q in range(4)]
        YBR = [f"YB{q}" for q in range(4)]
        for mm in range(KD):
            psa, pra = bank()
            psb, prb = bank()
            MMG(psa[:, :], [(wbA[:, kc * 1024 + mm * 128:kc * 1024 + (mm + 1) * 128], YA[:, kc, :]) for kc in range(4)], [wrA] + YAR, [pra])
            MMG(psb[:, :], [(wbBm[:, kc * 1024 + mm * 128:kc * 1024 + (mm + 1) * 128], YB[:, kc, :]) for kc in range(4)], [wrBm] + YBR, [prb])
            TT("dve", T[0][:], psa[:, :], SIGA[mm], ALU.mult, [pra, f"SIGA{mm}"], ["T0"])
            TT("dve", T[1][:], psb[:, :], SIGB[mm], ALU.mult, [prb, f"SIGB{mm}"], ["T1"])
            TT("pool", MRG[mm], T[0][:], T[1][:], ALU.add, ["T0", "T1"], [f"MRG{mm}"])
        MR = [f"MRG{q}" for q in range(KD)]
        if c == dump_c and l == 0:
            dump("mrg", BIG[:, 2 * KD:3 * KD, :], [128, KD, TC], MR)
        for bi in range(2):
            wb, wr = w_get(l, 14 + bi)
            for mm in range(4):
                t = bi * 4 + mm
                ps, pr = bank()
                MMG(ps[:, :], [(wb[:, kc * 512 + mm * 128:kc * 512 + (mm + 1) * 128], MRG[kc]) for kc in range(KD)], [wr] + MR, [pr])
                ACT(OT[:, t, :], ps[:, :], AF.Copy, [pr], [f"OT{t}"])
        if c == dump_c and l == 0:
            dump("ot", OT[:], [128, KD, TC], [f"OT{t}" for t in range(KD)])
        post_norm_residual(l, "g_post")
        if c == dump_c and l == 0:
            dump("xmid", X[:], [128, KD, TC], ["X"])

    def ffn(c, l):
        pre_norm(l, "gf_pre")
        alias = [f"SIGA{t}" for t in range(KD)] + [f"SIGB{t}" for t in range(KD)] + [f"MRG{t}" for t in range(KD)]
        for bi in range(6):
            wbg, wrg = w_get(l, 16 + 2 * bi)
            wbu, wru = w_get(l, 17 + 2 * bi)
            ncols = 512 if bi < 5 else 256
            for mm in range(ncols // 128):
                j = bi * 4 + mm
                psg, prg = proj_tile(wbg, wrg, mm, ncols)
                psu, pru = proj_tile(wbu, wru, mm, ncols)
                o = _off["wfc"][0] + j * 3
                ACT(GP[:, 2:2 + TC], psg[:, :], AF.Copy, [prg], ["BB0", "BB1"])
                CP("pool", GP[:, 0:2], FH[:, l, j, :], ["FH"], ["BB0", "BB1"])
                ACT(ACC[:], GP[:, 0:TC], AF.Identity, ["BB0", "BB1", "SM"], ["T3"], scale=SM[:, l, o:o + 1], bias=sm(l, "bfc", j))
                STT("dve", ACC[:], GP[:, 1:1 + TC], SM[:, l, o + 1:o + 2], ACC[:], ALU.mult, ALU.add, ["BB0", "BB1", "SM"], ["T3"])
                STT("dve", ACC[:], GP[:, 2:2 + TC], SM[:, l, o + 2:o + 3], ACC[:], ALU.mult, ALU.add, ["BB0", "BB1", "SM"], ["T3"])
                CP("pool", FH[:, l, j, :], GP[:, TC:TC + 2], ["BB0", "BB1"], ["FH"])
                ACT(GE[:], ACC[:], AF.Gelu_apprx_tanh, ["T3"], ["GE"])
                TT("dve", ACTT[j], psu[:, :], GE[:], ALU.mult, [pru, "GE"], [f"ACTT{j}"] + alias)
        AR = [f"ACTT{j}" for j in range(NFT)]
        for mm in range(KD):
            wb, wr = w_get(l, 28 + mm)
            ps, pr = bank()
            MMG(ps[:, :], [(wb[:, j * 128:(j + 1) * 128], ACTT[j]) for j in range(NFT)], [wr] + AR, [pr])
            ACT(OT[:, mm, :], ps[:, :], AF.Copy, [pr], [f"OT{mm}"])
        post_norm_residual(l, "gf_post")
        P.op("act", lambda e: e.activation(out=GE[:, 0:1], in_=GE[:, 0:1], func=AF.Copy), [], AR + alias + ["GE"])

    for c in range(nch_run):
        P.dma("sp", X[:], xT[c], W=["X"])
        for l in range(DEPTH):
            mixer(c, l)
            ffn(c, l)
        P.dma("sp", yT[c], X[:], R=["X"], W=[f"yT{c}"])
    evs = [P.R(f"yT{c}").w for c in range(nch_run)] + list(dump_out.values())
    P.final_wait("sp", evs)
    P.emit()
    st.close()
    return nc


def _kblock(W, n):
    K = W.shape[0]
    kc = K // 128
    a = W.reshape(kc, 128, n).transpose(1, 0, 2).reshape(128, kc * n)
    out = np.zeros((128, 4096), np.float32)
    out[:, :kc * n] = a
    return out


def pack_weights(inp):
    wst = np.zeros((DEPTH, NBLK, 128, 4096), np.float32)
    small = np.zeros((128, DEPTH, NSMALL), np.float32)
    craw = np.zeros((DEPTH, 128, 16, 256), np.float32)

    def put(l, name, arr):
        o, w = _off[name]
        small[:, l, o:o + w] = arr

    for l in range(DEPTH):
        w_in = np.asarray(inp["w_in"][l], np.float32)
        for b, c0 in enumerate((0, 512, 1024, 1536, 2048)):
            wst[l, b] = _kblock(w_in[:, c0:c0 + 512], 512)
        for b, c0 in ((5, 2568), (6, 3080), (7, 3592), (8, 4104)):
            wst[l, b] = _kblock(w_in[:, c0:c0 + 512], 512)
        bre = np.asarray(inp["ssm_b_re"][l], np.float32)
        bim = np.asarray(inp["ssm_b_im"][l], np.float32)
        cre = np.asarray(inp["ssm_c_re"][l], np.float32)
        cim = np.asarray(inp["ssm_c_im"][l], np.float32)
        blkB = np.zeros((128, 16, 256), np.float32)
        for k in range(16):
            for gi in range(2):
                g = 2 * k + gi
                gg = g % 8
                blkB[gg * 16:(gg + 1) * 16, k, gi * 64:(gi + 1) * 64] = bre[g].T
                blkB[gg * 16:(gg + 1) * 16, k, 128 + gi * 64:128 + (gi + 1) * 64] = bim[g].T
                craw[l, gi * 64:(gi + 1) * 64, k, gg * 16:(gg + 1) * 16] = cre[g].T
                craw[l, gi * 64:(gi + 1) * 64, k, 128 + gg * 16:128 + (gg + 1) * 16] = cim[g].T
        wst[l, 9] = blkB.reshape(128, 4096)
        wst[l, 11] = _kblock(np.asarray(inp["w_ssm_glu"][l], np.float32), 512)
        wst[l, 12] = _kblock(np.asarray(inp["w_branch_ssm"][l], np.float32), 1024)
        wst[l, 13] = _kblock(np.asarray(inp["w_branch_mlstm"][l], np.float32), 1024)
        w_out = np.asarray(inp["w_out"][l], np.float32)
        wst[l, 14] = _kblock(w_out[:, 0:512], 512)
        wst[l, 15] = _kblock(w_out[:, 512:1024], 512)
        wg = np.asarray(inp["w_ffn_gate"][l], np.float32)
        wu = np.asarray(inp["w_ffn_up"][l], np.float32)
        for bi in range(6):
            n = 512 if bi < 5 else 256
            wst[l, 16 + 2 * bi] = _kblock(wg[:, bi * 512:bi * 512 + n], n)
            wst[l, 17 + 2 * bi] = _kblock(wu[:, bi * 512:bi * 512 + n], n)
        wd = np.asarray(inp["w_ffn_down"][l], np.float32)
        for m in range(8):
            wst[l, 28 + m] = _kblock(wd[:, m * 128:(m + 1) * 128], 128)
        put(l, "g_pre", np.asarray(inp["g_mix_pre"][l]).reshape(8, 128).T)
        put(l, "g_post", np.asarray(inp["g_mix_post"][l]).reshape(8, 128).T)
        put(l, "gf_pre", np.asarray(inp["g_ffn_pre"][l]).reshape(8, 128).T)
        put(l, "gf_post", np.asarray(inp["g_ffn_post"][l]).reshape(8, 128).T)
        put(l, "wgate", w_in[:, 2560:2568].reshape(8, 128, 8).transpose(1, 0, 2).reshape(128, 64))
        put(l, "bgate", np.broadcast_to(np.asarray(inp["b_gates"][l], np.float32)[None, :], (128, 8)))
        wqk = np.asarray(inp["w_qk_conv"][l], np.float32)
        put(l, "wqk", wqk.reshape(4, 8, 128).transpose(2, 1, 0).reshape(128, 32))
        put(l, "bqk", np.asarray(inp["b_qk_conv"][l]).reshape(8, 128).T)
        put(l, "ghead", np.asarray(inp["g_head_norm"][l]).reshape(4, 128).T)
        wfc = np.asarray(inp["w_ffn_conv"][l], np.float32)
        put(l, "wfc", wfc.reshape(3, 22, 128).transpose(2, 1, 0).reshape(128, 66))
        put(l, "bfc", np.asarray(inp["b_ffn_conv"][l]).reshape(22, 128).T)
        lre = np.asarray(inp["ssm_lambda_re"][l], np.float32)
        lim = np.asarray(inp["ssm_lambda_im"][l], np.float32)
        ldt = np.asarray(inp["ssm_log_dt"][l], np.float32)
        put(l, "lre", lre.reshape(16, 128).T)
        put(l, "lim", lim.reshape(16, 128).T)
        put(l, "ldt", np.repeat(ldt, 64).reshape(16, 128).T)
        put(l, "dvec", np.asarray(inp["ssm_d"][l], np.float32).reshape(4, 128).T)
    return wst, small, craw


def make_consts():
    cst = np.zeros((128, 128 * 3 + TC), np.float32)
    cst[:, 0:128] = np.eye(128, dtype=np.float32)
    cst[:, 128:256] = np.triu(np.ones((128, 128), np.float32))
    cst[:, 256:384] = 1.0
    cst[:, 384:] = np.arange(TC, dtype=np.float32)[None, :]
    return cst


def pack_tokens(x, meta, nch):
    seq = np.zeros((NCH * TC, D), np.float32)
    seq[PADF:PADF + NMETA] = meta
    seq[PADF + NMETA:] = x
    seq = seq[:nch * TC]
    xT = np.ascontiguousarray(seq.reshape(nch, TC, KD, 128).transpose(0, 3, 2, 1))
    tok = np.arange(nch * TC).reshape(nch * NSUB, 128).T
    padb = np.where(tok < PADF, np.float32(-30000.0), np.float32(0.0)).astype(np.float32)
    return xT, np.ascontiguousarray(padb)


_CACHE = {}


def run(inputs, nch, dumps=()):
    key = (nch, tuple(dumps))
    if key not in _CACHE:
        _CACHE[key] = build_program(nch, dumps)
    nc = _CACHE[key]
    wst, small, craw = pack_weights(inputs)
    xT, padb = pack_tokens(np.asarray(inputs["x"], np.float32)[0], np.asarray(inputs["meta_tokens"], np.float32), nch)
    in_map = {"xT": xT, "wst": wst, "small": small, "craw": craw, "consts": make_consts(), "padb": padb}
    res = run_bass_kernel_spmd(nc, [in_map], core_ids=[0])
    return res.results[0]


def kernel(**inputs):
    r = run(inputs, NCH)
    yT = r["yT"]
    seq = yT.transpose(0, 3, 2, 1).reshape(NCH * TC, D)
    out = seq[PADF + NMETA:].reshape(1, SEQ, D)
    return np.ascontiguousarray(out.astype(np.float32))
```

```python
import math
from contextlib import ExitStack
import numpy as np
import concourse.bass as bass
import concourse.mybir as mybir
from concourse.bass_utils import run_bass_kernel_spmd

F32 = mybir.dt.float32
BF16 = mybir.dt.bfloat16
I32 = mybir.dt.int32
AF = mybir.ActivationFunctionType
ALU = mybir.AluOpType

D = 1024
KD = 8
TC = 512
NSUB = 4
DEPTH = 4
NMETA = 16
SEQ = 16384
NCH = 33
PADF = NCH * TC - SEQ - NMETA
FFN = 2816
NFT = 22
NBLK = 36
NBUF = 4
EPS = 1e-6
LN_SCALE = math.log(128.0 ** -0.5)
TWO_PI = 2.0 * math.pi
CW1 = 6.28125
CW2 = TWO_PI - CW1
SEM_LIMIT = 30000
NO_SELF_WAIT = ("pe",)

_off = {}
_o = 0
for _n, _w in [("g_pre", 8), ("g_post", 8), ("gf_pre", 8), ("gf_post", 8), ("wgate", 64), ("bgate", 8),
               ("wqk", 32), ("bqk", 8), ("ghead", 4), ("wfc", 66), ("bfc", 22), ("lre", 16), ("lim", 16),
               ("ldt", 16), ("dvec", 4)]:
    _off[_n] = (_o, _w)
    _o += _w
NSMALL = _o


class Reg:
    __slots__ = ("w", "r", "name")

    def __init__(self, name=""):
        self.w = None
        self.r = []
        self.name = name


class Prog:
    ENGS = ("pe", "act", "dve", "pool", "sp")

    def __init__(self, nc, stack):
        self.nc = nc
        self.stack = stack
        self.q = {e: [] for e in self.ENGS}
        self.cnt = {e: 0 for e in self.ENGS}
        self.sems = {e: [stack.enter_context(nc.semaphore(f"s_{e}_0"))] for e in self.ENGS}
        self.waited = {e: {} for e in self.ENGS}
        self.dma_sems = [stack.enter_context(nc.semaphore(f"s_dma_{i}")) for i in range(8 + NBUF)]
        self.dma_cnt = [0] * (8 + NBUF)
        self.dma_rr = 0
        self.regs = {}
        self.nops = 0
        self.no_self_wait = set(NO_SELF_WAIT)
        self.own_sem_ids = {e: {id(self.sems[e][0])} for e in self.ENGS}

    def R(self, x):
        if isinstance(x, Reg):
            return x
        if x not in self.regs:
            self.regs[x] = Reg(x)
        return self.regs[x]

    def _collect(self, eng, R, W, extra=()):
        evs = list(extra)
        for r in R:
            if r.w is not None:
                evs.append(r.w)
        for w in W:
            if w.w is not None:
                evs.append(w.w)
            evs.extend(w.r)
        need = {}
        own = self.own_sem_ids[eng] if eng in self.no_self_wait else ()
        for (s, v) in evs:
            k = id(s)
            if k in own:
                continue
            if self.waited[eng].get(k, 0) >= v:
                continue
            if k not in need or need[k][1] < v:
                need[k] = (s, v)
        for k, (s, v) in need.items():
            self.waited[eng][k] = v
        return list(need.values())

    def _mark(self, ev, R, W):
        for r in R:
            r.r.append(ev)
        for w in W:
            w.w = ev
            w.r = []

    def op(self, eng, fn, R=(), W=()):
        R = [self.R(x) for x in R]
        W = [self.R(x) for x in W]
        waits = self._collect(eng, R, W)
        if self.cnt[eng] >= SEM_LIMIT:
            self.sems[eng].append(self.stack.enter_context(self.nc.semaphore(f"s_{eng}_{len(self.sems[eng])}")))
            self.own_sem_ids[eng].add(id(self.sems[eng][-1]))
            self.cnt[eng] = 0
        self.cnt[eng] += 1
        sem = self.sems[eng][-1]
        ev = (sem, self.cnt[eng])
        self.q[eng].append((waits, fn, (sem, 1)))
        self._mark(ev, R, W)
        self.nops += 1
        return ev

    def dma(self, eng, out, in_, R=(), W=(), sem_idx=None):
        R = [self.R(x) for x in R]
        W = [self.R(x) for x in W]
        if sem_idx is None:
            sem_idx = self.dma_rr
            self.dma_rr = (self.dma_rr + 1) % 8
        s = self.dma_sems[sem_idx]
        extra = [(s, self.dma_cnt[sem_idx])] if self.dma_cnt[sem_idx] else []
        waits = self._collect(eng, R, W, extra)
        self.dma_cnt[sem_idx] += 16
        ev = (s, self.dma_cnt[sem_idx])
        self.q[eng].append((waits, (lambda e, o=out, i=in_: e.dma_start(out=o, in_=i)), (s, 16)))
        self._mark(ev, R, W)
        return ev

    def final_wait(self, eng, evs):
        self.q[eng].append((list(evs), None, None))

    def emit(self):
        nc = self.nc
        with nc.Block() as block:
            def mk(name):
                def body(e):
                    for waits, fn, inc in self.q[name]:
                        for (s, v) in waits:
                            e.wait_ge(s, v)
                        if fn is not None:
                            ins = fn(e)
                            ins.then_inc(inc[0], inc[1])
                return body
            block.tensor(mk("pe"))
            block.scalar(mk("act"))
            block.vector(mk("dve"))
            block.gpsimd(mk("pool"))
            block.sync(mk("sp"))


def build_program(nch_run, dumps=()):
    nc = bass.Bass("TRN2", target_bir_lowering=False)
    xT = nc.dram_tensor("xT", [nch_run, 128, KD, TC], F32, kind="ExternalInput").ap()
    yT = nc.dram_tensor("yT", [nch_run, 128, KD, TC], F32, kind="ExternalOutput").ap()
    wst = nc.dram_tensor("wst", [DEPTH, NBLK, 128, 4096], F32, kind="ExternalInput").ap()
    small = nc.dram_tensor("small", [128, DEPTH, NSMALL], F32, kind="ExternalInput").ap()
    craw = nc.dram_tensor("craw", [DEPTH, 128, 16, 256], F32, kind="ExternalInput").ap()
    consts = nc.dram_tensor("consts", [128, 128 * 3 + TC], F32, kind="ExternalInput").ap()
    padb = nc.dram_tensor("padb", [128, nch_run * NSUB], F32, kind="ExternalInput").ap()
    tabs = nc.dram_tensor("tabs", [DEPTH, 16, 128, 2 * TC], F32).ap()
    cz = nc.dram_tensor("cz", [DEPTH, 128, 4096], F32).ap()
    dump_out = {}
    dump_c = 1 if nch_run > 1 else 0

    st = ExitStack()
    P = Prog(nc, st)

    def sb(name, shape, dt=F32):
        return st.enter_context(nc.sbuf_tensor(name, shape, dt))

    X = sb("X", [128, KD, TC])
    HT = sb("HT", [128, KD, TC], BF16)
    SQ = sb("SQ", [128, 2, TC], BF16)
    RSTD = sb("RSTD", [128, TC])
    OT = sb("OT", [128, KD, TC])
    WB = [sb(f"WB{i}", [128, 4096], BF16) for i in range(NBUF)]
    SM = sb("SM", [128, DEPTH, NSMALL])
    CONS = sb("CONS", [128, 128 * 3 + TC])
    PADB = sb("PADB", [128, nch_run * NSUB])
    IDB = sb("IDB", [128, 128], BF16)
    ONESB = sb("ONESB", [128, 128], BF16)
    WGB = sb("WGB", [128, DEPTH, 64], BF16)
    ONE1 = sb("ONE1", [128, 1])
    LNS = sb("LNS", [128, 1])
    EPS1 = sb("EPS1", [128, 1])
    UT = sb("UT", [128, 4, TC], BF16)
    QKPH = sb("QKPH", [128, DEPTH, 8, 3])
    QKP1 = sb("QKP1", [128, 2, 3 + TC])
    QKA = sb("QKA", [128, 2, TC])
    QKT = sb("QKT", [128, 8, TC], BF16)
    V = sb("V", [128, NSUB, 4, 129], BF16)
    SIGO = sb("SIGO", [128, 4, TC], BF16)
    BIG = sb("BIG", [128, 3 * KD, TC], BF16)
    G = sb("G", [128, NSUB, 8])
    YA = sb("YA", [128, 4, TC], BF16)
    YG = sb("YG", [128, 4, TC], BF16)
    YB = sb("YB", [128, 4, TC], BF16)
    S5DEC = sb("S5DEC", [128, DEPTH, 16])
    S5ROT = sb("S5ROT", [128, DEPTH, 2, 16])
    S5ST = sb("S5ST", [128, DEPTH, 2, 16])
    S5E = sb("S5E", [128, 2, 16])
    TAB = [sb(f"TAB{i}", [128, 2 * TC]) for i in range(2)]
    BB = sb("BB", [128, 2, TC])
    SS = sb("SS", [128, 2, TC])
    T = [sb(f"T{i}", [128, TC]) for i in range(4)]
    XR = [sb(f"XR{i}", [128, 2, TC], BF16) for i in range(2)]
    YS = sb("YS", [128, TC])
    CT = sb("CT", [128, DEPTH, 4, 129])
    CTB = sb("CTB", [128, 4, 128], BF16)
    NB = sb("NB", [128, 4, 128], BF16)
    SP_ = sb("SP_", [128, 4])
    LI = sb("LI", [128, 4])
    SPB = sb("SPB", [128, 4, 128])
    BD = sb("BD", [128, 4])
    TW = sb("TW", [128, 4])
    WEX = sb("WEX", [128, 4])
    DEC = sb("DEC", [128, 4])
    DT_ = sb("DT_", [128, TC])
    DTM = sb("DTM", [128, TC])
    EF = sb("EF", [128, TC])
    QP = sb("QP", [128, TC], BF16)
    WT = sb("WT", [128, TC], BF16)
    KW = sb("KW", [128, TC], BF16)
    SQH = sb("SQH", [128, TC], BF16)
    FH = sb("FH", [128, DEPTH, NFT, 2])
    GE = sb("GE", [128, TC])
    PRK = GE[:].bitcast(I32)
    SCN = ("dt", "th", "ar", "c", "s", "abr", "abi", "nr", "den", "t1", "t2", "zre", "zim", "a16", "c2", "s2", "nzim")
    SCT = sb("SCT", [128, len(SCN), 16])
    SC = {n: SCT[:, i, :] for i, n in enumerate(SCN)}
    SIGA = [BIG[:, t, :] for t in range(KD)]
    SIGB = [BIG[:, KD + t, :] for t in range(KD)]
    MRG = [BIG[:, 2 * KD + t, :] for t in range(KD)]
    ACTT = [BIG[:, j, :] for j in range(NFT)]
    HTF = HT[:].rearrange("p a t -> p (a t)").bitcast(F32)
    AD, HH, RS = HTF[:, 0:TC], HTF[:, TC:2 * TC], HTF[:, 2 * TC:3 * TC]
    ADR, HHR, RSR = ["HT0", "HT1"], ["HT2", "HT3"], ["HT4", "HT5"]
    ACC = T[3]
    GP = BB[:].rearrange("p a t -> p (a t)")

    PS = [st.enter_context(nc.psum_tensor(f"PS{i}", [128, TC], F32)) for i in range(7)]
    PST = st.enter_context(nc.psum_tensor("PST", [128, TC], BF16))
    PSR = [Reg(f"ps{i}") for i in range(7)]
    bank_rr = [0]

    def bank():
        i = bank_rr[0]
        bank_rr[0] = (i + 1) % 6
        return PS[i], PSR[i]

    def ACT(out, in_, func, R, W, **kw):
        P.op("act", lambda e: e.activation(out=out, in_=in_, func=func, **kw), R, W)

    def TT(eng, out, in0, in1, op, R, W):
        P.op(eng, lambda e: e.tensor_tensor(out=out, in0=in0, in1=in1, op=op), R, W)

    def TS(eng, out, in0, s1, s2, op0, op1, R, W):
        if s2 is None:
            P.op(eng, lambda e: e.tensor_scalar(out=out, in0=in0, scalar1=s1, scalar2=None, op0=op0), R, W)
        else:
            P.op(eng, lambda e: e.tensor_scalar(out=out, in0=in0, scalar1=s1, scalar2=s2, op0=op0, op1=op1), R, W)

    def STT(eng, out, in0, scalar, in1, op0, op1, R, W):
        P.op(eng, lambda e: e.scalar_tensor_tensor(out=out, in0=in0, scalar=scalar, in1=in1, op0=op0, op1=op1), R, W)

    def CP(eng, out, in_, R, W):
        P.op(eng, lambda e: e.tensor_copy(out=out, in_=in_), R, W)

    def RECIP(out, in_, R, W):
        P.op("dve", lambda e: e.reciprocal(out=out, in_=in_), R, W)

    def MM(out, lhsT, rhs, R, W, start=True, stop=True):
        P.op("pe", lambda e: e.matmul(out, lhsT=lhsT, rhs=rhs, start=start, stop=stop), R, W)

    def MMG(out, pairs, R, W):
        pairs = list(pairs)

        def fn(e):
            ins = None
            n = len(pairs)
            for i, (a, b) in enumerate(pairs):
                ins = e.matmul(out, lhsT=a, rhs=b, start=(i == 0), stop=(i == n - 1))
            return ins
        P.op("pe", fn, R, W)

    def dump(name, ap, shape, R):
        if name in dumps:
            t = nc.dram_tensor("dbg_" + name, list(shape), F32 if ap.dtype == F32 else BF16, kind="ExternalOutput").ap()
            dump_out[name] = P.dma("sp", t, ap, R=R, W=["dbg_" + name])

    P.dma("sp", SM[:], small[:, :, :], W=["SM"])
    P.dma("sp", CONS[:], consts[:, :], W=["CONS"])
    P.dma("sp", PADB[:], padb[:, :], W=["PADB"])
    IDF = CONS[:, 0:128]
    TRIU = CONS[:, 128:256]
    ONESF = CONS[:, 256:384]
    IOTA = CONS[:, 384:384 + TC]
    CP("dve", IDB[:], IDF, ["CONS"], ["IDB"])
    CP("dve", ONESB[:], ONESF, ["CONS"], ["ONESB"])
    for l in range(DEPTH):
        o = _off["wgate"][0]
        CP("dve", WGB[:, l, :], SM[:, l, o:o + 64], ["SM"], ["WGB"])
    P.op("pool", lambda e: e.memset(V[:], 1.0), W=["V0", "V1", "V2", "V3"])
    P.op("pool", lambda e: e.memset(CT[:], 0.0), W=["CT"])
    P.op("pool", lambda e: e.memset(S5ST[:], 0.0), W=["S5ST"])
    P.op("pool", lambda e: e.memset(QKPH[:], 0.0), W=["QKPH"])
    P.op("pool", lambda e: e.memset(FH[:], 0.0), W=["FH"])
    P.op("pool", lambda e: e.memset(ONE1[:], 1.0), W=["ONE1"])
    P.op("pool", lambda e: e.memset(LNS[:], LN_SCALE), W=["LNS"])
    P.op("pool", lambda e: e.memset(EPS1[:], EPS), W=["EPS1"])

    def sm(l, name, j=None):
        o, w = _off[name]
        if j is None:
            return SM[:, l, o:o + w]
        return SM[:, l, o + j:o + j + 1]

    PRC = OT[:].rearrange("p a t -> p (a t)")
    PRZ = X[:].rearrange("p a t -> p (a t)")
    PRA, PRF, PRT0, PRT1 = T[0], T[1], T[2], T[3]

    OTR = [f"OT{t}" for t in range(KD)]

    def prologue_layer(l):
        s = SC
        lre, lim, ldt = sm(l, "lre"), sm(l, "lim"), sm(l, "ldt")
        dec = S5DEC[:, l, :]
        def exp_taylor(dst, dreg, src, sreg):
            TS("dve", dst, src, 1.0 / 5040, 1.0 / 720, ALU.mult, ALU.add, [sreg], [dreg])
            for cf in (1.0 / 120, 1.0 / 24, 1.0 / 6, 0.5, 1.0, 1.0):
                TT("dve", dst, dst, src, ALU.mult, [sreg], [dreg])
                TS("dve", dst, dst, cf, None, ALU.add, None, [], [dreg])

        def sin_reduced(dst, dreg, ang, areg, shift):
            TS("dve", s["t1"], ang, shift, 1.0 / TWO_PI, ALU.add, ALU.mult, [areg], ["sc_t1"])
            CP("dve", PRK[:, 0:16], s["t1"], ["sc_t1"], ["PRK"])
            CP("dve", s["t1"], PRK[:, 0:16], ["PRK"], ["sc_t1"])
            STT("dve", s["t2"], s["t1"], -CW1, ang, ALU.mult, ALU.add, ["sc_t1", areg], ["sc_t2"])
            STT("dve", s["t2"], s["t1"], -CW2, s["t2"], ALU.mult, ALU.add, ["sc_t1"], ["sc_t2"])
            TS("dve", s["t2"], s["t2"], shift, -math.pi, ALU.add, ALU.max, [], ["sc_t2"])
            TS("dve", s["t2"], s["t2"], math.pi, None, ALU.min, None, [], ["sc_t2"])
            ACT(dst, s["t2"], AF.Sin, ["sc_t2"], [dreg])

        TS("dve", s["a16"], ldt, 1.0 / 16, None, ALU.mult, None, ["SM"], ["sc_a16"])
        exp_taylor(s["dt"], "sc_dt", s["a16"], "sc_a16")
        for _ in range(4):
            TT("dve", s["dt"], s["dt"], s["dt"], ALU.mult, [], ["sc_dt"])
        TT("dve", s["ar"], lre, s["dt"], ALU.mult, ["SM", "sc_dt"], ["sc_ar"])
        TT("dve", s["th"], lim, s["dt"], ALU.mult, ["SM", "sc_dt"], ["sc_th"])
        exp_taylor(dec, "S5DEC", s["ar"], "sc_ar")
        sin_reduced(s["s"], "sc_s", s["th"], "sc_th", 0.0)
        sin_reduced(s["c"], "sc_c", s["th"], "sc_th", math.pi / 2)
        TT("dve", s["abr"], dec, s["c"], ALU.mult, ["S5DEC", "sc_c"], ["sc_abr"])
        TT("dve", s["abi"], dec, s["s"], ALU.mult, ["S5DEC", "sc_s"], ["sc_abi"])
        TS("dve", s["nr"], s["abr"], -1.0, None, ALU.add, None, ["sc_abr"], ["sc_nr"])
        TT("dve", s["t1"], lre, lre, ALU.mult, ["SM"], ["sc_t1"])
        TT("dve", s["t2"], lim, lim, ALU.mult, ["SM"], ["sc_t2"])
        TT("dve", s["den"], s["t1"], s["t2"], ALU.add, ["sc_t1", "sc_t2"], ["sc_den"])
        RECIP(s["den"], s["den"], ["sc_den"], ["sc_den"])
        TT("dve", s["t1"], s["nr"], lre, ALU.mult, ["sc_nr", "SM"], ["sc_t1"])
        TT("dve", s["t2"], s["abi"], lim, ALU.mult, ["sc_abi", "SM"], ["sc_t2"])
        TT("dve", s["t1"], s["t1"], s["t2"], ALU.add, ["sc_t1", "sc_t2"], ["sc_t1"])
        TT("dve", s["zre"], s["t1"], s["den"], ALU.mult, ["sc_t1", "sc_den"], ["sc_zre"])
        TT("dve", s["t1"], s["abi"], lre, ALU.mult, ["sc_abi", "SM"], ["sc_t1"])
        TT("dve", s["t2"], s["nr"], lim, ALU.mult, ["sc_nr", "SM"], ["sc_t2"])
        TT("dve", s["t1"], s["t1"], s["t2"], ALU.subtract, ["sc_t1", "sc_t2"], ["sc_t1"])
        TT("dve", s["zim"], s["t1"], s["den"], ALU.mult, ["sc_t1", "sc_den"], ["sc_zim"])
        TS("dve", s["nzim"], s["zim"], -1.0, None, ALU.mult, None, ["sc_zim"], ["sc_nzim"])
        P.dma("sp", PRC, craw[l].rearrange("p a t -> p (a t)"), W=OTR)
        for k in range(16):
            cre, cim = PRC[:, k * 256:k * 256 + 128], PRC[:, k * 256 + 128:k * 256 + 256]
            o1, o2 = PRZ[:, k * 256:k * 256 + 128], PRZ[:, k * 256 + 128:k * 256 + 256]
            TS("dve", o1, cim, s["nzim"][:, k:k + 1], None, ALU.mult, None, OTR + ["sc_nzim"], ["X"])
            STT("dve", o1, cre, s["zre"][:, k:k + 1], o1, ALU.mult, ALU.add, OTR + ["sc_zre"], ["X"])
            TS("dve", o2, cim, s["zre"][:, k:k + 1], -1.0, ALU.mult, ALU.mult, OTR + ["sc_zre"], ["X"])
            STT("dve", o2, cre, s["nzim"][:, k:k + 1], o2, ALU.mult, ALU.add, OTR + ["sc_nzim"], ["X"])
        P.dma("sp", cz[l], PRZ, R=["X"], W=[f"cz{l}"])
        for k in range(16):
            th = s["th"][:, k:k + 1]
            TS("dve", PRA[:], IOTA, th, None, ALU.mult, None, ["CONS", "sc_th"], ["T0"])
            tb = TAB[k % 2]
            for half, shift in ((1, 0.0), (0, math.pi / 2)):
                TS("dve", PRF[:], PRA[:], shift, 1.0 / TWO_PI, ALU.add, ALU.mult, ["T0"], ["T1"])
                CP("dve", PRK[:], PRF[:], ["T1"], ["PRK"])
                CP("dve", PRF[:], PRK[:], ["PRK"], ["T1"])
                STT("dve", PRT0[:], PRF[:], -CW1, PRA[:], ALU.mult, ALU.add, ["T1", "T0"], ["T2"])
                STT("dve", PRT0[:], PRF[:], -CW2, PRT0[:], ALU.mult, ALU.add, ["T1", "T2"], ["T2"])
                TS("dve", PRT0[:], PRT0[:], shift, -math.pi, ALU.add, ALU.max, ["T2"], ["T2"])
                TS("dve", PRT0[:], PRT0[:], math.pi, None, ALU.min, None, ["T2"], ["T2"])
                ACT(tb[:, half * TC:(half + 1) * TC], PRT0[:], AF.Sin, ["T2"], [f"TAB{k % 2}"])
            c1, s1 = s["c"][:, k:k + 1], s["s"][:, k:k + 1]
            c511, s511 = tb[:, TC - 1:TC], tb[:, 2 * TC - 1:2 * TC]
            tr = f"TAB{k % 2}"
            TT("dve", s["c2"][:, k:k + 1], c511, c1, ALU.mult, [tr, "sc_c"], ["sc_c2"])
            TT("dve", s["s2"][:, k:k + 1], s511, s1, ALU.mult, [tr, "sc_s"], ["sc_s2"])
            TT("dve", S5ROT[:, l, 0, k:k + 1], s["c2"][:, k:k + 1], s["s2"][:, k:k + 1], ALU.subtract, ["sc_c2", "sc_s2"], ["S5ROT"])
            TT("dve", s["c2"][:, k:k + 1], s511, c1, ALU.mult, [tr, "sc_c"], ["sc_c2"])
            TT("dve", s["s2"][:, k:k + 1], c511, s1, ALU.mult, [tr, "sc_s"], ["sc_s2"])
            TT("dve", S5ROT[:, l, 1, k:k + 1], s["c2"][:, k:k + 1], s["s2"][:, k:k + 1], ALU.add, ["sc_c2", "sc_s2"], ["S5ROT"])
            P.dma("sp", tabs[l, k], tb[:], R=[tr], W=[f"tab{l}_{k}", tr])

    for l in range(DEPTH):
        prologue_layer(l)

    wq = [(l, b) for c in range(nch_run) for l in range(DEPTH) for b in range(NBLK) if b != -1]
    wstate = {"next": 0, "use": 0}
    WBR = [Reg(f"wb{i}") for i in range(NBUF)]

    def w_issue():
        i = wstate["next"]
        l, b = wq[i]
        buf = i % NBUF
        if b == 10:
            P.dma("pool", WB[buf][:], cz[l], R=[f"cz{l}"], W=[WBR[buf]], sem_idx=8 + buf)
        else:
            P.dma("pool", WB[buf][:], wst[l, b], W=[WBR[buf]], sem_idx=8 + buf)
        wstate["next"] += 1

    def w_get(l, b):
        i = wstate["use"]
        assert wq[i] == (l, b), (wq[i], l, b)
        wstate["use"] += 1
        while wstate["next"] < min(len(wq), i + NBUF - 1):
            w_issue()
        return WB[i % NBUF], WBR[i % NBUF]

    HTR = [f"HT{kt}" for kt in range(KD)]

    def rmsnorm_stats(src_tile, src_reg):
        ps, pr = bank()
        for kt in range(KD):
            ACT(SQ[:, kt % 2, :], src_tile(kt), AF.Square, [src_reg(kt)], [f"SQ{kt % 2}"])
            MM(ps[:, :], ONESB[:], SQ[:, kt % 2, :], ["ONESB", f"SQ{kt % 2}"], [pr], start=(kt == 0), stop=(kt == KD - 1))
        ACT(RSTD[:], ps[:, :], AF.Ln, [pr, "EPS1"], ["RSTD"], scale=1.0 / D, bias=EPS1[:, 0:1])
        ACT(RSTD[:], RSTD[:], AF.Exp, [], ["RSTD"], scale=-0.5)

    def pre_norm(l, gname):
        rmsnorm_stats(lambda kt: X[:, kt, :], lambda kt: "X")
        for kt in range(KD):
            STT("dve", HT[:, kt, :], X[:, kt, :], sm(l, gname, kt), RSTD[:], ALU.mult, ALU.mult, ["X", "RSTD", "SM"], [f"HT{kt}"])

    def post_norm_residual(l, gname):
        rmsnorm_stats(lambda kt: OT[:, kt, :], lambda kt: f"OT{kt}")
        for kt in range(KD):
            STT("dve", OT[:, kt, :], OT[:, kt, :], sm(l, gname, kt), RSTD[:], ALU.mult, ALU.mult, ["RSTD", "SM"], [f"OT{kt}"])
            TT("pool", X[:, kt, :], X[:, kt, :], OT[:, kt, :], ALU.add, [f"OT{kt}"], ["X"])

    def proj_tile(wb, wr, mm, ncols=512):
        ps, pr = bank()
        MMG(ps[:, :], [(wb[:, kc * ncols + mm * 128: kc * ncols + (mm + 1) * 128], HT[:, kc, :]) for kc in range(KD)], [wr] + HTR, [pr])
        return ps, pr

    def refresh_state(l):
        ACT(CTB[:], CT[:, l, :, 0:128], AF.Copy, ["CT"], ["CTB"])
        for h in range(4):
            TS("pool", NB[:, h, :], ONESB[:], CT[:, l, h, 128:129], None, ALU.mult, None, ["ONESB", "CT"], ["NB"])

    def mixer(c, l):
        pre_norm(l, "g_pre")
        if c == dump_c and l == 0:
            dump("ht", HT[:], [128, KD, TC], HTR)
        wb, wr = w_get(l, 0)
        for mm in range(4):
            ps, pr = proj_tile(wb, wr, mm)
            ACT(UT[:, mm, :], ps[:, :], AF.Copy, [pr], [f"UT{mm}"])
        for blk, base in ((1, 0), (2, 4)):
            wb, wr = w_get(l, blk)
            for mm in range(4):
                t = base + mm
                pb = t % 2
                ps, pr = proj_tile(wb, wr, mm)
                rq = f"QKP1_{pb}"
                ACT(QKP1[:, pb, 3:3 + TC], ps[:, :], AF.Copy, [pr], [rq])
                CP("pool", QKP1[:, pb, 0:3], QKPH[:, l, t, :], ["QKPH"], [rq])
                o = _off["wqk"][0] + t * 4
                ACT(QKA[:, pb, :], QKP1[:, pb, 0:TC], AF.Identity, [rq, "SM"], [f"QKA{pb}"], scale=SM[:, l, o:o + 1], bias=sm(l, "bqk", t))
                for j in (1, 2, 3):
                    STT("dve", QKA[:, pb, :], QKP1[:, pb, j:j + TC], SM[:, l, o + j:o + j + 1], QKA[:, pb, :], ALU.mult, ALU.add, [rq, "SM"], [f"QKA{pb}"])
                ACT(QKT[:, t, :], QKA[:, pb, :], AF.Silu, [f"QKA{pb}"], [f"QKT{t}"])
                CP("pool", QKPH[:, l, t, :], QKP1[:, pb, TC:TC + 3], [rq], ["QKPH"])
        wb, wr = w_get(l, 3)
        for sc in range(NSUB):
            ps, pr = bank()
            MMG(ps[:, :], [(HT[:, kc, sc * 128:(sc + 1) * 128], wb[:, kc * 512:(kc + 1) * 512]) for kc in range(KD)], [wr] + HTR, [pr])
            for h in range(4):
                ACT(V[:, sc, h, 0:128], ps[:, h * 128:(h + 1) * 128], AF.Copy, [pr], [f"V{sc}"])
        for sc in range(NSUB):
            ps, pr = bank()
            MMG(ps[:, 0:8], [(HT[:, kc, sc * 128:(sc + 1) * 128], WGB[:, l, kc * 8:kc * 8 + 8]) for kc in range(KD)], ["WGB"] + HTR, [pr])
            TT("dve", G[:, sc, :], ps[:, 0:8], sm(l, "bgate"), ALU.add, [pr, "SM"], [f"G{sc}"])
        wb, wr = w_get(l, 4)
        for mm in range(4):
            ps, pr = proj_tile(wb, wr, mm)
            ACT(SIGO[:, mm, :], ps[:, :], AF.Sigmoid, [pr], [f"SIGO{mm}"])
        for dst, nm, blks in ((SIGA, "SIGA", (5, 6)), (SIGB, "SIGB", (7, 8))):
            for bi, blk in enumerate(blks):
                wb, wr = w_get(l, blk)
                for mm in range(4):
                    t = bi * 4 + mm
                    ps, pr = proj_tile(wb, wr, mm)
                    ACT(dst[t], ps[:, :], AF.Sigmoid, [pr], [f"{nm}{t}"])
        if c == dump_c and l == 0:
            dump("ut", UT[:], [128, 4, TC], [f"UT{m}" for m in range(4)])
            dump("qkt", QKT[:], [128, 8, TC], [f"QKT{m}" for m in range(8)])
        wbB, wrB = w_get(l, 9)
        wbC, wrC = w_get(l, 10)
        ys_ps, ys_pr = PS[6], PSR[6]

        def s5_front(k):
            q4 = k // 4
            tb, tbr = TAB[k % 2], f"TAB{k % 2}"
            P.dma("sp", tb[:], tabs[l, k], R=[f"tab{l}_{k}"], W=[tbr])
            cs, sn = tb[:, 0:TC], tb[:, TC:2 * TC]
            psr_, prr = bank()
            psi_, pri = bank()
            MM(psr_[:, :], wbB[:, k * 256:k * 256 + 128], UT[:, q4, :], [wrB, f"UT{q4}"], [prr])
            MM(psi_[:, :], wbB[:, k * 256 + 128:k * 256 + 256], UT[:, q4, :], [wrB, f"UT{q4}"], [pri])
            TA, TAR = HTF[:, 3 * TC:4 * TC], ["HT6", "HT7"]
            TT("dve", T[0][:], psr_[:, :], cs, ALU.mult, [prr, tbr], ["T0"])
            TT("dve", T[1][:], psi_[:, :], sn, ALU.mult, [pri, tbr], ["T1"])
            TT("dve", TA, psi_[:, :], cs, ALU.mult, [pri, tbr], TAR)
            TT("dve", GE[:], psr_[:, :], sn, ALU.mult, [prr, tbr], ["GE"])
            TT("dve", BB[:, 0, :], T[0][:], T[1][:], ALU.add, [], ["BB0", "T0", "T1"])
            TT("dve", BB[:, 1, :], TA, GE[:], ALU.subtract, [], ["BB1", "GE"] + TAR)
            ssb = SS if k % 2 == 0 else QKA
            ssr = ("SS0", "SS1") if k % 2 == 0 else ("QKA0", "QKA1")
            for ri in range(2):
                dec_b = S5DEC[:, l, k:k + 1].to_broadcast([128, TC])
                P.op("dve", (lambda e, ri=ri, dec_b=dec_b, init=S5ST[:, l, ri, k:k + 1], o=ssb[:, ri, :]:
                             e.tensor_tensor_scan(out=o, data0=dec_b, data1=BB[:, ri, :], initial=init, op0=ALU.mult, op1=ALU.add)),
                     ["S5DEC", "S5ST", f"BB{ri}"], [ssr[ri]])
                ACT(S5E[:, ri, k:k + 1], ssb[:, ri, TC - 1:TC], AF.Copy, [ssr[ri]], ["S5E"])

        def s5_back(k):
            q4 = k // 4
            tb, tbr = TAB[k % 2], f"TAB{k % 2}"
            cs, sn = tb[:, 0:TC], tb[:, TC:2 * TC]
            ssb = SS if k % 2 == 0 else QKA
            ssr = ("SS0", "SS1") if k % 2 == 0 else ("QKA0", "QKA1")
            xr, xrr = XR[k % 2], f"XR{k % 2}"
            TT("pool", T[2][:], ssb[:, 0, :], cs, ALU.mult, [ssr[0], tbr], ["T2"])
            TT("pool", T[3][:], ssb[:, 1, :], sn, ALU.mult, [ssr[1], tbr], ["T3"])
            TT("pool", xr[:, 0, :], T[2][:], T[3][:], ALU.subtract, [], [xrr, "T2", "T3"])
            TT("pool", T[2][:], ssb[:, 0, :], sn, ALU.mult, [ssr[0], tbr], ["T2"])
            TT("pool", T[3][:], ssb[:, 1, :], cs, ALU.mult, [ssr[1], tbr], ["T3"])
            TT("pool", xr[:, 1, :], T[2][:], T[3][:], ALU.add, [], [xrr, "T2", "T3"])
            MM(ys_ps[:, :], wbC[:, k * 256:k * 256 + 128], xr[:, 0, :], [wrC, xrr], [ys_pr], start=(k % 4 == 0), stop=False)
            MM(ys_ps[:, :], wbC[:, k * 256 + 128:k * 256 + 256], xr[:, 1, :], [wrC, xrr], [ys_pr], start=False, stop=(k % 4 == 3))
            if k % 4 == 3:
                STT("dve", YG[:, q4, :], UT[:, q4, :], sm(l, "dvec", q4), ys_ps[:, :], ALU.mult, ALU.add, [f"UT{q4}", "SM", ys_pr], [f"YG{q4}"])

        def mlstm_gen():
            refresh_state(l)
            yield
            for sc in range(NSUB):
                tsl = slice(sc * 128, (sc + 1) * 128)
                gcol = c * NSUB + sc
                ACT(SP_[:], G[:, sc, 4:8], AF.Exp, [f"G{sc}"], ["SP_"], scale=-1.0)
                ACT(SP_[:], SP_[:], AF.Ln, ["ONE1"], ["SP_"], bias=ONE1[:, 0:1])
                TS("dve", LI[:], G[:, sc, 0:4], PADB[:, gcol:gcol + 1], None, ALU.add, None, [f"G{sc}", "PADB"], ["LI"])
                yield
                for h in range(4):
                    TS("dve", SPB[:, h, :], ONESF, SP_[:, h:h + 1], None, ALU.mult, None, ["CONS", "SP_"], ["SPB"])
                psF, prF = bank()
                MM(psF[:, 0:4], TRIU, SP_[:], ["CONS", "SP_"], [prF])
                MM(psF[:, 4:8], ONESF, SP_[:], ["CONS", "SP_"], [prF])
                yield
                psFr, prFr = bank()
                for h in range(4):
                    MM(psFr[:, h * 128:(h + 1) * 128], SPB[:, h, :], TRIU, ["SPB", "CONS"], [prFr])
                TT("dve", BD[:], psF[:, 0:4], LI[:], ALU.add, [prF, "LI"], ["BD"])
                TT("dve", TW[:], BD[:], psF[:, 4:8], ALU.subtract, [prF, "BD"], ["TW"])
                ACT(WEX[:], TW[:], AF.Exp, ["TW"], ["WEX"])
                ACT(DEC[:], psF[:, 4:8], AF.Exp, [prF], ["DEC"], scale=-1.0)
                TS("dve", BD[:], BD[:], LN_SCALE, None, ALU.add, None, ["TW"], ["BD"])
                yield
                for h in range(4):
                    hs = slice(h * 128, (h + 1) * 128)
                    ACT(DT_[:, hs], psFr[:, hs], AF.Exp, [prFr, "BD"], ["DT_"], scale=-1.0, bias=BD[:, h:h + 1])
                    TT("pool", DTM[:, hs], DT_[:, hs], TRIU, ALU.mult, ["DT_", "CONS"], ["DTM"])
                ACT(EF[:], psFr[:, :], AF.Exp, [prFr, "LNS"], ["EF"], scale=-1.0, bias=LNS[:, 0:1])
                psS, prS = bank()
                for h in range(4):
                    MM(psS[:, h * 128:(h + 1) * 128], QKT[:, 4 + h, tsl], QKT[:, h, tsl], [f"QKT{4 + h}", f"QKT{h}"], [prS])
                for h in range(4):
                    P.op("pe", (lambda e, o=PST[:, h * 128:(h + 1) * 128], i=QKT[:, 4 + h, tsl]: e.transpose(o, i, IDB[:])),
                         [f"QKT{4 + h}", "IDB"], ["PST"])
                yield
                TT("dve", QP[:].rearrange("p (h t) -> p h t", h=4), QKT[:, 0:4, tsl], EF[:].rearrange("p (h t) -> p h t", h=4), ALU.mult,
                   [f"QKT{h}" for h in range(4)] + ["EF"], ["QP"])
                TT("dve", WT[:], psS[:, :], DTM[:], ALU.mult, [prS, "DTM"], ["WT"])
                for h in range(4):
                    hs = slice(h * 128, (h + 1) * 128)
                    TS("dve", KW[:, hs], PST[:, hs], WEX[:, h:h + 1], None, ALU.mult, None, ["PST", "WEX"], ["KW"])
                yield
                psN, prN = bank()
                psD, prD = bank()
                for h in range(4):
                    hs = slice(h * 128, (h + 1) * 128)
                    MM(psN[:, hs], V[:, sc, h, 0:128], WT[:, hs], [f"V{sc}", "WT"], [prN], start=True, stop=False)
                    MM(psN[:, hs], CTB[:, h, :], QP[:, hs], ["CTB", "QP"], [prN], start=False, stop=True)
                    MM(psD[:, hs], ONESB[:], WT[:, hs], ["ONESB", "WT"], [prD], start=True, stop=False)
                    MM(psD[:, hs], NB[:, h, :], QP[:, hs], ["NB", "QP"], [prD], start=False, stop=True)
                psU = []
                for half in range(2):
                    pu, pru = bank()
                    psU.append((pu, pru))
                    for hh in range(2):
                        h = half * 2 + hh
                        MM(pu[:, hh * 129:(hh + 1) * 129], KW[:, h * 128:(h + 1) * 128], V[:, sc, h, :], ["KW", f"V{sc}"], [pru])
                yield
                TS("dve", AD, psD[:, :], -1.0, 1.0, ALU.mult, ALU.max, [prD], ADR)
                TT("dve", AD, AD, psD[:, :], ALU.max, [prD], ADR)
                RECIP(AD, AD, [], ADR)
                TT("dve", HH, psN[:, :], AD, ALU.mult, [prN] + ADR, HHR)
                for half in range(2):
                    pu, pru = psU[half]
                    for hh in range(2):
                        h = half * 2 + hh
                        STT("dve", CT[:, l, h, :], CT[:, l, h, :], DEC[:, h:h + 1], pu[:, hh * 129:(hh + 1) * 129], ALU.mult, ALU.add, [pru, "DEC"], ["CT"])
                yield
                ACT(SQH[:], HH, AF.Square, HHR, ["SQH"])
                if sc < NSUB - 1:
                    refresh_state(l)
                psH, prH = bank()
                for h in range(4):
                    hs = slice(h * 128, (h + 1) * 128)
                    MM(psH[:, hs], ONESB[:], SQH[:, hs], ["ONESB", "SQH"], [prH])
                yield
                ACT(RS, psH[:, :], AF.Ln, [prH, "EPS1"], RSR, scale=1.0 / 128, bias=EPS1[:, 0:1])
                ACT(RS, RS, AF.Exp, [], RSR, scale=-0.5)
                yield
                for h in range(4):
                    hs = slice(h * 128, (h + 1) * 128)
                    STT("dve", HH[:, hs], HH[:, hs], sm(l, "ghead", h), RS[:, hs], ALU.mult, ALU.mult, RSR + ["SM"], HHR)
                TT("pool", YB[:, :, tsl], HH.rearrange("p (h t) -> p h t", h=4), SIGO[:, :, tsl], ALU.mult,
                   HHR + [f"SIGO{h}" for h in range(4)], [f"YB{h}" for h in range(4)])
                yield

        mg = mlstm_gen()

        def pull(n):
            for _ in range(n):
                try:
                    next(mg)
                except StopIteration:
                    return
        s5_front(0)
        pull(3)
        for k in range(1, 16):
            s5_front(k)
            s5_back(k - 1)
            pull(3)
        s5_back(15)
        for _ in mg:
            pass
        c5, s5 = S5ROT[:, l, 0, :], S5ROT[:, l, 1, :]
        TT("dve", SC["t1"], S5E[:, 0, :], c5, ALU.mult, ["S5E", "S5ROT"], ["sc_t1"])
        TT("dve", SC["t2"], S5E[:, 1, :], s5, ALU.mult, ["S5E", "S5ROT"], ["sc_t2"])
        TT("dve", S5ST[:, l, 0, :], SC["t1"], SC["t2"], ALU.subtract, ["sc_t1", "sc_t2"], ["S5ST"])
        TT("dve", SC["t1"], S5E[:, 0, :], s5, ALU.mult, ["S5E", "S5ROT"], ["sc_t1"])
        TT("dve", SC["t2"], S5E[:, 1, :], c5, ALU.mult, ["S5E", "S5ROT"], ["sc_t2"])
        TT("dve", S5ST[:, l, 1, :], SC["t1"], SC["t2"], ALU.add, ["sc_t1", "sc_t2"], ["S5ST"])
        for q4 in range(4):
            ACT(YG[:, q4, :], YG[:, q4, :], AF.Gelu_apprx_tanh, [], [f"YG{q4}"])
        wb, wr = w_get(l, 11)
        YGR = [f"YG{q}" for q in range(4)]
        for mm in range(4):
            ps, pr = bank()
            MMG(ps[:, :], [(wb[:, kc * 512 + mm * 128:kc * 512 + (mm + 1) * 128], YG[:, kc, :]) for kc in range(4)], [wr] + YGR, [pr])
            ACT(GE[:], ps[:, :], AF.Sigmoid, [pr], ["GE"])
            TT("dve", YA[:, mm, :], YG[:, mm, :], GE[:], ALU.mult, ["GE", f"YG{mm}"], [f"YA{mm}"])
        if c == dump_c and l == 0:
            dump("ya", YA[:], [128, 4, TC], [f"YA{m}" for m in range(4)])
        if c == dump_c and l == 0:
            dump("yb", YB[:], [128, 4, TC], [f"YB{m}" for m in range(4)])
        wbA, wrA = w_get(l, 12)
        wbBm, wrBm = w_get(l, 13)
        YAR = [f"YA{q}" for q in range(4)]
        YBR = [f"YB{q}" for q in range(4)]
        for mm in range(KD):
            psa, pra = bank()
            psb, prb = bank()
            MMG(psa[:, :], [(wbA[:, kc * 1024 + mm * 128:kc * 1024 + (mm + 1) * 128], YA[:, kc, :]) for kc in range(4)], [wrA] + YAR, [pra])
            MMG(psb[:, :], [(wbBm[:, kc * 1024 + mm * 128:kc * 1024 + (mm + 1) * 128], YB[:, kc, :]) for kc in range(4)], [wrBm] + YBR, [prb])
            TT("dve", T[0][:], psa[:, :], SIGA[mm], ALU.mult, [pra, f"SIGA{mm}"], ["T0"])
            TT("dve", T[1][:], psb[:, :], SIGB[mm], ALU.mult, [prb, f"SIGB{mm}"], ["T1"])
            TT("pool", MRG[mm], T[0][:], T[1][:], ALU.add, ["T0", "T1"], [f"MRG{mm}"])
        MR = [f"MRG{q}" for q in range(KD)]
        if c == dump_c and l == 0:
            dump("mrg", BIG[:, 2 * KD:3 * KD, :], [128, KD, TC], MR)
        for bi in range(2):
            wb, wr = w_get(l, 14 + bi)
            for mm in range(4):
                t = bi * 4 + mm
                ps, pr = bank()
                MMG(ps[:, :], [(wb[:, kc * 512 + mm * 128:kc * 512 + (mm + 1) * 128], MRG[kc]) for kc in range(KD)], [wr] + MR, [pr])
                ACT(OT[:, t, :], ps[:, :], AF.Copy, [pr], [f"OT{t}"])
        if c == dump_c and l == 0:
            dump("ot", OT[:], [128, KD, TC], [f"OT{t}" for t in range(KD)])
        post_norm_residual(l, "g_post")
        if c == dump_c and l == 0:
            dump("xmid", X[:], [128, KD, TC], ["X"])

    def ffn(c, l):
        pre_norm(l, "gf_pre")
        alias = [f"SIGA{t}" for t in range(KD)] + [f"SIGB{t}" for t in range(KD)] + [f"MRG{t}" for t in range(KD)]
        for bi in range(6):
            wbg, wrg = w_get(l, 16 + 2 * bi)
            wbu, wru = w_get(l, 17 + 2 * bi)
            ncols = 512 if bi < 5 else 256
            for mm in range(ncols // 128):
                j = bi * 4 + mm
                psg, prg = proj_tile(wbg, wrg, mm, ncols)
                psu, pru = proj_tile(wbu, wru, mm, ncols)
                o = _off["wfc"][0] + j * 3
                ACT(GP[:, 2:2 + TC], psg[:, :], AF.Copy, [prg], ["BB0", "BB1"])
                CP("pool", GP[:, 0:2], FH[:, l, j, :], ["FH"], ["BB0", "BB1"])
                ACT(ACC[:], GP[:, 0:TC], AF.Identity, ["BB0", "BB1", "SM"], ["T3"], scale=SM[:, l, o:o + 1], bias=sm(l, "bfc", j))
                STT("dve", ACC[:], GP[:, 1:1 + TC], SM[:, l, o + 1:o + 2], ACC[:], ALU.mult, ALU.add, ["BB0", "BB1", "SM"], ["T3"])
                STT("dve", ACC[:], GP[:, 2:2 + TC], SM[:, l, o + 2:o + 3], ACC[:], ALU.mult, ALU.add, ["BB0", "BB1", "SM"], ["T3"])
                CP("pool", FH[:, l, j, :], GP[:, TC:TC + 2], ["BB0", "BB1"], ["FH"])
                ACT(GE[:], ACC[:], AF.Gelu_apprx_tanh, ["T3"], ["GE"])
                TT("dve", ACTT[j], psu[:, :], GE[:], ALU.mult, [pru, "GE"], [f"ACTT{j}"] + alias)
        AR = [f"ACTT{j}" for j in range(NFT)]
        for mm in range(KD):
            wb, wr = w_get(l, 28 + mm)
            ps, pr = bank()
            MMG(ps[:, :], [(wb[:, j * 128:(j + 1) * 128], ACTT[j]) for j in range(NFT)], [wr] + AR, [pr])
            ACT(OT[:, mm, :], ps[:, :], AF.Copy, [pr], [f"OT{mm}"])
        post_norm_residual(l, "gf_post")
        P.op("act", lambda e: e.activation(out=GE[:, 0:1], in_=GE[:, 0:1], func=AF.Copy), [], AR + alias + ["GE"])

    for c in range(nch_run):
        P.dma("sp", X[:], xT[c], W=["X"])
        for l in range(DEPTH):
            mixer(c, l)
            ffn(c, l)
        P.dma("sp", yT[c], X[:], R=["X"], W=[f"yT{c}"])
    evs = [P.R(f"yT{c}").w for c in range(nch_run)] + list(dump_out.values())
    P.final_wait("sp", evs)
    P.emit()
    st.close()
    return nc


def _kblock(W, n):
    K = W.shape[0]
    kc = K // 128
    a = W.reshape(kc, 128, n).transpose(1, 0, 2).reshape(128, kc * n)
    out = np.zeros((128, 4096), np.float32)
    out[:, :kc * n] = a
    return out


def pack_weights(inp):
    wst = np.zeros((DEPTH, NBLK, 128, 4096), np.float32)
    small = np.zeros((128, DEPTH, NSMALL), np.float32)
    craw = np.zeros((DEPTH, 128, 16, 256), np.float32)

    def put(l, name, arr):
        o, w = _off[name]
        small[:, l, o:o + w] = arr

    for l in range(DEPTH):
        w_in = np.asarray(inp["w_in"][l], np.float32)
        for b, c0 in enumerate((0, 512, 1024, 1536, 2048)):
            wst[l, b] = _kblock(w_in[:, c0:c0 + 512], 512)
        for b, c0 in ((5, 2568), (6, 3080), (7, 3592), (8, 4104)):
            wst[l, b] = _kblock(w_in[:, c0:c0 + 512], 512)
        bre = np.asarray(inp["ssm_b_re"][l], np.float32)
        bim = np.asarray(inp["ssm_b_im"][l], np.float32)
        cre = np.asarray(inp["ssm_c_re"][l], np.float32)
        cim = np.asarray(inp["ssm_c_im"][l], np.float32)
        blkB = np.zeros((128, 16, 256), np.float32)
        for k in range(16):
            for gi in range(2):
                g = 2 * k + gi
                gg = g % 8
                blkB[gg * 16:(gg + 1) * 16, k, gi * 64:(gi + 1) * 64] = bre[g].T
                blkB[gg * 16:(gg + 1) * 16, k, 128 + gi * 64:128 + (gi + 1) * 64] = bim[g].T
                craw[l, gi * 64:(gi + 1) * 64, k, gg * 16:(gg + 1) * 16] = cre[g].T
                craw[l, gi * 64:(gi + 1) * 64, k, 128 + gg * 16:128 + (gg + 1) * 16] = cim[g].T
        wst[l, 9] = blkB.reshape(128, 4096)
        wst[l, 11] = _kblock(np.asarray(inp["w_ssm_glu"][l], np.float32), 512)
        wst[l, 12] = _kblock(np.asarray(inp["w_branch_ssm"][l], np.float32), 1024)
        wst[l, 13] = _kblock(np.asarray(inp["w_branch_mlstm"][l], np.float32), 1024)
        w_out = np.asarray(inp["w_out"][l], np.float32)
        wst[l, 14] = _kblock(w_out[:, 0:512], 512)
        wst[l, 15] = _kblock(w_out[:, 512:1024], 512)
        wg = np.asarray(inp["w_ffn_gate"][l], np.float32)
        wu = np.asarray(inp["w_ffn_up"][l], np.float32)
        for bi in range(6):
            n = 512 if bi < 5 else 256
            wst[l, 16 + 2 * bi] = _kblock(wg[:, bi * 512:bi * 512 + n], n)
            wst[l, 17 + 2 * bi] = _kblock(wu[:, bi * 512:bi * 512 + n], n)
        wd = np.asarray(inp["w_ffn_down"][l], np.float32)
        for m in range(8):
            wst[l, 28 + m] = _kblock(wd[:, m * 128:(m + 1) * 128], 128)
        put(l, "g_pre", np.asarray(inp["g_mix_pre"][l]).reshape(8, 128).T)
        put(l, "g_post", np.asarray(inp["g_mix_post"][l]).reshape(8, 128).T)
        put(l, "gf_pre", np.asarray(inp["g_ffn_pre"][l]).reshape(8, 128).T)
        put(l, "gf_post", np.asarray(inp["g_ffn_post"][l]).reshape(8, 128).T)
        put(l, "wgate", w_in[:, 2560:2568].reshape(8, 128, 8).transpose(1, 0, 2).reshape(128, 64))
        put(l, "bgate", np.broadcast_to(np.asarray(inp["b_gates"][l], np.float32)[None, :], (128, 8)))
        wqk = np.asarray(inp["w_qk_conv"][l], np.float32)
        put(l, "wqk", wqk.reshape(4, 8, 128).transpose(2, 1, 0).reshape(128, 32))
        put(l, "bqk", np.asarray(inp["b_qk_conv"][l]).reshape(8, 128).T)
        put(l, "ghead", np.asarray(inp["g_head_norm"][l]).reshape(4, 128).T)
        wfc = np.asarray(inp["w_ffn_conv"][l], np.float32)
        put(l, "wfc", wfc.reshape(3, 22, 128).transpose(2, 1, 0).reshape(128, 66))
        put(l, "bfc", np.asarray(inp["b_ffn_conv"][l]).reshape(22, 128).T)
        lre = np.asarray(inp["ssm_lambda_re"][l], np.float32)
        lim = np.asarray(inp["ssm_lambda_im"][l], np.float32)
        ldt = np.asarray(inp["ssm_log_dt"][l], np.float32)
        put(l, "lre", lre.reshape(16, 128).T)
        put(l, "lim", lim.reshape(16, 128).T)
        put(l, "ldt", np.repeat(ldt, 64).reshape(16, 128).T)
        put(l, "dvec", np.asarray(inp["ssm_d"][l], np.float32).reshape(4, 128).T)
    return wst, small, craw


def make_consts():
    cst = np.zeros((128, 128 * 3 + TC), np.float32)
    cst[:, 0:128] = np.eye(128, dtype=np.float32)
    cst[:, 128:256] = np.triu(np.ones((128, 128), np.float32))
    cst[:, 256:384] = 1.0
    cst[:, 384:] = np.arange(TC, dtype=np.float32)[None, :]
    return cst


def pack_tokens(x, meta, nch):
    seq = np.zeros((NCH * TC, D), np.float32)
    seq[PADF:PADF + NMETA] = meta
    seq[PADF + NMETA:] = x
    seq = seq[:nch * TC]
    xT = np.ascontiguousarray(seq.reshape(nch, TC, KD, 128).transpose(0, 3, 2, 1))
    tok = np.arange(nch * TC).reshape(nch * NSUB, 128).T
    padb = np.where(tok < PADF, np.float32(-30000.0), np.float32(0.0)).astype(np.float32)
    return xT, np.ascontiguousarray(padb)


_CACHE = {}


def run(inputs, nch, dumps=()):
    key = (nch, tuple(dumps))
    if key not in _CACHE:
        _CACHE[key] = build_program(nch, dumps)
    nc = _CACHE[key]
    wst, small, craw = pack_weights(inputs)
    xT, padb = pack_tokens(np.asarray(inputs["x"], np.float32)[0], np.asarray(inputs["meta_tokens"], np.float32), nch)
    in_map = {"xT": xT, "wst": wst, "small": small, "craw": craw, "consts": make_consts(), "padb": padb}
    res = run_bass_kernel_spmd(nc, [in_map], core_ids=[0])
    return res.results[0]


def kernel(**inputs):
    r = run(inputs, NCH)
    yT = r["yT"]
    seq = yT.transpose(0, 3, 2, 1).reshape(NCH * TC, D)
    out = seq[PADF + NMETA:].reshape(1, SEQ, D)
    return np.ascontiguousarray(out.astype(np.float32))
```

```python
import math
from contextlib import ExitStack
import numpy as np
import concourse.bass as bass
import concourse.mybir as mybir
from concourse.bass_utils import run_bass_kernel_spmd

F32 = mybir.dt.float32
BF16 = mybir.dt.bfloat16
I32 = mybir.dt.int32
AF = mybir.ActivationFunctionType
ALU = mybir.AluOpType

D = 1024
KD = 8
TC = 512
NSUB = 4
DEPTH = 4
NMETA = 16
SEQ = 16384
NCH = 33
PADF = NCH * TC - SEQ - NMETA
FFN = 2816
NFT = 22
NBLK = 36
NBUF = 4
EPS = 1e-6
LN_SCALE = math.log(128.0 ** -0.5)
TWO_PI = 2.0 * math.pi
CW1 = 6.28125
CW2 = TWO_PI - CW1
SEM_LIMIT = 30000
NO_SELF_WAIT = ("pe",)

_off = {}
_o = 0
for _n, _w in [("g_pre", 8), ("g_post", 8), ("gf_pre", 8), ("gf_post", 8), ("wgate", 64), ("bgate", 8),
               ("wqk", 32), ("bqk", 8), ("ghead", 4), ("wfc", 66), ("bfc", 22), ("lre", 16), ("lim", 16),
               ("ldt", 16), ("dvec", 4)]:
    _off[_n] = (_o, _w)
    _o += _w
NSMALL = _o


class Reg:
    __slots__ = ("w", "r", "name")

    def __init__(self, name=""):
        self.w = None
        self.r = []
        self.name = name


class Prog:
    ENGS = ("pe", "act", "dve", "pool", "sp")

    def __init__(self, nc, stack):
        self.nc = nc
        self.stack = stack
        self.q = {e: [] for e in self.ENGS}
        self.cnt = {e: 0 for e in self.ENGS}
        self.sems = {e: [stack.enter_context(nc.semaphore(f"s_{e}_0"))] for e in self.ENGS}
        self.waited = {e: {} for e in self.ENGS}
        self.dma_sems = [stack.enter_context(nc.semaphore(f"s_dma_{i}")) for i in range(8 + NBUF)]
        self.dma_cnt = [0] * (8 + NBUF)
        self.dma_rr = 0
        self.regs = {}
        self.nops = 0
        self.no_self_wait = set(NO_SELF_WAIT)
        self.own_sem_ids = {e: {id(self.sems[e][0])} for e in self.ENGS}

    def R(self, x):
        if isinstance(x, Reg):
            return x
        if x not in self.regs:
            self.regs[x] = Reg(x)
        return self.regs[x]

    def _collect(self, eng, R, W, extra=()):
        evs = list(extra)
        for r in R:
            if r.w is not None:
                evs.append(r.w)
        for w in W:
            if w.w is not None:
                evs.append(w.w)
            evs.extend(w.r)
        need = {}
        own = self.own_sem_ids[eng] if eng in self.no_self_wait else ()
        for (s, v) in evs:
            k = id(s)
            if k in own:
                continue
            if self.waited[eng].get(k, 0) >= v:
                continue
            if k not in need or need[k][1] < v:
                need[k] = (s, v)
        for k, (s, v) in need.items():
            self.waited[eng][k] = v
        return list(need.values())

    def _mark(self, ev, R, W):
        for r in R:
            r.r.append(ev)
        for w in W:
            w.w = ev
            w.r = []

    def op(self, eng, fn, R=(), W=()):
        R = [self.R(x) for x in R]
        W = [self.R(x) for x in W]
        waits = self._collect(eng, R, W)
        if self.cnt[eng] >= SEM_LIMIT:
            self.sems[eng].append(self.stack.enter_context(self.nc.semaphore(f"s_{eng}_{len(self.sems[eng])}")))
            self.own_sem_ids[eng].add(id(self.sems[eng][-1]))
            self.cnt[eng] = 0
        self.cnt[eng] += 1
        sem = self.sems[eng][-1]
        ev = (sem, self.cnt[eng])
        self.q[eng].append((waits, fn, (sem, 1)))
        self._mark(ev, R, W)
        self.nops += 1
        return ev

    def dma(self, eng, out, in_, R=(), W=(), sem_idx=None):
        R = [self.R(x) for x in R]
        W = [self.R(x) for x in W]
        if sem_idx is None:
            sem_idx = self.dma_rr
            self.dma_rr = (self.dma_rr + 1) % 8
        s = self.dma_sems[sem_idx]
        extra = [(s, self.dma_cnt[sem_idx])] if self.dma_cnt[sem_idx] else []
        waits = self._collect(eng, R, W, extra)
        self.dma_cnt[sem_idx] += 16
        ev = (s, self.dma_cnt[sem_idx])
        self.q[eng].append((waits, (lambda e, o=out, i=in_: e.dma_start(out=o, in_=i)), (s, 16)))
        self._mark(ev, R, W)
        return ev

    def final_wait(self, eng, evs):
        self.q[eng].append((list(evs), None, None))

    def emit(self):
        nc = self.nc
        with nc.Block() as block:
            def mk(name):
                def body(e):
                    for waits, fn, inc in self.q[name]:
                        for (s, v) in waits:
                            e.wait_ge(s, v)
                        if fn is not None:
                            ins = fn(e)
                            ins.then_inc(inc[0], inc[1])
                return body
            block.tensor(mk("pe"))
            block.scalar(mk("act"))
            block.vector(mk("dve"))
            block.gpsimd(mk("pool"))
            block.sync(mk("sp"))


def build_program(nch_run, dumps=()):
    nc = bass.Bass("TRN2", target_bir_lowering=False)
    xT = nc.dram_tensor("xT", [nch_run, 128, KD, TC], F32, kind="ExternalInput").ap()
    yT = nc.dram_tensor("yT", [nch_run, 128, KD, TC], F32, kind="ExternalOutput").ap()
    wst = nc.dram_tensor("wst", [DEPTH, NBLK, 128, 4096], F32, kind="ExternalInput").ap()
    small = nc.dram_tensor("small", [128, DEPTH, NSMALL], F32, kind="ExternalInput").ap()
    craw = nc.dram_tensor("craw", [DEPTH, 128, 16, 256], F32, kind="ExternalInput").ap()
    consts = nc.dram_tensor("consts", [128, 128 * 3 + TC], F32, kind="ExternalInput").ap()
    padb = nc.dram_tensor("padb", [128, nch_run * NSUB], F32, kind="ExternalInput").ap()
    tabs = nc.dram_tensor("tabs", [DEPTH, 16, 128, 2 * TC], F32).ap()
    cz = nc.dram_tensor("cz", [DEPTH, 128, 4096], F32).ap()
    czn = nc.dram_tensor("czn", [DEPTH, 128, 4096], F32).ap()
    dump_out = {}
    dump_c = 1 if nch_run > 1 else 0

    st = ExitStack()
    P = Prog(nc, st)

    def sb(name, shape, dt=F32):
        return st.enter_context(nc.sbuf_tensor(name, shape, dt))

    X = sb("X", [128, KD, TC])
    HT = sb("HT", [128, KD, TC], BF16)
    SQ = sb("SQ", [128, 2, TC], BF16)
    RSTD = sb("RSTD", [128, TC])
    OT = sb("OT", [128, KD, TC])
    WB = [sb(f"WB{i}", [128, 4096], BF16) for i in range(NBUF)]
    SM = sb("SM", [128, DEPTH, NSMALL])
    CONS = sb("CONS", [128, 128 * 3 + TC])
    PADB = sb("PADB", [128, nch_run * NSUB])
    IDB = sb("IDB", [128, 128], BF16)
    ONESB = sb("ONESB", [128, 128], BF16)
    WGB = sb("WGB", [128, DEPTH, 64], BF16)
    ONE1 = sb("ONE1", [128, 1])
    LNS = sb("LNS", [128, 1])
    EPS1 = sb("EPS1", [128, 1])
    UT = sb("UT", [128, 4, TC], BF16)
    QKPH = sb("QKPH", [128, DEPTH, 8, 3])
    QKP1 = sb("QKP1", [128, 2, 3 + TC])
    QKA = sb("QKA", [128, 2, TC])
    QKT = sb("QKT", [128, 8, TC], BF16)
    V = sb("V", [128, NSUB, 4, 129], BF16)
    SIGO = sb("SIGO", [128, 4, TC], BF16)
    BIG = sb("BIG", [128, 3 * KD, TC], BF16)
    G = sb("G", [128, NSUB, 8])
    YA = sb("YA", [128, 4, TC], BF16)
    YG = sb("YG", [128, 4, TC], BF16)
    YB = sb("YB", [128, 4, TC], BF16)
    S5DEC = sb("S5DEC", [128, DEPTH, 16])
    S5ROT = sb("S5ROT", [128, DEPTH, 2, 16])
    S5ST = sb("S5ST", [128, DEPTH, 2, 16])
    S5E = sb("S5E", [128, 2, 16])
    TAB = [sb(f"TAB{i}", [128, 2 * TC]) for i in range(2)]
    BB = sb("BB", [128, 2, TC])
    SS = sb("SS", [128, 2, TC])
    T = [sb(f"T{i}", [128, TC]) for i in range(4)]
    XR = [sb(f"XR{i}", [128, 2, TC], BF16) for i in range(2)]
    YS = sb("YS", [128, TC])
    CT = sb("CT", [128, DEPTH, 4, 129])
    CTB = sb("CTB", [128, 4, 128], BF16)
    NB = sb("NB", [128, 4, 128], BF16)
    SP_ = sb("SP_", [128, 4])
    LI = sb("LI", [128, 4])
    SPB = sb("SPB", [128, 4, 128])
    BD = sb("BD", [128, 4])
    TW = sb("TW", [128, 4])
    WEX = sb("WEX", [128, 4])
    DEC = sb("DEC", [128, 4])
    DT_ = sb("DT_", [128, TC])
    DTM = sb("DTM", [128, TC])
    EF = sb("EF", [128, TC])
    QP = sb("QP", [128, TC], BF16)
    WT = sb("WT", [128, TC], BF16)
    KW = sb("KW", [128, TC], BF16)
    SQH = sb("SQH", [128, TC], BF16)
    FH = sb("FH", [128, DEPTH, NFT, 2])
    GE = sb("GE", [128, TC])
    PRK = GE[:].bitcast(I32)
    SCN = ("dt", "th", "ar", "c", "s", "abr", "abi", "nr", "den", "t1", "t2", "zre", "zim", "a16", "c2", "s2", "nzim")
    SCT = sb("SCT", [128, len(SCN), 16])
    SC = {n: SCT[:, i, :] for i, n in enumerate(SCN)}
    SIGA = [BIG[:, t, :] for t in range(KD)]
    SIGB = [BIG[:, KD + t, :] for t in range(KD)]
    MRG = [BIG[:, 2 * KD + t, :] for t in range(KD)]
    ACTT = [BIG[:, j, :] for j in range(NFT)]
    HTF = HT[:].rearrange("p a t -> p (a t)").bitcast(F32)
    AD, HH, RS = HTF[:, 0:TC], HTF[:, TC:2 * TC], HTF[:, 2 * TC:3 * TC]
    ADR, HHR, RSR = ["HT0", "HT1"], ["HT2", "HT3"], ["HT4", "HT5"]
    ACC = T[3]
    GP = BB[:].rearrange("p a t -> p (a t)")

    PS = [st.enter_context(nc.psum_tensor(f"PS{i}", [128, TC], F32)) for i in range(7)]
    PST = st.enter_context(nc.psum_tensor("PST", [128, TC], BF16))
    PSR = [Reg(f"ps{i}") for i in range(7)]
    bank_rr = [0]

    def bank():
        i = bank_rr[0]
        bank_rr[0] = (i + 1) % 6
        return PS[i], PSR[i]

    def ACT(out, in_, func, R, W, **kw):
        P.op("act", lambda e: e.activation(out=out, in_=in_, func=func, **kw), R, W)

    def TT(eng, out, in0, in1, op, R, W):
        P.op(eng, lambda e: e.tensor_tensor(out=out, in0=in0, in1=in1, op=op), R, W)

    def TS(eng, out, in0, s1, s2, op0, op1, R, W):
        if s2 is None:
            P.op(eng, lambda e: e.tensor_scalar(out=out, in0=in0, scalar1=s1, scalar2=None, op0=op0), R, W)
        else:
            P.op(eng, lambda e: e.tensor_scalar(out=out, in0=in0, scalar1=s1, scalar2=s2, op0=op0, op1=op1), R, W)

    def STT(eng, out, in0, scalar, in1, op0, op1, R, W):
        P.op(eng, lambda e: e.scalar_tensor_tensor(out=out, in0=in0, scalar=scalar, in1=in1, op0=op0, op1=op1), R, W)

    def CP(eng, out, in_, R, W):
        P.op(eng, lambda e: e.tensor_copy(out=out, in_=in_), R, W)

    def RECIP(out, in_, R, W):
        P.op("dve", lambda e: e.reciprocal(out=out, in_=in_), R, W)

    def MM(out, lhsT, rhs, R, W, start=True, stop=True):
        P.op("pe", lambda e: e.matmul(out, lhsT=lhsT, rhs=rhs, start=start, stop=stop), R, W)

    def MMG(out, pairs, R, W):
        pairs = list(pairs)

        def fn(e):
            ins = None
            n = len(pairs)
            for i, (a, b) in enumerate(pairs):
                ins = e.matmul(out, lhsT=a, rhs=b, start=(i == 0), stop=(i == n - 1))
            return ins
        P.op("pe", fn, R, W)

    def dump(name, ap, shape, R):
        if name in dumps:
            t = nc.dram_tensor("dbg_" + name, list(shape), F32 if ap.dtype == F32 else BF16, kind="ExternalOutput").ap()
            dump_out[name] = P.dma("sp", t, ap, R=R, W=["dbg_" + name])

    P.dma("sp", SM[:], small[:, :, :], W=["SM"])
    P.dma("sp", CONS[:], consts[:, :], W=["CONS"])
    P.dma("sp", PADB[:], padb[:, :], W=["PADB"])
    IDF = CONS[:, 0:128]
    TRIU = CONS[:, 128:256]
    ONESF = CONS[:, 256:384]
    IOTA = CONS[:, 384:384 + TC]
    CP("dve", IDB[:], IDF, ["CONS"], ["IDB"])
    CP("dve", ONESB[:], ONESF, ["CONS"], ["ONESB"])
    for l in range(DEPTH):
        o = _off["wgate"][0]
        CP("dve", WGB[:, l, :], SM[:, l, o:o + 64], ["SM"], ["WGB"])
    P.op("pool", lambda e: e.memset(V[:], 1.0), W=["V0", "V1", "V2", "V3"])
    P.op("pool", lambda e: e.memset(CT[:], 0.0), W=["CT"])
    P.op("pool", lambda e: e.memset(S5ST[:], 0.0), W=["S5ST"])
    P.op("pool", lambda e: e.memset(QKPH[:], 0.0), W=["QKPH"])
    P.op("pool", lambda e: e.memset(FH[:], 0.0), W=["FH"])
    P.op("pool", lambda e: e.memset(ONE1[:], 1.0), W=["ONE1"])
    P.op("pool", lambda e: e.memset(LNS[:], LN_SCALE), W=["LNS"])
    P.op("pool", lambda e: e.memset(EPS1[:], EPS), W=["EPS1"])

    def sm(l, name, j=None):
        o, w = _off[name]
        if j is None:
            return SM[:, l, o:o + w]
        return SM[:, l, o + j:o + j + 1]

    PRC = OT[:].rearrange("p a t -> p (a t)")
    PRZ = X[:].rearrange("p a t -> p (a t)")
    PRA, PRF, PRT0, PRT1 = T[0], T[1], T[2], T[3]

    OTR = [f"OT{t}" for t in range(KD)]

    def prologue_layer(l):
        s = SC
        lre, lim, ldt = sm(l, "lre"), sm(l, "lim"), sm(l, "ldt")
        dec = S5DEC[:, l, :]
        def exp_taylor(dst, dreg, src, sreg):
            TS("dve", dst, src, 1.0 / 5040, 1.0 / 720, ALU.mult, ALU.add, [sreg], [dreg])
            for cf in (1.0 / 120, 1.0 / 24, 1.0 / 6, 0.5, 1.0, 1.0):
                TT("dve", dst, dst, src, ALU.mult, [sreg], [dreg])
                TS("dve", dst, dst, cf, None, ALU.add, None, [], [dreg])

        def sin_reduced(dst, dreg, ang, areg, shift):
            TS("dve", s["t1"], ang, shift, 1.0 / TWO_PI, ALU.add, ALU.mult, [areg], ["sc_t1"])
            CP("dve", PRK[:, 0:16], s["t1"], ["sc_t1"], ["PRK"])
            CP("dve", s["t1"], PRK[:, 0:16], ["PRK"], ["sc_t1"])
            STT("dve", s["t2"], s["t1"], -CW1, ang, ALU.mult, ALU.add, ["sc_t1", areg], ["sc_t2"])
            STT("dve", s["t2"], s["t1"], -CW2, s["t2"], ALU.mult, ALU.add, ["sc_t1"], ["sc_t2"])
            TS("dve", s["t2"], s["t2"], shift, -math.pi, ALU.add, ALU.max, [], ["sc_t2"])
            TS("dve", s["t2"], s["t2"], math.pi, None, ALU.min, None, [], ["sc_t2"])
            ACT(dst, s["t2"], AF.Sin, ["sc_t2"], [dreg])

        TS("dve", s["a16"], ldt, 1.0 / 16, None, ALU.mult, None, ["SM"], ["sc_a16"])
        exp_taylor(s["dt"], "sc_dt", s["a16"], "sc_a16")
        for _ in range(4):
            TT("dve", s["dt"], s["dt"], s["dt"], ALU.mult, [], ["sc_dt"])
        TT("dve", s["ar"], lre, s["dt"], ALU.mult, ["SM", "sc_dt"], ["sc_ar"])
        TT("dve", s["th"], lim, s["dt"], ALU.mult, ["SM", "sc_dt"], ["sc_th"])
        exp_taylor(dec, "S5DEC", s["ar"], "sc_ar")
        sin_reduced(s["s"], "sc_s", s["th"], "sc_th", 0.0)
        sin_reduced(s["c"], "sc_c", s["th"], "sc_th", math.pi / 2)
        TT("dve", s["abr"], dec, s["c"], ALU.mult, ["S5DEC", "sc_c"], ["sc_abr"])
        TT("dve", s["abi"], dec, s["s"], ALU.mult, ["S5DEC", "sc_s"], ["sc_abi"])
        TS("dve", s["nr"], s["abr"], -1.0, None, ALU.add, None, ["sc_abr"], ["sc_nr"])
        TT("dve", s["t1"], lre, lre, ALU.mult, ["SM"], ["sc_t1"])
        TT("dve", s["t2"], lim, lim, ALU.mult, ["SM"], ["sc_t2"])
        TT("dve", s["den"], s["t1"], s["t2"], ALU.add, ["sc_t1", "sc_t2"], ["sc_den"])
        RECIP(s["den"], s["den"], ["sc_den"], ["sc_den"])
        TT("dve", s["t1"], s["nr"], lre, ALU.mult, ["sc_nr", "SM"], ["sc_t1"])
        TT("dve", s["t2"], s["abi"], lim, ALU.mult, ["sc_abi", "SM"], ["sc_t2"])
        TT("dve", s["t1"], s["t1"], s["t2"], ALU.add, ["sc_t1", "sc_t2"], ["sc_t1"])
        TT("dve", s["zre"], s["t1"], s["den"], ALU.mult, ["sc_t1", "sc_den"], ["sc_zre"])
        TT("dve", s["t1"], s["abi"], lre, ALU.mult, ["sc_abi", "SM"], ["sc_t1"])
        TT("dve", s["t2"], s["nr"], lim, ALU.mult, ["sc_nr", "SM"], ["sc_t2"])
        TT("dve", s["t1"], s["t1"], s["t2"], ALU.subtract, ["sc_t1", "sc_t2"], ["sc_t1"])
        TT("dve", s["zim"], s["t1"], s["den"], ALU.mult, ["sc_t1", "sc_den"], ["sc_zim"])
        TS("dve", s["nzim"], s["zim"], -1.0, None, ALU.mult, None, ["sc_zim"], ["sc_nzim"])
        P.dma("sp", PRC, craw[l].rearrange("p a t -> p (a t)"), W=OTR)
        for k in range(16):
            cre, cim = PRC[:, k * 256:k * 256 + 128], PRC[:, k * 256 + 128:k * 256 + 256]
            o1, o2 = PRZ[:, k * 256:k * 256 + 128], PRZ[:, k * 256 + 128:k * 256 + 256]
            TS("dve", o1, cim, s["nzim"][:, k:k + 1], None, ALU.mult, None, OTR + ["sc_nzim"], ["X"])
            STT("dve", o1, cre, s["zre"][:, k:k + 1], o1, ALU.mult, ALU.add, OTR + ["sc_zre"], ["X"])
            TS("dve", o2, cim, s["zre"][:, k:k + 1], -1.0, ALU.mult, ALU.mult, OTR + ["sc_zre"], ["X"])
            STT("dve", o2, cre, s["nzim"][:, k:k + 1], o2, ALU.mult, ALU.add, OTR + ["sc_nzim"], ["X"])
        P.dma("sp", cz[l], PRZ, R=["X"], W=[f"cz{l}"])
        for k in range(16):
            TS("dve", PRC[:, k * 128:(k + 1) * 128], PRZ[:, k * 256:k * 256 + 128], -1.0, None, ALU.mult, None, ["X"], OTR)
        P.dma("sp", czn[l], PRC, R=OTR, W=[f"czn{l}"] + OTR)
        for k in range(16):
            th = s["th"][:, k:k + 1]
            TS("dve", PRA[:], IOTA, th, None, ALU.mult, None, ["CONS", "sc_th"], ["T0"])
            tb = TAB[k % 2]
            for half, shift in ((1, 0.0), (0, math.pi / 2)):
                TS("dve", PRF[:], PRA[:], shift, 1.0 / TWO_PI, ALU.add, ALU.mult, ["T0"], ["T1"])
                CP("dve", PRK[:], PRF[:], ["T1"], ["PRK"])
                CP("dve", PRF[:], PRK[:], ["PRK"], ["T1"])
                STT("dve", PRT0[:], PRF[:], -CW1, PRA[:], ALU.mult, ALU.add, ["T1", "T0"], ["T2"])
                STT("dve", PRT0[:], PRF[:], -CW2, PRT0[:], ALU.mult, ALU.add, ["T1", "T2"], ["T2"])
                TS("dve", PRT0[:], PRT0[:], shift, -math.pi, ALU.add, ALU.max, ["T2"], ["T2"])
                TS("dve", PRT0[:], PRT0[:], math.pi, None, ALU.min, None, ["T2"], ["T2"])
                ACT(tb[:, half * TC:(half + 1) * TC], PRT0[:], AF.Sin, ["T2"], [f"TAB{k % 2}"])
            c1, s1 = s["c"][:, k:k + 1], s["s"][:, k:k + 1]
            c511, s511 = tb[:, TC - 1:TC], tb[:, 2 * TC - 1:2 * TC]
            tr = f"TAB{k % 2}"
            TT("dve", s["c2"][:, k:k + 1], c511, c1, ALU.mult, [tr, "sc_c"], ["sc_c2"])
            TT("dve", s["s2"][:, k:k + 1], s511, s1, ALU.mult, [tr, "sc_s"], ["sc_s2"])
            TT("dve", S5ROT[:, l, 0, k:k + 1], s["c2"][:, k:k + 1], s["s2"][:, k:k + 1], ALU.subtract, ["sc_c2", "sc_s2"], ["S5ROT"])
            TT("dve", s["c2"][:, k:k + 1], s511, c1, ALU.mult, [tr, "sc_c"], ["sc_c2"])
            TT("dve", s["s2"][:, k:k + 1], c511, s1, ALU.mult, [tr, "sc_s"], ["sc_s2"])
            TT("dve", S5ROT[:, l, 1, k:k + 1], s["c2"][:, k:k + 1], s["s2"][:, k:k + 1], ALU.add, ["sc_c2", "sc_s2"], ["S5ROT"])
            P.dma("sp", tabs[l, k], tb[:], R=[tr], W=[f"tab{l}_{k}", tr])

    for l in range(DEPTH):
        prologue_layer(l)

    border = list(range(11)) + [36] + list(range(11, NBLK))
    wq = [(l, b) for c in range(nch_run) for l in range(DEPTH) for b in border]
    wstate = {"next": 0, "use": 0}
    WBR = [Reg(f"wb{i}") for i in range(NBUF)]

    def w_issue():
        i = wstate["next"]
        l, b = wq[i]
        buf = i % NBUF
        if b == 10:
            P.dma("pool", WB[buf][:], cz[l], R=[f"cz{l}"], W=[WBR[buf]], sem_idx=8 + buf)
        elif b == 36:
            P.dma("pool", WB[buf][:], czn[l], R=[f"czn{l}"], W=[WBR[buf]], sem_idx=8 + buf)
        else:
            P.dma("pool", WB[buf][:], wst[l, b], W=[WBR[buf]], sem_idx=8 + buf)
        wstate["next"] += 1

    def w_get(l, b, extra_hold=0):
        i = wstate["use"]
        assert wq[i] == (l, b), (wq[i], l, b)
        wstate["use"] += 1
        while wstate["next"] < min(len(wq), i + NBUF - 1 - extra_hold):
            w_issue()
        return WB[i % NBUF], WBR[i % NBUF]

    HTR = [f"HT{kt}" for kt in range(KD)]

    def rmsnorm_stats(src_tile, src_reg):
        ps, pr = bank()
        for kt in range(KD):
            ACT(SQ[:, kt % 2, :], src_tile(kt), AF.Square, [src_reg(kt)], [f"SQ{kt % 2}"])
            MM(ps[:, :], ONESB[:], SQ[:, kt % 2, :], ["ONESB", f"SQ{kt % 2}"], [pr], start=(kt == 0), stop=(kt == KD - 1))
        ACT(RSTD[:], ps[:, :], AF.Ln, [pr, "EPS1"], ["RSTD"], scale=1.0 / D, bias=EPS1[:, 0:1])
        ACT(RSTD[:], RSTD[:], AF.Exp, [], ["RSTD"], scale=-0.5)

    def pre_norm(l, gname):
        rmsnorm_stats(lambda kt: X[:, kt, :], lambda kt: "X")
        for kt in range(KD):
            STT("dve", HT[:, kt, :], X[:, kt, :], sm(l, gname, kt), RSTD[:], ALU.mult, ALU.mult, ["X", "RSTD", "SM"], [f"HT{kt}"])

    def post_norm_residual(l, gname):
        rmsnorm_stats(lambda kt: OT[:, kt, :], lambda kt: f"OT{kt}")
        for kt in range(KD):
            STT("dve", OT[:, kt, :], OT[:, kt, :], sm(l, gname, kt), RSTD[:], ALU.mult, ALU.mult, ["RSTD", "SM"], [f"OT{kt}"])
            TT("pool", X[:, kt, :], X[:, kt, :], OT[:, kt, :], ALU.add, [f"OT{kt}"], ["X"])

    def proj_tile(wb, wr, mm, ncols=512):
        ps, pr = bank()
        MMG(ps[:, :], [(wb[:, kc * ncols + mm * 128: kc * ncols + (mm + 1) * 128], HT[:, kc, :]) for kc in range(KD)], [wr] + HTR, [pr])
        return ps, pr

    def refresh_state(l):
        ACT(CTB[:], CT[:, l, :, 0:128], AF.Copy, ["CT"], ["CTB"])
        for h in range(4):
            TS("pool", NB[:, h, :], ONESB[:], CT[:, l, h, 128:129], None, ALU.mult, None, ["ONESB", "CT"], ["NB"])

    def mixer(c, l):
        pre_norm(l, "g_pre")
        if c == dump_c and l == 0:
            dump("ht", HT[:], [128, KD, TC], HTR)
        wb, wr = w_get(l, 0)
        for mm in range(4):
            ps, pr = proj_tile(wb, wr, mm)
            ACT(UT[:, mm, :], ps[:, :], AF.Copy, [pr], [f"UT{mm}"])
        for blk, base in ((1, 0), (2, 4)):
            wb, wr = w_get(l, blk)
            for mm in range(4):
                t = base + mm
                pb = t % 2
                ps, pr = proj_tile(wb, wr, mm)
                rq = f"QKP1_{pb}"
                ACT(QKP1[:, pb, 3:3 + TC], ps[:, :], AF.Copy, [pr], [rq])
                CP("pool", QKP1[:, pb, 0:3], QKPH[:, l, t, :], ["QKPH"], [rq])
                o = _off["wqk"][0] + t * 4
                ACT(QKA[:, pb, :], QKP1[:, pb, 0:TC], AF.Identity, [rq, "SM"], [f"QKA{pb}"], scale=SM[:, l, o:o + 1], bias=sm(l, "bqk", t))
                for j in (1, 2, 3):
                    STT("dve", QKA[:, pb, :], QKP1[:, pb, j:j + TC], SM[:, l, o + j:o + j + 1], QKA[:, pb, :], ALU.mult, ALU.add, [rq, "SM"], [f"QKA{pb}"])
                ACT(QKT[:, t, :], QKA[:, pb, :], AF.Silu, [f"QKA{pb}"], [f"QKT{t}"])
                CP("pool", QKPH[:, l, t, :], QKP1[:, pb, TC:TC + 3], [rq], ["QKPH"])
        wb, wr = w_get(l, 3)
        for sc in range(NSUB):
            ps, pr = bank()
            MMG(ps[:, :], [(HT[:, kc, sc * 128:(sc + 1) * 128], wb[:, kc * 512:(kc + 1) * 512]) for kc in range(KD)], [wr] + HTR, [pr])
            for h in range(4):
                ACT(V[:, sc, h, 0:128], ps[:, h * 128:(h + 1) * 128], AF.Copy, [pr], [f"V{sc}"])
        for sc in range(NSUB):
            ps, pr = bank()
            MMG(ps[:, 0:8], [(HT[:, kc, sc * 128:(sc + 1) * 128], WGB[:, l, kc * 8:kc * 8 + 8]) for kc in range(KD)], ["WGB"] + HTR, [pr])
            TT("dve", G[:, sc, :], ps[:, 0:8], sm(l, "bgate"), ALU.add, [pr, "SM"], [f"G{sc}"])
        wb, wr = w_get(l, 4)
        for mm in range(4):
            ps, pr = proj_tile(wb, wr, mm)
            ACT(SIGO[:, mm, :], ps[:, :], AF.Sigmoid, [pr], [f"SIGO{mm}"])
        for dst, nm, blks in ((SIGA, "SIGA", (5, 6)), (SIGB, "SIGB", (7, 8))):
            for bi, blk in enumerate(blks):
                wb, wr = w_get(l, blk)
                for mm in range(4):
                    t = bi * 4 + mm
                    ps, pr = proj_tile(wb, wr, mm)
                    ACT(dst[t], ps[:, :], AF.Sigmoid, [pr], [f"{nm}{t}"])
        if c == dump_c and l == 0:
            dump("ut", UT[:], [128, 4, TC], [f"UT{m}" for m in range(4)])
            dump("qkt", QKT[:], [128, 8, TC], [f"QKT{m}" for m in range(8)])
        wbB, wrB = w_get(l, 9)
        wbC, wrC = w_get(l, 10)
        wbN, wrN = w_get(l, 36, extra_hold=1)
        ys_ps, ys_pr = PS[6], PSR[6]

        def s5_front(k):
            q4 = k // 4
            tb, tbr = TAB[k % 2], f"TAB{k % 2}"
            P.dma("sp", tb[:], tabs[l, k], R=[f"tab{l}_{k}"], W=[tbr])
            cs, sn = tb[:, 0:TC], tb[:, TC:2 * TC]
            psr_, prr = bank()
            psi_, pri = bank()
            MM(psr_[:, :], wbB[:, k * 256:k * 256 + 128], UT[:, q4, :], [wrB, f"UT{q4}"], [prr])
            MM(psi_[:, :], wbB[:, k * 256 + 128:k * 256 + 256], UT[:, q4, :], [wrB, f"UT{q4}"], [pri])
            TA, TAR = HTF[:, 3 * TC:4 * TC], ["HT6", "HT7"]
            TT("dve", T[0][:], psr_[:, :], cs, ALU.mult, [prr, tbr], ["T0"])
            TT("dve", T[1][:], psi_[:, :], sn, ALU.mult, [pri, tbr], ["T1"])
            TT("dve", TA, psi_[:, :], cs, ALU.mult, [pri, tbr], TAR)
            TT("dve", GE[:], psr_[:, :], sn, ALU.mult, [prr, tbr], ["GE"])
            TT("dve", BB[:, 0, :], T[0][:], T[1][:], ALU.add, [], ["BB0", "T0", "T1"])
            TT("dve", BB[:, 1, :], TA, GE[:], ALU.subtract, [], ["BB1", "GE"] + TAR)
            ssb = SS if k % 2 == 0 else QKA
            ssr = ("SS0", "SS1") if k % 2 == 0 else ("QKA0", "QKA1")
            for ri in range(2):
                dec_b = S5DEC[:, l, k:k + 1].to_broadcast([128, TC])
                P.op("dve", (lambda e, ri=ri, dec_b=dec_b, init=S5ST[:, l, ri, k:k + 1], o=ssb[:, ri, :]:
                             e.tensor_tensor_scan(out=o, data0=dec_b, data1=BB[:, ri, :], initial=init, op0=ALU.mult, op1=ALU.add)),
                     ["S5DEC", "S5ST", f"BB{ri}"], [ssr[ri]])
                ACT(S5E[:, ri, k:k + 1], ssb[:, ri, TC - 1:TC], AF.Copy, [ssr[ri]], ["S5E"])

        def s5_back(k):
            q4 = k // 4
            tb, tbr = TAB[k % 2], f"TAB{k % 2}"
            cs, sn = tb[:, 0:TC], tb[:, TC:2 * TC]
            ssb = SS if k % 2 == 0 else QKA
            ssr = ("SS0", "SS1") if k % 2 == 0 else ("QKA0", "QKA1")
            xr, xrr = XR[k % 2], f"XR{k % 2}"
            xq = T[2 + k % 2][:].bitcast(BF16).rearrange("p (a t) -> p a t", a=2)
            xqr = f"T{2 + k % 2}"
            TT("pool", xr[:, 0, :], ssb[:, 0, :], cs, ALU.mult, [ssr[0], tbr], [xrr + "a"])
            TT("pool", xr[:, 1, :], ssb[:, 1, :], sn, ALU.mult, [ssr[1], tbr], [xrr + "b"])
            link = [xqr] if k < 2 else []
            TT("pool", xq[:, 0, :], ssb[:, 0, :], sn, ALU.mult, [ssr[0], tbr], [xqr + "a"] + link)
            TT("pool", xq[:, 1, :], ssb[:, 1, :], cs, ALU.mult, [ssr[1], tbr], [xqr + "b"] + link)
            cre = wbC[:, k * 256:k * 256 + 128]
            cimn = wbC[:, k * 256 + 128:k * 256 + 256]
            cren = wbN[:, k * 128:(k + 1) * 128]
            MM(ys_ps[:, :], cre, xr[:, 0, :], [wrC, xrr + "a"], [ys_pr], start=(k % 4 == 0), stop=False)
            MM(ys_ps[:, :], cren, xr[:, 1, :], [wrN, xrr + "b"], [ys_pr], start=False, stop=False)
            MM(ys_ps[:, :], cimn, xq[:, 0, :], [wrC, xqr + "a"], [ys_pr], start=False, stop=False)
            MM(ys_ps[:, :], cimn, xq[:, 1, :], [wrC, xqr + "b"], [ys_pr], start=False, stop=(k % 4 == 3))
            if k % 4 == 3:
                STT("dve", YG[:, q4, :], UT[:, q4, :], sm(l, "dvec", q4), ys_ps[:, :], ALU.mult, ALU.add, [f"UT{q4}", "SM", ys_pr], [f"YG{q4}"])

        def mlstm_gen():
            refresh_state(l)
            yield
            for sc in range(NSUB):
                tsl = slice(sc * 128, (sc + 1) * 128)
                gcol = c * NSUB + sc
                ACT(SP_[:], G[:, sc, 4:8], AF.Exp, [f"G{sc}"], ["SP_"], scale=-1.0)
                ACT(SP_[:], SP_[:], AF.Ln, ["ONE1"], ["SP_"], bias=ONE1[:, 0:1])
                TS("dve", LI[:], G[:, sc, 0:4], PADB[:, gcol:gcol + 1], None, ALU.add, None, [f"G{sc}", "PADB"], ["LI"])
                yield
                for h in range(4):
                    TS("dve", SPB[:, h, :], ONESF, SP_[:, h:h + 1], None, ALU.mult, None, ["CONS", "SP_"], ["SPB"])
                psF, prF = bank()
                MM(psF[:, 0:4], TRIU, SP_[:], ["CONS", "SP_"], [prF])
                MM(psF[:, 4:8], ONESF, SP_[:], ["CONS", "SP_"], [prF])
                yield
                psFr, prFr = bank()
                for h in range(4):
                    MM(psFr[:, h * 128:(h + 1) * 128], SPB[:, h, :], TRIU, ["SPB", "CONS"], [prFr])
                TT("dve", BD[:], psF[:, 0:4], LI[:], ALU.add, [prF, "LI"], ["BD"])
                TT("dve", TW[:], BD[:], psF[:, 4:8], ALU.subtract, [prF, "BD"], ["TW"])
                ACT(WEX[:], TW[:], AF.Exp, ["TW"], ["WEX"])
                ACT(DEC[:], psF[:, 4:8], AF.Exp, [prF], ["DEC"], scale=-1.0)
                TS("dve", BD[:], BD[:], LN_SCALE, None, ALU.add, None, ["TW"], ["BD"])
                yield
                for h in range(4):
                    hs = slice(h * 128, (h + 1) * 128)
                    ACT(DT_[:, hs], psFr[:, hs], AF.Exp, [prFr, "BD"], ["DT_"], scale=-1.0, bias=BD[:, h:h + 1])
                    TT("pool", DTM[:, hs], DT_[:, hs], TRIU, ALU.mult, ["DT_", "CONS"], ["DTM"])
                ACT(EF[:], psFr[:, :], AF.Exp, [prFr, "LNS"], ["EF"], scale=-1.0, bias=LNS[:, 0:1])
                psS, prS = bank()
                for h in range(4):
                    MM(psS[:, h * 128:(h + 1) * 128], QKT[:, 4 + h, tsl], QKT[:, h, tsl], [f"QKT{4 + h}", f"QKT{h}"], [prS])
                for h in range(4):
                    P.op("pe", (lambda e, o=PST[:, h * 128:(h + 1) * 128], i=QKT[:, 4 + h, tsl]: e.transpose(o, i, IDB[:])),
                         [f"QKT{4 + h}", "IDB"], ["PST"])
                yield
                TT("dve", QP[:].rearrange("p (h t) -> p h t", h=4), QKT[:, 0:4, tsl], EF[:].rearrange("p (h t) -> p h t", h=4), ALU.mult,
                   [f"QKT{h}" for h in range(4)] + ["EF"], ["QP"])
                TT("dve", WT[:], psS[:, :], DTM[:], ALU.mult, [prS, "DTM"], ["WT"])
                for h in range(4):
                    hs = slice(h * 128, (h + 1) * 128)
                    TS("dve", KW[:, hs], PST[:, hs], WEX[:, h:h + 1], None, ALU.mult, None, ["PST", "WEX"], ["KW"])
                yield
                psN, prN = bank()
                psD, prD = bank()
                for h in range(4):
                    hs = slice(h * 128, (h + 1) * 128)
                    MM(psN[:, hs], V[:, sc, h, 0:128], WT[:, hs], [f"V{sc}", "WT"], [prN], start=True, stop=False)
                    MM(psN[:, hs], CTB[:, h, :], QP[:, hs], ["CTB", "QP"], [prN], start=False, stop=True)
                    MM(psD[:, hs], ONESB[:], WT[:, hs], ["ONESB", "WT"], [prD], start=True, stop=False)
                    MM(psD[:, hs], NB[:, h, :], QP[:, hs], ["NB", "QP"], [prD], start=False, stop=True)
                psU = []
                for half in range(2):
                    pu, pru = bank()
                    psU.append((pu, pru))
                    for hh in range(2):
                        h = half * 2 + hh
                        MM(pu[:, hh * 129:(hh + 1) * 129], KW[:, h * 128:(h + 1) * 128], V[:, sc, h, :], ["KW", f"V{sc}"], [pru])
                yield
                TS("dve", AD, psD[:, :], -1.0, 1.0, ALU.mult, ALU.max, [prD], ADR)
                TT("dve", AD, AD, psD[:, :], ALU.max, [prD], ADR)
                RECIP(AD, AD, [], ADR)
                TT("dve", HH, psN[:, :], AD, ALU.mult, [prN] + ADR, HHR)
                for half in range(2):
                    pu, pru = psU[half]
                    for hh in range(2):
                        h = half * 2 + hh
                        STT("dve", CT[:, l, h, :], CT[:, l, h, :], DEC[:, h:h + 1], pu[:, hh * 129:(hh + 1) * 129], ALU.mult, ALU.add, [pru, "DEC"], ["CT"])
                yield
                ACT(SQH[:], HH, AF.Square, HHR, ["SQH"])
                if sc < NSUB - 1:
                    refresh_state(l)
                psH, prH = bank()
                for h in range(4):
                    hs = slice(h * 128, (h + 1) * 128)
                    MM(psH[:, hs], ONESB[:], SQH[:, hs], ["ONESB", "SQH"], [prH])
                yield
                ACT(RS, psH[:, :], AF.Ln, [prH, "EPS1"], RSR, scale=1.0 / 128, bias=EPS1[:, 0:1])
                ACT(RS, RS, AF.Exp, [], RSR, scale=-0.5)
                yield
                for h in range(4):
                    hs = slice(h * 128, (h + 1) * 128)
                    STT("dve", HH[:, hs], HH[:, hs], sm(l, "ghead", h), RS[:, hs], ALU.mult, ALU.mult, RSR + ["SM"], HHR)
                TT("pool", YB[:, :, tsl], HH.rearrange("p (h t) -> p h t", h=4), SIGO[:, :, tsl], ALU.mult,
                   HHR + [f"SIGO{h}" for h in range(4)], [f"YB{h}" for h in range(4)])
                yield

        mg = mlstm_gen()

        def pull(n):
            for _ in range(n):
                try:
                    next(mg)
                except StopIteration:
                    return
        s5_front(0)
        pull(3)
        for k in range(1, 16):
            s5_front(k)
            s5_back(k - 1)
            pull(3)
        s5_back(15)
        for _ in mg:
            pass
        c5, s5 = S5ROT[:, l, 0, :], S5ROT[:, l, 1, :]
        TT("dve", SC["t1"], S5E[:, 0, :], c5, ALU.mult, ["S5E", "S5ROT"], ["sc_t1"])
        TT("dve", SC["t2"], S5E[:, 1, :], s5, ALU.mult, ["S5E", "S5ROT"], ["sc_t2"])
        TT("dve", S5ST[:, l, 0, :], SC["t1"], SC["t2"], ALU.subtract, ["sc_t1", "sc_t2"], ["S5ST"])
        TT("dve", SC["t1"], S5E[:, 0, :], s5, ALU.mult, ["S5E", "S5ROT"], ["sc_t1"])
        TT("dve", SC["t2"], S5E[:, 1, :], c5, ALU.mult, ["S5E", "S5ROT"], ["sc_t2"])
        TT("dve", S5ST[:, l, 1, :], SC["t1"], SC["t2"], ALU.add, ["sc_t1", "sc_t2"], ["S5ST"])
        for q4 in range(4):
            ACT(YG[:, q4, :], YG[:, q4, :], AF.Gelu_apprx_tanh, [], [f"YG{q4}"])
        wb, wr = w_get(l, 11)
        YGR = [f"YG{q}" for q in range(4)]
        for mm in range(4):
            ps, pr = bank()
            MMG(ps[:, :], [(wb[:, kc * 512 + mm * 128:kc * 512 + (mm + 1) * 128], YG[:, kc, :]) for kc in range(4)], [wr] + YGR, [pr])
            ACT(GE[:], ps[:, :], AF.Sigmoid, [pr], ["GE"])
            TT("dve", YA[:, mm, :], YG[:, mm, :], GE[:], ALU.mult, ["GE", f"YG{mm}"], [f"YA{mm}"])
        if c == dump_c and l == 0:
            dump("ya", YA[:], [128, 4, TC], [f"YA{m}" for m in range(4)])
        if c == dump_c and l == 0:
            dump("yb", YB[:], [128, 4, TC], [f"YB{m}" for m in range(4)])
        wbA, wrA = w_get(l, 12)
        wbBm, wrBm = w_get(l, 13)
        YAR = [f"YA{q}" for q in range(4)]
        YBR = [f"YB{q}" for q in range(4)]
        for mm in range(KD):
            psa, pra = bank()
            psb, prb = bank()
            MMG(psa[:, :], [(wbA[:, kc * 1024 + mm * 128:kc * 1024 + (mm + 1) * 128], YA[:, kc, :]) for kc in range(4)], [wrA] + YAR, [pra])
            MMG(psb[:, :], [(wbBm[:, kc * 1024 + mm * 128:kc * 1024 + (mm + 1) * 128], YB[:, kc, :]) for kc in range(4)], [wrBm] + YBR, [prb])
            TT("dve", T[0][:], psa[:, :], SIGA[mm], ALU.mult, [pra, f"SIGA{mm}"], ["T0"])
            TT("dve", T[1][:], psb[:, :], SIGB[mm], ALU.mult, [prb, f"SIGB{mm}"], ["T1"])
            TT("pool", MRG[mm], T[0][:], T[1][:], ALU.add, ["T0", "T1"], [f"MRG{mm}"])
        MR = [f"MRG{q}" for q in range(KD)]
        if c == dump_c and l == 0:
            dump("mrg", BIG[:, 2 * KD:3 * KD, :], [128, KD, TC], MR)
        for bi in range(2):
            wb, wr = w_get(l, 14 + bi)
            for mm in range(4):
                t = bi * 4 + mm
                ps, pr = bank()
                MMG(ps[:, :], [(wb[:, kc * 512 + mm * 128:kc * 512 + (mm + 1) * 128], MRG[kc]) for kc in range(KD)], [wr] + MR, [pr])
                ACT(OT[:, t, :], ps[:, :], AF.Copy, [pr], [f"OT{t}"])
        if c == dump_c and l == 0:
            dump("ot", OT[:], [128, KD, TC], [f"OT{t}" for t in range(KD)])
        post_norm_residual(l, "g_post")
        if c == dump_c and l == 0:
            dump("xmid", X[:], [128, KD, TC], ["X"])

    def ffn(c, l):
        pre_norm(l, "gf_pre")
        alias = [f"SIGA{t}" for t in range(KD)] + [f"SIGB{t}" for t in range(KD)] + [f"MRG{t}" for t in range(KD)]
        for bi in range(6):
            wbg, wrg = w_get(l, 16 + 2 * bi)
            wbu, wru = w_get(l, 17 + 2 * bi)
            ncols = 512 if bi < 5 else 256
            for mm in range(ncols // 128):
                j = bi * 4 + mm
                psg, prg = proj_tile(wbg, wrg, mm, ncols)
                psu, pru = proj_tile(wbu, wru, mm, ncols)
                o = _off["wfc"][0] + j * 3
                ACT(GP[:, 2:2 + TC], psg[:, :], AF.Copy, [prg], ["BB0", "BB1"])
                CP("pool", GP[:, 0:2], FH[:, l, j, :], ["FH"], ["BB0", "BB1"])
                ACT(ACC[:], GP[:, 0:TC], AF.Identity, ["BB0", "BB1", "SM"], ["T3"] + (["T3a", "T3b"] if j == 0 else []),
                    scale=SM[:, l, o:o + 1], bias=sm(l, "bfc", j))
                STT("dve", ACC[:], GP[:, 1:1 + TC], SM[:, l, o + 1:o + 2], ACC[:], ALU.mult, ALU.add, ["BB0", "BB1", "SM"], ["T3"])
                STT("dve", ACC[:], GP[:, 2:2 + TC], SM[:, l, o + 2:o + 3], ACC[:], ALU.mult, ALU.add, ["BB0", "BB1", "SM"], ["T3"])
                CP("pool", FH[:, l, j, :], GP[:, TC:TC + 2], ["BB0", "BB1"], ["FH"])
                ACT(GE[:], ACC[:], AF.Gelu_apprx_tanh, ["T3"], ["GE"])
                TT("dve", ACTT[j], psu[:, :], GE[:], ALU.mult, [pru, "GE"], [f"ACTT{j}"] + alias)
        AR = [f"ACTT{j}" for j in range(NFT)]
        for mm in range(KD):
            wb, wr = w_get(l, 28 + mm)
            ps, pr = bank()
            MMG(ps[:, :], [(wb[:, j * 128:(j + 1) * 128], ACTT[j]) for j in range(NFT)], [wr] + AR, [pr])
            ACT(OT[:, mm, :], ps[:, :], AF.Copy, [pr], [f"OT{mm}"])
        post_norm_residual(l, "gf_post")
        P.op("act", lambda e: e.activation(out=GE[:, 0:1], in_=GE[:, 0:1], func=AF.Copy), [], AR + alias + ["GE"])

    for c in range(nch_run):
        P.dma("sp", X[:], xT[c], W=["X"])
        for l in range(DEPTH):
            mixer(c, l)
            ffn(c, l)
        P.dma("sp", yT[c], X[:], R=["X"], W=[f"yT{c}"])
    evs = [P.R(f"yT{c}").w for c in range(nch_run)] + list(dump_out.values())
    P.final_wait("sp", evs)
    P.emit()
    st.close()
    return nc


def _kblock(W, n):
    K = W.shape[0]
    kc = K // 128
    a = W.reshape(kc, 128, n).transpose(1, 0, 2).reshape(128, kc * n)
    out = np.zeros((128, 4096), np.float32)
    out[:, :kc * n] = a
    return out


def pack_weights(inp):
    wst = np.zeros((DEPTH, NBLK, 128, 4096), np.float32)
    small = np.zeros((128, DEPTH, NSMALL), np.float32)
    craw = np.zeros((DEPTH, 128, 16, 256), np.float32)

    def put(l, name, arr):
        o, w = _off[name]
        small[:, l, o:o + w] = arr

    for l in range(DEPTH):
        w_in = np.asarray(inp["w_in"][l], np.float32)
        for b, c0 in enumerate((0, 512, 1024, 1536, 2048)):
            wst[l, b] = _kblock(w_in[:, c0:c0 + 512], 512)
        for b, c0 in ((5, 2568), (6, 3080), (7, 3592), (8, 4104)):
            wst[l, b] = _kblock(w_in[:, c0:c0 + 512], 512)
        bre = np.asarray(inp["ssm_b_re"][l], np.float32)
        bim = np.asarray(inp["ssm_b_im"][l], np.float32)
        cre = np.asarray(inp["ssm_c_re"][l], np.float32)
        cim = np.asarray(inp["ssm_c_im"][l], np.float32)
        blkB = np.zeros((128, 16, 256), np.float32)
        for k in range(16):
            for gi in range(2):
                g = 2 * k + gi
                gg = g % 8
                blkB[gg * 16:(gg + 1) * 16, k, gi * 64:(gi + 1) * 64] = bre[g].T
                blkB[gg * 16:(gg + 1) * 16, k, 128 + gi * 64:128 + (gi + 1) * 64] = bim[g].T
                craw[l, gi * 64:(gi + 1) * 64, k, gg * 16:(gg + 1) * 16] = cre[g].T
                craw[l, gi * 64:(gi + 1) * 64, k, 128 + gg * 16:128 + (gg + 1) * 16] = cim[g].T
        wst[l, 9] = blkB.reshape(128, 4096)
        wst[l, 11] = _kblock(np.asarray(inp["w_ssm_glu"][l], np.float32), 512)
        wst[l, 12] = _kblock(np.asarray(inp["w_branch_ssm"][l], np.float32), 1024)
        wst[l, 13] = _kblock(np.asarray(inp["w_branch_mlstm"][l], np.float32), 1024)
        w_out = np.asarray(inp["w_out"][l], np.float32)
        wst[l, 14] = _kblock(w_out[:, 0:512], 512)
        wst[l, 15] = _kblock(w_out[:, 512:1024], 512)
        wg = np.asarray(inp["w_ffn_gate"][l], np.float32)
        wu = np.asarray(inp["w_ffn_up"][l], np.float32)
        for bi in range(6):
            n = 512 if bi < 5 else 256
            wst[l, 16 + 2 * bi] = _kblock(wg[:, bi * 512:bi * 512 + n], n)
            wst[l, 17 + 2 * bi] = _kblock(wu[:, bi * 512:bi * 512 + n], n)
        wd = np.asarray(inp["w_ffn_down"][l], np.float32)
        for m in range(8):
            wst[l, 28 + m] = _kblock(wd[:, m * 128:(m + 1) * 128], 128)
        put(l, "g_pre", np.asarray(inp["g_mix_pre"][l]).reshape(8, 128).T)
        put(l, "g_post", np.asarray(inp["g_mix_post"][l]).reshape(8, 128).T)
        put(l, "gf_pre", np.asarray(inp["g_ffn_pre"][l]).reshape(8, 128).T)
        put(l, "gf_post", np.asarray(inp["g_ffn_post"][l]).reshape(8, 128).T)
        put(l, "wgate", w_in[:, 2560:2568].reshape(8, 128, 8).transpose(1, 0, 2).reshape(128, 64))
        put(l, "bgate", np.broadcast_to(np.asarray(inp["b_gates"][l], np.float32)[None, :], (128, 8)))
        wqk = np.asarray(inp["w_qk_conv"][l], np.float32)
        put(l, "wqk", wqk.reshape(4, 8, 128).transpose(2, 1, 0).reshape(128, 32))
        put(l, "bqk", np.asarray(inp["b_qk_conv"][l]).reshape(8, 128).T)
        put(l, "ghead", np.asarray(inp["g_head_norm"][l]).reshape(4, 128).T)
        wfc = np.asarray(inp["w_ffn_conv"][l], np.float32)
        put(l, "wfc", wfc.reshape(3, 22, 128).transpose(2, 1, 0).reshape(128, 66))
        put(l, "bfc", np.asarray(inp["b_ffn_conv"][l]).reshape(22, 128).T)
        lre = np.asarray(inp["ssm_lambda_re"][l], np.float32)
        lim = np.asarray(inp["ssm_lambda_im"][l], np.float32)
        ldt = np.asarray(inp["ssm_log_dt"][l], np.float32)
        put(l, "lre", lre.reshape(16, 128).T)
        put(l, "lim", lim.reshape(16, 128).T)
        put(l, "ldt", np.repeat(ldt, 64).reshape(16, 128).T)
        put(l, "dvec", np.asarray(inp["ssm_d"][l], np.float32).reshape(4, 128).T)
    return wst, small, craw


def make_consts():
    cst = np.zeros((128, 128 * 3 + TC), np.float32)
    cst[:, 0:128] = np.eye(128, dtype=np.float32)
    cst[:, 128:256] = np.triu(np.ones((128, 128), np.float32))
    cst[:, 256:384] = 1.0
    cst[:, 384:] = np.arange(TC, dtype=np.float32)[None, :]
    return cst


def pack_tokens(x, meta, nch):
    seq = np.zeros((NCH * TC, D), np.float32)
    seq[PADF:PADF + NMETA] = meta
    seq[PADF + NMETA:] = x
    seq = seq[:nch * TC]
    xT = np.ascontiguousarray(seq.reshape(nch, TC, KD, 128).transpose(0, 3, 2, 1))
    tok = np.arange(nch * TC).reshape(nch * NSUB, 128).T
    padb = np.where(tok < PADF, np.float32(-30000.0), np.float32(0.0)).astype(np.float32)
    return xT, np.ascontiguousarray(padb)


_CACHE = {}


def run(inputs, nch, dumps=()):
    key = (nch, tuple(dumps))
    if key not in _CACHE:
        _CACHE[key] = build_program(nch, dumps)
    nc = _CACHE[key]
    wst, small, craw = pack_weights(inputs)
    xT, padb = pack_tokens(np.asarray(inputs["x"], np.float32)[0], np.asarray(inputs["meta_tokens"], np.float32), nch)
    in_map = {"xT": xT, "wst": wst, "small": small, "craw": craw, "consts": make_consts(), "padb": padb}
    res = run_bass_kernel_spmd(nc, [in_map], core_ids=[0])
    return res.results[0]


def kernel(**inputs):
    r = run(inputs, NCH)
    yT = r["yT"]
    seq = yT.transpose(0, 3, 2, 1).reshape(NCH * TC, D)
    out = seq[PADF + NMETA:].reshape(1, SEQ, D)
    return np.ascontiguousarray(out.astype(np.float32))
```

```python
import math
from contextlib import ExitStack
import numpy as np
import concourse.bass as bass
import concourse.mybir as mybir
from concourse.bass_utils import run_bass_kernel_spmd

F32 = mybir.dt.float32
BF16 = mybir.dt.bfloat16
I32 = mybir.dt.int32
AF = mybir.ActivationFunctionType
ALU = mybir.AluOpType

D = 1024
KD = 8
TC = 512
NSUB = 4
DEPTH = 4
NMETA = 16
SEQ = 16384
NCH = 33
PADF = NCH * TC - SEQ - NMETA
FFN = 2816
NFT = 22
NBLK = 36
NBUF = 4
EPS = 1e-6
LN_SCALE = math.log(128.0 ** -0.5)
TWO_PI = 2.0 * math.pi
CW1 = 6.28125
CW2 = TWO_PI - CW1
SEM_LIMIT = 30000
NO_SELF_WAIT = ("pe",)

_off = {}
_o = 0
for _n, _w in [("g_pre", 8), ("g_post", 8), ("gf_pre", 8), ("gf_post", 8), ("wgate", 64), ("bgate", 8),
               ("wqk", 32), ("bqk", 8), ("ghead", 4), ("wfc", 66), ("bfc", 22), ("lre", 16), ("lim", 16),
               ("ldt", 16), ("dvec", 4)]:
    _off[_n] = (_o, _w)
    _o += _w
NSMALL = _o


class Reg:
    __slots__ = ("w", "r", "name")

    def __init__(self, name=""):
        self.w = None
        self.r = []
        self.name = name


class Prog:
    ENGS = ("pe", "act", "dve", "pool", "sp")

    def __init__(self, nc, stack):
        self.nc = nc
        self.stack = stack
        self.q = {e: [] for e in self.ENGS}
        self.cnt = {e: 0 for e in self.ENGS}
        self.sems = {e: [stack.enter_context(nc.semaphore(f"s_{e}_0"))] for e in self.ENGS}
        self.waited = {e: {} for e in self.ENGS}
        self.dma_sems = [stack.enter_context(nc.semaphore(f"s_dma_{i}")) for i in range(8 + NBUF)]
        self.dma_cnt = [0] * (8 + NBUF)
        self.dma_rr = 0
        self.regs = {}
        self.nops = 0
        self.no_self_wait = set(NO_SELF_WAIT)
        self.own_sem_ids = {e: {id(self.sems[e][0])} for e in self.ENGS}

    def R(self, x):
        if isinstance(x, Reg):
            return x
        if x not in self.regs:
            self.regs[x] = Reg(x)
        return self.regs[x]

    def _collect(self, eng, R, W, extra=()):
        evs = list(extra)
        for r in R:
            if r.w is not None:
                evs.append(r.w)
        for w in W:
            if w.w is not None:
                evs.append(w.w)
            evs.extend(w.r)
        need = {}
        own = self.own_sem_ids[eng] if eng in self.no_self_wait else ()
        for (s, v) in evs:
            k = id(s)
            if k in own:
                continue
            if self.waited[eng].get(k, 0) >= v:
                continue
            if k not in need or need[k][1] < v:
                need[k] = (s, v)
        for k, (s, v) in need.items():
            self.waited[eng][k] = v
        return list(need.values())

    def _mark(self, ev, R, W):
        for r in R:
            r.r.append(ev)
        for w in W:
            w.w = ev
            w.r = []

    def op(self, eng, fn, R=(), W=()):
        R = [self.R(x) for x in R]
        W = [self.R(x) for x in W]
        waits = self._collect(eng, R, W)
        if self.cnt[eng] >= SEM_LIMIT:
            self.sems[eng].append(self.stack.enter_context(self.nc.semaphore(f"s_{eng}_{len(self.sems[eng])}")))
            self.own_sem_ids[eng].add(id(self.sems[eng][-1]))
            self.cnt[eng] = 0
        self.cnt[eng] += 1
        sem = self.sems[eng][-1]
        ev = (sem, self.cnt[eng])
        self.q[eng].append((waits, fn, (sem, 1)))
        self._mark(ev, R, W)
        self.nops += 1
        return ev

    def dma(self, eng, out, in_, R=(), W=(), sem_idx=None):
        R = [self.R(x) for x in R]
        W = [self.R(x) for x in W]
        if sem_idx is None:
            sem_idx = self.dma_rr
            self.dma_rr = (self.dma_rr + 1) % 8
        s = self.dma_sems[sem_idx]
        extra = [(s, self.dma_cnt[sem_idx])] if self.dma_cnt[sem_idx] else []
        waits = self._collect(eng, R, W, extra)
        self.dma_cnt[sem_idx] += 16
        ev = (s, self.dma_cnt[sem_idx])
        self.q[eng].append((waits, (lambda e, o=out, i=in_: e.dma_start(out=o, in_=i)), (s, 16)))
        self._mark(ev, R, W)
        return ev

    def final_wait(self, eng, evs):
        self.q[eng].append((list(evs), None, None))

    def emit(self):
        nc = self.nc
        with nc.Block() as block:
            def mk(name):
                def body(e):
                    for waits, fn, inc in self.q[name]:
                        for (s, v) in waits:
                            e.wait_ge(s, v)
                        if fn is not None:
                            ins = fn(e)
                            ins.then_inc(inc[0], inc[1])
                return body
            block.tensor(mk("pe"))
            block.scalar(mk("act"))
            block.vector(mk("dve"))
            block.gpsimd(mk("pool"))
            block.sync(mk("sp"))


def build_program(nch_run, dumps=()):
    nc = bass.Bass("TRN2", target_bir_lowering=False)
    xT = nc.dram_tensor("xT", [nch_run, 128, KD, TC], F32, kind="ExternalInput").ap()
    yT = nc.dram_tensor("yT", [nch_run, 128, KD, TC], F32, kind="ExternalOutput").ap()
    wst = nc.dram_tensor("wst", [DEPTH, NBLK, 128, 4096], F32, kind="ExternalInput").ap()
    small = nc.dram_tensor("small", [128, DEPTH, NSMALL], F32, kind="ExternalInput").ap()
    craw = nc.dram_tensor("craw", [DEPTH, 128, 16, 256], F32, kind="ExternalInput").ap()
    consts = nc.dram_tensor("consts", [128, 128 * 3 + TC], F32, kind="ExternalInput").ap()
    padb = nc.dram_tensor("padb", [128, nch_run * NSUB], F32, kind="ExternalInput").ap()
    tabs = nc.dram_tensor("tabs", [DEPTH, 16, 128, 2 * TC], F32).ap()
    cz = nc.dram_tensor("cz", [DEPTH, 128, 4096], F32).ap()
    czn = nc.dram_tensor("czn", [DEPTH, 128, 4096], F32).ap()
    dump_out = {}
    dump_c = 1 if nch_run > 1 else 0

    st = ExitStack()
    P = Prog(nc, st)

    def sb(name, shape, dt=F32):
        return st.enter_context(nc.sbuf_tensor(name, shape, dt))

    X = sb("X", [128, KD, TC])
    HT = sb("HT", [128, KD, TC], BF16)
    SQ = sb("SQ", [128, 2, TC], BF16)
    RSTD = sb("RSTD", [128, TC])
    OT = sb("OT", [128, KD, TC])
    WB = [sb(f"WB{i}", [128, 4096], BF16) for i in range(NBUF)]
    SM = sb("SM", [128, DEPTH, NSMALL])
    CONS = sb("CONS", [128, 128 * 3 + TC])
    PADB = sb("PADB", [128, nch_run * NSUB])
    IDB = sb("IDB", [128, 128], BF16)
    ONESB = sb("ONESB", [128, 128], BF16)
    WGB = sb("WGB", [128, DEPTH, 64], BF16)
    ONE1 = sb("ONE1", [128, 1])
    LNS = sb("LNS", [128, 1])
    EPS1 = sb("EPS1", [128, 1])
    UT = sb("UT", [128, 4, TC], BF16)
    QKPH = sb("QKPH", [128, DEPTH, 8, 3])
    QKP1 = sb("QKP1", [128, 2, 3 + TC])
    QKA = sb("QKA", [128, 2, TC])
    QKT = sb("QKT", [128, 8, TC], BF16)
    V = sb("V", [128, NSUB, 4, 129], BF16)
    SIGO = sb("SIGO", [128, 4, TC], BF16)
    BIG = sb("BIG", [128, 3 * KD, TC], BF16)
    G = sb("G", [128, NSUB, 8])
    YA = sb("YA", [128, 4, TC], BF16)
    YG = sb("YG", [128, 4, TC], BF16)
    YB = sb("YB", [128, 4, TC], BF16)
    S5DEC = sb("S5DEC", [128, DEPTH, 16])
    S5ROT = sb("S5ROT", [128, DEPTH, 2, 16])
    S5ST = sb("S5ST", [128, DEPTH, 2, 16])
    S5E = sb("S5E", [128, 2, 16])
    TAB = [sb(f"TAB{i}", [128, 2 * TC]) for i in range(2)]
    BB = sb("BB", [128, 2, TC])
    SS = sb("SS", [128, 2, TC])
    T = [sb(f"T{i}", [128, TC]) for i in range(4)]
    XR = [sb(f"XR{i}", [128, 2, TC], BF16) for i in range(2)]
    YS = sb("YS", [128, TC])
    CT = sb("CT", [128, DEPTH, 4, 129])
    CTB = sb("CTB", [128, 4, 128], BF16)
    NB = sb("NB", [128, 4, 128], BF16)
    SP_ = sb("SP_", [128, 4])
    LI = sb("LI", [128, 4])
    SPB = sb("SPB", [128, 4, 128])
    BD = sb("BD", [128, 4])
    TW = sb("TW", [128, 4])
    WEX = sb("WEX", [128, 4])
    DEC = sb("DEC", [128, 4])
    DT_ = sb("DT_", [128, TC])
    DTM = sb("DTM", [128, TC])
    EF = sb("EF", [128, TC])
    QP = sb("QP", [128, TC], BF16)
    WT = sb("WT", [128, TC], BF16)
    KW = sb("KW", [128, TC], BF16)
    SQH = sb("SQH", [128, TC], BF16)
    FH = sb("FH", [128, DEPTH, NFT, 2])
    GE = sb("GE", [128, TC])
    PRK = GE[:].bitcast(I32)
    SCN = ("dt", "th", "ar", "c", "s", "abr", "abi", "nr", "den", "t1", "t2", "zre", "zim", "a16", "c2", "s2", "nzim")
    SCT = sb("SCT", [128, len(SCN), 16])
    SC = {n: SCT[:, i, :] for i, n in enumerate(SCN)}
    SIGA = [BIG[:, t, :] for t in range(KD)]
    SIGB = [BIG[:, KD + t, :] for t in range(KD)]
    MRG = [BIG[:, 2 * KD + t, :] for t in range(KD)]
    ACTT = [BIG[:, j, :] for j in range(NFT)]
    HTF = HT[:].rearrange("p a t -> p (a t)").bitcast(F32)
    AD, HH, RS = HTF[:, 0:TC], HTF[:, TC:2 * TC], HTF[:, 2 * TC:3 * TC]
    ADR, HHR, RSR = ["HT0", "HT1"], ["HT2", "HT3"], ["HT4", "HT5"]
    ACC = T[3]
    GP = BB[:].rearrange("p a t -> p (a t)")

    PS = [st.enter_context(nc.psum_tensor(f"PS{i}", [128, TC], F32)) for i in range(7)]
    PST = st.enter_context(nc.psum_tensor("PST", [128, TC], BF16))
    PSR = [Reg(f"ps{i}") for i in range(7)]
    bank_rr = [0]

    def bank():
        i = bank_rr[0]
        bank_rr[0] = (i + 1) % 6
        return PS[i], PSR[i]

    def ACT(out, in_, func, R, W, **kw):
        P.op("act", lambda e: e.activation(out=out, in_=in_, func=func, **kw), R, W)

    def TT(eng, out, in0, in1, op, R, W):
        P.op(eng, lambda e: e.tensor_tensor(out=out, in0=in0, in1=in1, op=op), R, W)

    def TS(eng, out, in0, s1, s2, op0, op1, R, W):
        if s2 is None:
            P.op(eng, lambda e: e.tensor_scalar(out=out, in0=in0, scalar1=s1, scalar2=None, op0=op0), R, W)
        else:
            P.op(eng, lambda e: e.tensor_scalar(out=out, in0=in0, scalar1=s1, scalar2=s2, op0=op0, op1=op1), R, W)

    def STT(eng, out, in0, scalar, in1, op0, op1, R, W):
        P.op(eng, lambda e: e.scalar_tensor_tensor(out=out, in0=in0, scalar=scalar, in1=in1, op0=op0, op1=op1), R, W)

    def CP(eng, out, in_, R, W):
        P.op(eng, lambda e: e.tensor_copy(out=out, in_=in_), R, W)

    def RECIP(out, in_, R, W):
        P.op("dve", lambda e: e.reciprocal(out=out, in_=in_), R, W)

    def MM(out, lhsT, rhs, R, W, start=True, stop=True):
        P.op("pe", lambda e: e.matmul(out, lhsT=lhsT, rhs=rhs, start=start, stop=stop), R, W)

    def MMG(out, pairs, R, W):
        pairs = list(pairs)

        def fn(e):
            ins = None
            n = len(pairs)
            for i, (a, b) in enumerate(pairs):
                ins = e.matmul(out, lhsT=a, rhs=b, start=(i == 0), stop=(i == n - 1))
            return ins
        P.op("pe", fn, R, W)

    def dump(name, ap, shape, R):
        if name in dumps:
            t = nc.dram_tensor("dbg_" + name, list(shape), F32 if ap.dtype == F32 else BF16, kind="ExternalOutput").ap()
            dump_out[name] = P.dma("sp", t, ap, R=R, W=["dbg_" + name])

    P.dma("sp", SM[:], small[:, :, :], W=["SM"])
    P.dma("sp", CONS[:], consts[:, :], W=["CONS"])
    P.dma("sp", PADB[:], padb[:, :], W=["PADB"])
    IDF = CONS[:, 0:128]
    TRIU = CONS[:, 128:256]
    ONESF = CONS[:, 256:384]
    IOTA = CONS[:, 384:384 + TC]
    CP("dve", IDB[:], IDF, ["CONS"], ["IDB"])
    CP("dve", ONESB[:], ONESF, ["CONS"], ["ONESB"])
    for l in range(DEPTH):
        o = _off["wgate"][0]
        CP("dve", WGB[:, l, :], SM[:, l, o:o + 64], ["SM"], ["WGB"])
    P.op("pool", lambda e: e.memset(V[:], 1.0), W=["V0", "V1", "V2", "V3"])
    P.op("pool", lambda e: e.memset(CT[:], 0.0), W=["CT"])
    P.op("pool", lambda e: e.memset(S5ST[:], 0.0), W=["S5ST"])
    P.op("pool", lambda e: e.memset(QKPH[:], 0.0), W=["QKPH"])
    P.op("pool", lambda e: e.memset(FH[:], 0.0), W=["FH"])
    P.op("pool", lambda e: e.memset(ONE1[:], 1.0), W=["ONE1"])
    P.op("pool", lambda e: e.memset(LNS[:], LN_SCALE), W=["LNS"])
    P.op("pool", lambda e: e.memset(EPS1[:], EPS), W=["EPS1"])

    def sm(l, name, j=None):
        o, w = _off[name]
        if j is None:
            return SM[:, l, o:o + w]
        return SM[:, l, o + j:o + j + 1]

    PRC = OT[:].rearrange("p a t -> p (a t)")
    PRZ = X[:].rearrange("p a t -> p (a t)")
    PRA, PRF, PRT0, PRT1 = T[0], T[1], T[2], T[3]

    OTR = [f"OT{t}" for t in range(KD)]
    XR_ALL = [f"X{t}" for t in range(KD)]

    def prologue_layer(l):
        s = SC
        lre, lim, ldt = sm(l, "lre"), sm(l, "lim"), sm(l, "ldt")
        dec = S5DEC[:, l, :]
        def exp_taylor(dst, dreg, src, sreg):
            TS("dve", dst, src, 1.0 / 5040, 1.0 / 720, ALU.mult, ALU.add, [sreg], [dreg])
            for cf in (1.0 / 120, 1.0 / 24, 1.0 / 6, 0.5, 1.0, 1.0):
                TT("dve", dst, dst, src, ALU.mult, [sreg], [dreg])
                TS("dve", dst, dst, cf, None, ALU.add, None, [], [dreg])

        def sin_reduced(dst, dreg, ang, areg, shift):
            TS("dve", s["t1"], ang, shift, 1.0 / TWO_PI, ALU.add, ALU.mult, [areg], ["sc_t1"])
            CP("dve", PRK[:, 0:16], s["t1"], ["sc_t1"], ["PRK"])
            CP("dve", s["t1"], PRK[:, 0:16], ["PRK"], ["sc_t1"])
            STT("dve", s["t2"], s["t1"], -CW1, ang, ALU.mult, ALU.add, ["sc_t1", areg], ["sc_t2"])
            STT("dve", s["t2"], s["t1"], -CW2, s["t2"], ALU.mult, ALU.add, ["sc_t1"], ["sc_t2"])
            TS("dve", s["t2"], s["t2"], shift, -math.pi, ALU.add, ALU.max, [], ["sc_t2"])
            TS("dve", s["t2"], s["t2"], math.pi, None, ALU.min, None, [], ["sc_t2"])
            ACT(dst, s["t2"], AF.Sin, ["sc_t2"], [dreg])

        TS("dve", s["a16"], ldt, 1.0 / 16, None, ALU.mult, None, ["SM"], ["sc_a16"])
        exp_taylor(s["dt"], "sc_dt", s["a16"], "sc_a16")
        for _ in range(4):
            TT("dve", s["dt"], s["dt"], s["dt"], ALU.mult, [], ["sc_dt"])
        TT("dve", s["ar"], lre, s["dt"], ALU.mult, ["SM", "sc_dt"], ["sc_ar"])
        TT("dve", s["th"], lim, s["dt"], ALU.mult, ["SM", "sc_dt"], ["sc_th"])
        exp_taylor(dec, "S5DEC", s["ar"], "sc_ar")
        sin_reduced(s["s"], "sc_s", s["th"], "sc_th", 0.0)
        sin_reduced(s["c"], "sc_c", s["th"], "sc_th", math.pi / 2)
        TT("dve", s["abr"], dec, s["c"], ALU.mult, ["S5DEC", "sc_c"], ["sc_abr"])
        TT("dve", s["abi"], dec, s["s"], ALU.mult, ["S5DEC", "sc_s"], ["sc_abi"])
        TS("dve", s["nr"], s["abr"], -1.0, None, ALU.add, None, ["sc_abr"], ["sc_nr"])
        TT("dve", s["t1"], lre, lre, ALU.mult, ["SM"], ["sc_t1"])
        TT("dve", s["t2"], lim, lim, ALU.mult, ["SM"], ["sc_t2"])
        TT("dve", s["den"], s["t1"], s["t2"], ALU.add, ["sc_t1", "sc_t2"], ["sc_den"])
        RECIP(s["den"], s["den"], ["sc_den"], ["sc_den"])
        TT("dve", s["t1"], s["nr"], lre, ALU.mult, ["sc_nr", "SM"], ["sc_t1"])
        TT("dve", s["t2"], s["abi"], lim, ALU.mult, ["sc_abi", "SM"], ["sc_t2"])
        TT("dve", s["t1"], s["t1"], s["t2"], ALU.add, ["sc_t1", "sc_t2"], ["sc_t1"])
        TT("dve", s["zre"], s["t1"], s["den"], ALU.mult, ["sc_t1", "sc_den"], ["sc_zre"])
        TT("dve", s["t1"], s["abi"], lre, ALU.mult, ["sc_abi", "SM"], ["sc_t1"])
        TT("dve", s["t2"], s["nr"], lim, ALU.mult, ["sc_nr", "SM"], ["sc_t2"])
        TT("dve", s["t1"], s["t1"], s["t2"], ALU.subtract, ["sc_t1", "sc_t2"], ["sc_t1"])
        TT("dve", s["zim"], s["t1"], s["den"], ALU.mult, ["sc_t1", "sc_den"], ["sc_zim"])
        TS("dve", s["nzim"], s["zim"], -1.0, None, ALU.mult, None, ["sc_zim"], ["sc_nzim"])
        P.dma("sp", PRC, craw[l].rearrange("p a t -> p (a t)"), W=OTR)
        for k in range(16):
            cre, cim = PRC[:, k * 256:k * 256 + 128], PRC[:, k * 256 + 128:k * 256 + 256]
            o1, o2 = PRZ[:, k * 256:k * 256 + 128], PRZ[:, k * 256 + 128:k * 256 + 256]
            TS("dve", o1, cim, s["nzim"][:, k:k + 1], None, ALU.mult, None, OTR + ["sc_nzim"], XR_ALL)
            STT("dve", o1, cre, s["zre"][:, k:k + 1], o1, ALU.mult, ALU.add, OTR + ["sc_zre"], XR_ALL)
            TS("dve", o2, cim, s["zre"][:, k:k + 1], -1.0, ALU.mult, ALU.mult, OTR + ["sc_zre"], XR_ALL)
            STT("dve", o2, cre, s["nzim"][:, k:k + 1], o2, ALU.mult, ALU.add, OTR + ["sc_nzim"], XR_ALL)
        P.dma("sp", cz[l], PRZ, R=XR_ALL, W=[f"cz{l}"])
        for k in range(16):
            TS("dve", PRC[:, k * 128:(k + 1) * 128], PRZ[:, k * 256:k * 256 + 128], -1.0, None, ALU.mult, None, XR_ALL, OTR)
        P.dma("sp", czn[l], PRC, R=OTR, W=[f"czn{l}"] + OTR)
        for k in range(16):
            th = s["th"][:, k:k + 1]
            TS("dve", PRA[:], IOTA, th, None, ALU.mult, None, ["CONS", "sc_th"], ["T0"])
            tb = TAB[k % 2]
            for half, shift in ((1, 0.0), (0, math.pi / 2)):
                TS("dve", PRF[:], PRA[:], shift, 1.0 / TWO_PI, ALU.add, ALU.mult, ["T0"], ["T1"])
                CP("dve", PRK[:], PRF[:], ["T1"], ["PRK"])
                CP("dve", PRF[:], PRK[:], ["PRK"], ["T1"])
                STT("dve", PRT0[:], PRF[:], -CW1, PRA[:], ALU.mult, ALU.add, ["T1", "T0"], ["T2"])
                STT("dve", PRT0[:], PRF[:], -CW2, PRT0[:], ALU.mult, ALU.add, ["T1", "T2"], ["T2"])
                TS("dve", PRT0[:], PRT0[:], shift, -math.pi, ALU.add, ALU.max, ["T2"], ["T2"])
                TS("dve", PRT0[:], PRT0[:], math.pi, None, ALU.min, None, ["T2"], ["T2"])
                ACT(tb[:, half * TC:(half + 1) * TC], PRT0[:], AF.Sin, ["T2"], [f"TAB{k % 2}"])
            c1, s1 = s["c"][:, k:k + 1], s["s"][:, k:k + 1]
            c511, s511 = tb[:, TC - 1:TC], tb[:, 2 * TC - 1:2 * TC]
            tr = f"TAB{k % 2}"
            TT("dve", s["c2"][:, k:k + 1], c511, c1, ALU.mult, [tr, "sc_c"], ["sc_c2"])
            TT("dve", s["s2"][:, k:k + 1], s511, s1, ALU.mult, [tr, "sc_s"], ["sc_s2"])
            TT("dve", S5ROT[:, l, 0, k:k + 1], s["c2"][:, k:k + 1], s["s2"][:, k:k + 1], ALU.subtract, ["sc_c2", "sc_s2"], ["S5ROT"])
            TT("dve", s["c2"][:, k:k + 1], s511, c1, ALU.mult, [tr, "sc_c"], ["sc_c2"])
            TT("dve", s["s2"][:, k:k + 1], c511, s1, ALU.mult, [tr, "sc_s"], ["sc_s2"])
            TT("dve", S5ROT[:, l, 1, k:k + 1], s["c2"][:, k:k + 1], s["s2"][:, k:k + 1], ALU.add, ["sc_c2", "sc_s2"], ["S5ROT"])
            P.dma("sp", tabs[l, k], tb[:], R=[tr], W=[f"tab{l}_{k}", tr])

    for l in range(DEPTH):
        prologue_layer(l)

    border = list(range(11)) + [36] + list(range(11, NBLK))
    wq = [(l, b) for c in range(nch_run) for l in range(DEPTH) for b in border]
    wstate = {"next": 0, "use": 0}
    WBR = [Reg(f"wb{i}") for i in range(NBUF)]

    def w_issue():
        i = wstate["next"]
        l, b = wq[i]
        buf = i % NBUF
        if b == 10:
            P.dma("pool", WB[buf][:], cz[l], R=[f"cz{l}"], W=[WBR[buf]], sem_idx=8 + buf)
        elif b == 36:
            P.dma("pool", WB[buf][:], czn[l], R=[f"czn{l}"], W=[WBR[buf]], sem_idx=8 + buf)
        else:
            P.dma("pool", WB[buf][:], wst[l, b], W=[WBR[buf]], sem_idx=8 + buf)
        wstate["next"] += 1

    def w_get(l, b, extra_hold=0):
        i = wstate["use"]
        assert wq[i] == (l, b), (wq[i], l, b)
        wstate["use"] += 1
        while wstate["next"] < min(len(wq), i + NBUF - 1 - extra_hold):
            w_issue()
        return WB[i % NBUF], WBR[i % NBUF]

    HTR = [f"HT{kt}" for kt in range(KD)]

    def rmsnorm_stats(src_tile, src_reg):
        ps, pr = bank()
        for kt in range(KD):
            ACT(SQ[:, kt % 2, :], src_tile(kt), AF.Square, [src_reg(kt)], [f"SQ{kt % 2}"])
            MM(ps[:, :], ONESB[:], SQ[:, kt % 2, :], ["ONESB", f"SQ{kt % 2}"], [pr], start=(kt == 0), stop=(kt == KD - 1))
        ACT(RSTD[:], ps[:, :], AF.Ln, [pr, "EPS1"], ["RSTD"], scale=1.0 / D, bias=EPS1[:, 0:1])
        ACT(RSTD[:], RSTD[:], AF.Exp, [], ["RSTD"], scale=-0.5)

    def pre_norm(l, gname):
        rmsnorm_stats(lambda kt: X[:, kt, :], lambda kt: f"X{kt}")
        for kt in range(KD):
            STT("dve", HT[:, kt, :], X[:, kt, :], sm(l, gname, kt), RSTD[:], ALU.mult, ALU.mult, [f"X{kt}", "RSTD", "SM"], [f"HT{kt}"])

    def stats_tile(t, ps, pr):
        ACT(SQ[:, t % 2, :], ps[:, :], AF.Square, [pr], [f"SQ{t % 2}"])
        MM(PS[6][:, :], ONESB[:], SQ[:, t % 2, :], ["ONESB", f"SQ{t % 2}"], [PSR[6]], start=(t == 0), stop=(t == KD - 1))

    def post_norm_residual(l, gname):
        rmsnorm_stats(lambda kt: OT[:, kt, :], lambda kt: f"OT{kt}")
        for kt in range(KD):
            STT("dve", OT[:, kt, :], OT[:, kt, :], sm(l, gname, kt), RSTD[:], ALU.mult, ALU.mult, ["RSTD", "SM"], [f"OT{kt}"])
            TT("pool", X[:, kt, :], X[:, kt, :], OT[:, kt, :], ALU.add, [f"OT{kt}"], [f"X{kt}"])

    def proj_tile(wb, wr, mm, ncols=512):
        ps, pr = bank()
        MMG(ps[:, :], [(wb[:, kc * ncols + mm * 128: kc * ncols + (mm + 1) * 128], HT[:, kc, :]) for kc in range(KD)], [wr] + HTR, [pr])
        return ps, pr

    def refresh_state(l):
        ACT(CTB[:], CT[:, l, :, 0:128], AF.Copy, ["CT"], ["CTB"])
        for h in range(4):
            TS("pool", NB[:, h, :], ONESB[:], CT[:, l, h, 128:129], None, ALU.mult, None, ["ONESB", "CT"], ["NB"])

    def mixer(c, l):
        pre_norm(l, "g_pre")
        if c == dump_c and l == 0:
            dump("ht", HT[:], [128, KD, TC], HTR)
        wb, wr = w_get(l, 0)
        for mm in range(4):
            ps, pr = proj_tile(wb, wr, mm)
            ACT(UT[:, mm, :], ps[:, :], AF.Copy, [pr], [f"UT{mm}"])
        for blk, base in ((1, 0), (2, 4)):
            wb, wr = w_get(l, blk)
            for mm in range(4):
                t = base + mm
                pb = t % 2
                ps, pr = proj_tile(wb, wr, mm)
                rq = f"QKP1_{pb}"
                ACT(QKP1[:, pb, 3:3 + TC], ps[:, :], AF.Copy, [pr], [rq])
                CP("pool", QKP1[:, pb, 0:3], QKPH[:, l, t, :], ["QKPH"], [rq])
                o = _off["wqk"][0] + t * 4
                ACT(QKA[:, pb, :], QKP1[:, pb, 0:TC], AF.Identity, [rq, "SM"], [f"QKA{pb}"], scale=SM[:, l, o:o + 1], bias=sm(l, "bqk", t))
                for j in (1, 2, 3):
                    STT("dve", QKA[:, pb, :], QKP1[:, pb, j:j + TC], SM[:, l, o + j:o + j + 1], QKA[:, pb, :], ALU.mult, ALU.add, [rq, "SM"], [f"QKA{pb}"])
                ACT(QKT[:, t, :], QKA[:, pb, :], AF.Silu, [f"QKA{pb}"], [f"QKT{t}"])
                CP("pool", QKPH[:, l, t, :], QKP1[:, pb, TC:TC + 3], [rq], ["QKPH"])
        wb, wr = w_get(l, 3)
        for sc in range(NSUB):
            ps, pr = bank()
            MMG(ps[:, :], [(HT[:, kc, sc * 128:(sc + 1) * 128], wb[:, kc * 512:(kc + 1) * 512]) for kc in range(KD)], [wr] + HTR, [pr])
            for h in range(4):
                ACT(V[:, sc, h, 0:128], ps[:, h * 128:(h + 1) * 128], AF.Copy, [pr], [f"V{sc}"])
        for sc in range(NSUB):
            ps, pr = bank()
            MMG(ps[:, 0:8], [(HT[:, kc, sc * 128:(sc + 1) * 128], WGB[:, l, kc * 8:kc * 8 + 8]) for kc in range(KD)], ["WGB"] + HTR, [pr])
            TT("dve", G[:, sc, :], ps[:, 0:8], sm(l, "bgate"), ALU.add, [pr, "SM"], [f"G{sc}"])
        wb, wr = w_get(l, 4)
        for mm in range(4):
            ps, pr = proj_tile(wb, wr, mm)
            ACT(SIGO[:, mm, :], ps[:, :], AF.Sigmoid, [pr], [f"SIGO{mm}"])
        for dst, nm, blks in ((SIGA, "SIGA", (5, 6)), (SIGB, "SIGB", (7, 8))):
            for bi, blk in enumerate(blks):
                wb, wr = w_get(l, blk)
                for mm in range(4):
                    t = bi * 4 + mm
                    ps, pr = proj_tile(wb, wr, mm)
                    ACT(dst[t], ps[:, :], AF.Sigmoid, [pr], [f"{nm}{t}"])
        if c == dump_c and l == 0:
            dump("ut", UT[:], [128, 4, TC], [f"UT{m}" for m in range(4)])
            dump("qkt", QKT[:], [128, 8, TC], [f"QKT{m}" for m in range(8)])
        wbB, wrB = w_get(l, 9)
        wbC, wrC = w_get(l, 10)
        wbN, wrN = w_get(l, 36, extra_hold=1)
        ys_ps, ys_pr = PS[6], PSR[6]

        def s5_front(k):
            q4 = k // 4
            tb, tbr = TAB[k % 2], f"TAB{k % 2}"
            P.dma("sp", tb[:], tabs[l, k], R=[f"tab{l}_{k}"], W=[tbr])
            cs, sn = tb[:, 0:TC], tb[:, TC:2 * TC]
            psr_, prr = bank()
            psi_, pri = bank()
            MM(psr_[:, :], wbB[:, k * 256:k * 256 + 128], UT[:, q4, :], [wrB, f"UT{q4}"], [prr])
            MM(psi_[:, :], wbB[:, k * 256 + 128:k * 256 + 256], UT[:, q4, :], [wrB, f"UT{q4}"], [pri])
            TA, TAR = HTF[:, 3 * TC:4 * TC], ["HT6", "HT7"]
            TT("dve", T[0][:], psr_[:, :], cs, ALU.mult, [prr, tbr], ["T0"])
            TT("dve", T[1][:], psi_[:, :], sn, ALU.mult, [pri, tbr], ["T1"])
            TT("dve", TA, psi_[:, :], cs, ALU.mult, [pri, tbr], TAR)
            TT("dve", GE[:], psr_[:, :], sn, ALU.mult, [prr, tbr], ["GE"])
            TT("dve", BB[:, 0, :], T[0][:], T[1][:], ALU.add, [], ["BB0", "T0", "T1"])
            TT("dve", BB[:, 1, :], TA, GE[:], ALU.subtract, [], ["BB1", "GE"] + TAR)
            ssb = SS if k % 2 == 0 else QKA
            ssr = ("SS0", "SS1") if k % 2 == 0 else ("QKA0", "QKA1")
            for ri in range(2):
                dec_b = S5DEC[:, l, k:k + 1].to_broadcast([128, TC])
                P.op("dve", (lambda e, ri=ri, dec_b=dec_b, init=S5ST[:, l, ri, k:k + 1], o=ssb[:, ri, :]:
                             e.tensor_tensor_scan(out=o, data0=dec_b, data1=BB[:, ri, :], initial=init, op0=ALU.mult, op1=ALU.add)),
                     ["S5DEC", "S5ST", f"BB{ri}"], [ssr[ri]])
                ACT(S5E[:, ri, k:k + 1], ssb[:, ri, TC - 1:TC], AF.Copy, [ssr[ri]], ["S5E"])

        def s5_back(k):
            q4 = k // 4
            tb, tbr = TAB[k % 2], f"TAB{k % 2}"
            cs, sn = tb[:, 0:TC], tb[:, TC:2 * TC]
            ssb = SS if k % 2 == 0 else QKA
            ssr = ("SS0", "SS1") if k % 2 == 0 else ("QKA0", "QKA1")
            xr, xrr = XR[k % 2], f"XR{k % 2}"
            xq = T[2 + k % 2][:].bitcast(BF16).rearrange("p (a t) -> p a t", a=2)
            xqr = f"T{2 + k % 2}"
            TT("pool", xr[:, 0, :], ssb[:, 0, :], cs, ALU.mult, [ssr[0], tbr], [xrr + "a"])
            TT("pool", xr[:, 1, :], ssb[:, 1, :], sn, ALU.mult, [ssr[1], tbr], [xrr + "b"])
            link = [xqr] if k < 2 else []
            TT("pool", xq[:, 0, :], ssb[:, 0, :], sn, ALU.mult, [ssr[0], tbr], [xqr + "a"] + link)
            TT("pool", xq[:, 1, :], ssb[:, 1, :], cs, ALU.mult, [ssr[1], tbr], [xqr + "b"] + link)
            cre = wbC[:, k * 256:k * 256 + 128]
            cimn = wbC[:, k * 256 + 128:k * 256 + 256]
            cren = wbN[:, k * 128:(k + 1) * 128]
            MM(ys_ps[:, :], cre, xr[:, 0, :], [wrC, xrr + "a"], [ys_pr], start=(k % 4 == 0), stop=False)
            MM(ys_ps[:, :], cren, xr[:, 1, :], [wrN, xrr + "b"], [ys_pr], start=False, stop=False)
            MM(ys_ps[:, :], cimn, xq[:, 0, :], [wrC, xqr + "a"], [ys_pr], start=False, stop=False)
            MM(ys_ps[:, :], cimn, xq[:, 1, :], [wrC, xqr + "b"], [ys_pr], start=False, stop=(k % 4 == 3))
            if k % 4 == 3:
                STT("dve", YG[:, q4, :], UT[:, q4, :], sm(l, "dvec", q4), ys_ps[:, :], ALU.mult, ALU.add, [f"UT{q4}", "SM", ys_pr], [f"YG{q4}"])

        def mlstm_gen():
            refresh_state(l)
            yield
            for sc in range(NSUB):
                tsl = slice(sc * 128, (sc + 1) * 128)
                gcol = c * NSUB + sc
                ACT(SP_[:], G[:, sc, 4:8], AF.Exp, [f"G{sc}"], ["SP_"], scale=-1.0)
                ACT(SP_[:], SP_[:], AF.Ln, ["ONE1"], ["SP_"], bias=ONE1[:, 0:1])
                TS("dve", LI[:], G[:, sc, 0:4], PADB[:, gcol:gcol + 1], None, ALU.add, None, [f"G{sc}", "PADB"], ["LI"])
                yield
                for h in range(4):
                    TS("dve", SPB[:, h, :], ONESF, SP_[:, h:h + 1], None, ALU.mult, None, ["CONS", "SP_"], ["SPB"])
                psF, prF = bank()
                MM(psF[:, 0:4], TRIU, SP_[:], ["CONS", "SP_"], [prF])
                MM(psF[:, 4:8], ONESF, SP_[:], ["CONS", "SP_"], [prF])
                yield
                psFr, prFr = bank()
                for h in range(4):
                    MM(psFr[:, h * 128:(h + 1) * 128], SPB[:, h, :], TRIU, ["SPB", "CONS"], [prFr])
                TT("dve", BD[:], psF[:, 0:4], LI[:], ALU.add, [prF, "LI"], ["BD"])
                TT("dve", TW[:], BD[:], psF[:, 4:8], ALU.subtract, [prF, "BD"], ["TW"])
                ACT(WEX[:], TW[:], AF.Exp, ["TW"], ["WEX"])
                ACT(DEC[:], psF[:, 4:8], AF.Exp, [prF], ["DEC"], scale=-1.0)
                TS("dve", BD[:], BD[:], LN_SCALE, None, ALU.add, None, ["TW"], ["BD"])
                yield
                for h in range(4):
                    hs = slice(h * 128, (h + 1) * 128)
                    ACT(DT_[:, hs], psFr[:, hs], AF.Exp, [prFr, "BD"], ["DT_"], scale=-1.0, bias=BD[:, h:h + 1])
                    TT("pool", DTM[:, hs], DT_[:, hs], TRIU, ALU.mult, ["DT_", "CONS"], ["DTM"])
                ACT(EF[:], psFr[:, :], AF.Exp, [prFr, "LNS"], ["EF"], scale=-1.0, bias=LNS[:, 0:1])
                psS, prS = bank()
                for h in range(4):
                    MM(psS[:, h * 128:(h + 1) * 128], QKT[:, 4 + h, tsl], QKT[:, h, tsl], [f"QKT{4 + h}", f"QKT{h}"], [prS])
                for h in range(4):
                    P.op("pe", (lambda e, o=PST[:, h * 128:(h + 1) * 128], i=QKT[:, 4 + h, tsl]: e.transpose(o, i, IDB[:])),
                         [f"QKT{4 + h}", "IDB"], ["PST"])
                yield
                TT("dve", QP[:].rearrange("p (h t) -> p h t", h=4), QKT[:, 0:4, tsl], EF[:].rearrange("p (h t) -> p h t", h=4), ALU.mult,
                   [f"QKT{h}" for h in range(4)] + ["EF"], ["QP"])
                TT("dve", WT[:], psS[:, :], DTM[:], ALU.mult, [prS, "DTM"], ["WT"])
                for h in range(4):
                    hs = slice(h * 128, (h + 1) * 128)
                    TS("dve", KW[:, hs], PST[:, hs], WEX[:, h:h + 1], None, ALU.mult, None, ["PST", "WEX"], ["KW"])
                yield
                psN, prN = bank()
                psD, prD = bank()
                for h in range(4):
                    hs = slice(h * 128, (h + 1) * 128)
                    MM(psN[:, hs], V[:, sc, h, 0:128], WT[:, hs], [f"V{sc}", "WT"], [prN], start=True, stop=False)
                    MM(psN[:, hs], CTB[:, h, :], QP[:, hs], ["CTB", "QP"], [prN], start=False, stop=True)
                    MM(psD[:, hs], ONESB[:], WT[:, hs], ["ONESB", "WT"], [prD], start=True, stop=False)
                    MM(psD[:, hs], NB[:, h, :], QP[:, hs], ["NB", "QP"], [prD], start=False, stop=True)
                psU = []
                for half in range(2):
                    pu, pru = bank()
                    psU.append((pu, pru))
                    for hh in range(2):
                        h = half * 2 + hh
                        MM(pu[:, hh * 129:(hh + 1) * 129], KW[:, h * 128:(h + 1) * 128], V[:, sc, h, :], ["KW", f"V{sc}"], [pru])
                yield
                TS("dve", AD, psD[:, :], -1.0, 1.0, ALU.mult, ALU.max, [prD], ADR)
                TT("dve", AD, AD, psD[:, :], ALU.max, [prD], ADR)
                RECIP(AD, AD, [], ADR)
                TT("dve", HH, psN[:, :], AD, ALU.mult, [prN] + ADR, HHR)
                for half in range(2):
                    pu, pru = psU[half]
                    for hh in range(2):
                        h = half * 2 + hh
                        STT("dve", CT[:, l, h, :], CT[:, l, h, :], DEC[:, h:h + 1], pu[:, hh * 129:(hh + 1) * 129], ALU.mult, ALU.add, [pru, "DEC"], ["CT"])
                yield
                ACT(SQH[:], HH, AF.Square, HHR, ["SQH"])
                if sc < NSUB - 1:
                    refresh_state(l)
                psH, prH = bank()
                for h in range(4):
                    hs = slice(h * 128, (h + 1) * 128)
                    MM(psH[:, hs], ONESB[:], SQH[:, hs], ["ONESB", "SQH"], [prH])
                yield
                ACT(RS, psH[:, :], AF.Ln, [prH, "EPS1"], RSR, scale=1.0 / 128, bias=EPS1[:, 0:1])
                ACT(RS, RS, AF.Exp, [], RSR, scale=-0.5)
                yield
                for h in range(4):
                    hs = slice(h * 128, (h + 1) * 128)
                    STT("dve", HH[:, hs], HH[:, hs], sm(l, "ghead", h), RS[:, hs], ALU.mult, ALU.mult, RSR + ["SM"], HHR)
                TT("pool", YB[:, :, tsl], HH.rearrange("p (h t) -> p h t", h=4), SIGO[:, :, tsl], ALU.mult,
                   HHR + [f"SIGO{h}" for h in range(4)], [f"YB{h}" for h in range(4)])
                yield

        mg = mlstm_gen()

        def pull(n):
            for _ in range(n):
                try:
                    next(mg)
                except StopIteration:
                    return
        s5_front(0)
        pull(3)
        for k in range(1, 16):
            s5_front(k)
            s5_back(k - 1)
            pull(3)
        s5_back(15)
        for _ in mg:
            pass
        c5, s5 = S5ROT[:, l, 0, :], S5ROT[:, l, 1, :]
        TT("dve", SC["t1"], S5E[:, 0, :], c5, ALU.mult, ["S5E", "S5ROT"], ["sc_t1"])
        TT("dve", SC["t2"], S5E[:, 1, :], s5, ALU.mult, ["S5E", "S5ROT"], ["sc_t2"])
        TT("dve", S5ST[:, l, 0, :], SC["t1"], SC["t2"], ALU.subtract, ["sc_t1", "sc_t2"], ["S5ST"])
        TT("dve", SC["t1"], S5E[:, 0, :], s5, ALU.mult, ["S5E", "S5ROT"], ["sc_t1"])
        TT("dve", SC["t2"], S5E[:, 1, :], c5, ALU.mult, ["S5E", "S5ROT"], ["sc_t2"])
        TT("dve", S5ST[:, l, 1, :], SC["t1"], SC["t2"], ALU.add, ["sc_t1", "sc_t2"], ["S5ST"])
        for q4 in range(4):
            ACT(YG[:, q4, :], YG[:, q4, :], AF.Gelu_apprx_tanh, [], [f"YG{q4}"])
        wb, wr = w_get(l, 11)
        YGR = [f"YG{q}" for q in range(4)]
        for mm in range(4):
            ps, pr = bank()
            MMG(ps[:, :], [(wb[:, kc * 512 + mm * 128:kc * 512 + (mm + 1) * 128], YG[:, kc, :]) for kc in range(4)], [wr] + YGR, [pr])
            ACT(GE[:], ps[:, :], AF.Sigmoid, [pr], ["GE"])
            TT("dve", YA[:, mm, :], YG[:, mm, :], GE[:], ALU.mult, ["GE", f"YG{mm}"], [f"YA{mm}"])
        if c == dump_c and l == 0:
            dump("ya", YA[:], [128, 4, TC], [f"YA{m}" for m in range(4)])
        if c == dump_c and l == 0:
            dump("yb", YB[:], [128, 4, TC], [f"YB{m}" for m in range(4)])
        wbA, wrA = w_get(l, 12)
        wbBm, wrBm = w_get(l, 13)
        YAR = [f"YA{q}" for q in range(4)]
        YBR = [f"YB{q}" for q in range(4)]
        for mm in range(KD):
            psa, pra = bank()
            psb, prb = bank()
            MMG(psa[:, :], [(wbA[:, kc * 1024 + mm * 128:kc * 1024 + (mm + 1) * 128], YA[:, kc, :]) for kc in range(4)], [wrA] + YAR, [pra])
            MMG(psb[:, :], [(wbBm[:, kc * 1024 + mm * 128:kc * 1024 + (mm + 1) * 128], YB[:, kc, :]) for kc in range(4)], [wrBm] + YBR, [prb])
            TT("dve", T[0][:], psa[:, :], SIGA[mm], ALU.mult, [pra, f"SIGA{mm}"], ["T0"])
            TT("dve", T[1][:], psb[:, :], SIGB[mm], ALU.mult, [prb, f"SIGB{mm}"], ["T1"])
            TT("pool", MRG[mm], T[0][:], T[1][:], ALU.add, ["T0", "T1"], [f"MRG{mm}"])
        MR = [f"MRG{q}" for q in range(KD)]
        if c == dump_c and l == 0:
            dump("mrg", BIG[:, 2 * KD:3 * KD, :], [128, KD, TC], MR)
        for bi in range(2):
            wb, wr = w_get(l, 14 + bi)
            for mm in range(4):
                t = bi * 4 + mm
                ps, pr = bank()
                MMG(ps[:, :], [(wb[:, kc * 512 + mm * 128:kc * 512 + (mm + 1) * 128], MRG[kc]) for kc in range(KD)], [wr] + MR, [pr])
                ACT(OT[:, t, :], ps[:, :], AF.Copy, [pr], [f"OT{t}"])
        if c == dump_c and l == 0:
            dump("ot", OT[:], [128, KD, TC], [f"OT{t}" for t in range(KD)])
        post_norm_residual(l, "g_post")
        if c == dump_c and l == 0:
            dump("xmid", X[:], [128, KD, TC], XR_ALL)

    def ffn(c, l):
        pre_norm(l, "gf_pre")
        alias = [f"SIGA{t}" for t in range(KD)] + [f"SIGB{t}" for t in range(KD)] + [f"MRG{t}" for t in range(KD)]
        for bi in range(6):
            wbg, wrg = w_get(l, 16 + 2 * bi)
            wbu, wru = w_get(l, 17 + 2 * bi)
            ncols = 512 if bi < 5 else 256
            for mm in range(ncols // 128):
                j = bi * 4 + mm
                psg, prg = proj_tile(wbg, wrg, mm, ncols)
                psu, pru = proj_tile(wbu, wru, mm, ncols)
                o = _off["wfc"][0] + j * 3
                ACT(GP[:, 2:2 + TC], psg[:, :], AF.Copy, [prg], ["BB0", "BB1"])
                CP("pool", GP[:, 0:2], FH[:, l, j, :], ["FH"], ["BB0", "BB1"])
                ACT(ACC[:], GP[:, 0:TC], AF.Identity, ["BB0", "BB1", "SM"], ["T3"] + (["T3a", "T3b"] if j == 0 else []),
                    scale=SM[:, l, o:o + 1], bias=sm(l, "bfc", j))
                STT("dve", ACC[:], GP[:, 1:1 + TC], SM[:, l, o + 1:o + 2], ACC[:], ALU.mult, ALU.add, ["BB0", "BB1", "SM"], ["T3"])
                STT("dve", ACC[:], GP[:, 2:2 + TC], SM[:, l, o + 2:o + 3], ACC[:], ALU.mult, ALU.add, ["BB0", "BB1", "SM"], ["T3"])
                CP("pool", FH[:, l, j, :], GP[:, TC:TC + 2], ["BB0", "BB1"], ["FH"])
                ACT(GE[:], ACC[:], AF.Gelu_apprx_tanh, ["T3"], ["GE"])
                TT("dve", ACTT[j], psu[:, :], GE[:], ALU.mult, [pru, "GE"], [f"ACTT{j}"] + alias)
        AR = [f"ACTT{j}" for j in range(NFT)]
        for mm in range(KD):
            wb, wr = w_get(l, 28 + mm)
            ps, pr = bank()
            MMG(ps[:, :], [(wb[:, j * 128:(j + 1) * 128], ACTT[j]) for j in range(NFT)], [wr] + AR, [pr])
            ACT(OT[:, mm, :], ps[:, :], AF.Copy, [pr], [f"OT{mm}"])
        post_norm_residual(l, "gf_post")
        P.op("act", lambda e: e.activation(out=GE[:, 0:1], in_=GE[:, 0:1], func=AF.Copy), [], AR + alias + ["GE"])

    for c in range(nch_run):
        P.dma("sp", X[:], xT[c], W=XR_ALL)
        for l in range(DEPTH):
            mixer(c, l)
            ffn(c, l)
        P.dma("sp", yT[c], X[:], R=XR_ALL, W=[f"yT{c}"])
    evs = [P.R(f"yT{c}").w for c in range(nch_run)] + list(dump_out.values())
    P.final_wait("sp", evs)
    P.emit()
    st.close()
    return nc


def _kblock(W, n):
    K = W.shape[0]
    kc = K // 128
    a = W.reshape(kc, 128, n).transpose(1, 0, 2).reshape(128, kc * n)
    out = np.zeros((128, 4096), np.float32)
    out[:, :kc * n] = a
    return out


def pack_weights(inp):
    wst = np.zeros((DEPTH, NBLK, 128, 4096), np.float32)
    small = np.zeros((128, DEPTH, NSMALL), np.float32)
    craw = np.zeros((DEPTH, 128, 16, 256), np.float32)

    def put(l, name, arr):
        o, w = _off[name]
        small[:, l, o:o + w] = arr

    for l in range(DEPTH):
        w_in = np.asarray(inp["w_in"][l], np.float32)
        for b, c0 in enumerate((0, 512, 1024, 1536, 2048)):
            wst[l, b] = _kblock(w_in[:, c0:c0 + 512], 512)
        for b, c0 in ((5, 2568), (6, 3080), (7, 3592), (8, 4104)):
            wst[l, b] = _kblock(w_in[:, c0:c0 + 512], 512)
        bre = np.asarray(inp["ssm_b_re"][l], np.float32)
        bim = np.asarray(inp["ssm_b_im"][l], np.float32)
        cre = np.asarray(inp["ssm_c_re"][l], np.float32)
        cim = np.asarray(inp["ssm_c_im"][l], np.float32)
        blkB = np.zeros((128, 16, 256), np.float32)
        for k in range(16):
            for gi in range(2):
                g = 2 * k + gi
                gg = g % 8
                blkB[gg * 16:(gg + 1) * 16, k, gi * 64:(gi + 1) * 64] = bre[g].T
                blkB[gg * 16:(gg + 1) * 16, k, 128 + gi * 64:128 + (gi + 1) * 64] = bim[g].T
                craw[l, gi * 64:(gi + 1) * 64, k, gg * 16:(gg + 1) * 16] = cre[g].T
                craw[l, gi * 64:(gi + 1) * 64, k, 128 + gg * 16:128 + (gg + 1) * 16] = cim[g].T
        wst[l, 9] = blkB.reshape(128, 4096)
        wst[l, 11] = _kblock(np.asarray(inp["w_ssm_glu"][l], np.float32), 512)
        wst[l, 12] = _kblock(np.asarray(inp["w_branch_ssm"][l], np.float32), 1024)
        wst[l, 13] = _kblock(np.asarray(inp["w_branch_mlstm"][l], np.float32), 1024)
        w_out = np.asarray(inp["w_out"][l], np.float32)
        wst[l, 14] = _kblock(w_out[:, 0:512], 512)
        wst[l, 15] = _kblock(w_out[:, 512:1024], 512)
        wg = np.asarray(inp["w_ffn_gate"][l], np.float32)
        wu = np.asarray(inp["w_ffn_up"][l], np.float32)
        for bi in range(6):
            n = 512 if bi < 5 else 256
            wst[l, 16 + 2 * bi] = _kblock(wg[:, bi * 512:bi * 512 + n], n)
            wst[l, 17 + 2 * bi] = _kblock(wu[:, bi * 512:bi * 512 + n], n)
        wd = np.asarray(inp["w_ffn_down"][l], np.float32)
        for m in range(8):
            wst[l, 28 + m] = _kblock(wd[:, m * 128:(m + 1) * 128], 128)
        put(l, "g_pre", np.asarray(inp["g_mix_pre"][l]).reshape(8, 128).T)
        put(l, "g_post", np.asarray(inp["g_mix_post"][l]).reshape(8, 128).T)
        put(l, "gf_pre", np.asarray(inp["g_ffn_pre"][l]).reshape(8, 128).T)
        put(l, "gf_post", np.asarray(inp["g_ffn_post"][l]).reshape(8, 128).T)
        put(l, "wgate", w_in[:, 2560:2568].reshape(8, 128, 8).transpose(1, 0, 2).reshape(128, 64))
        put(l, "bgate", np.broadcast_to(np.asarray(inp["b_gates"][l], np.float32)[None, :], (128, 8)))
        wqk = np.asarray(inp["w_qk_conv"][l], np.float32)
        put(l, "wqk", wqk.reshape(4, 8, 128).transpose(2, 1, 0).reshape(128, 32))
        put(l, "bqk", np.asarray(inp["b_qk_conv"][l]).reshape(8, 128).T)
        put(l, "ghead", np.asarray(inp["g_head_norm"][l]).reshape(4, 128).T)
        wfc = np.asarray(inp["w_ffn_conv"][l], np.float32)
        put(l, "wfc", wfc.reshape(3, 22, 128).transpose(2, 1, 0).reshape(128, 66))
        put(l, "bfc", np.asarray(inp["b_ffn_conv"][l]).reshape(22, 128).T)
        lre = np.asarray(inp["ssm_lambda_re"][l], np.float32)
        lim = np.asarray(inp["ssm_lambda_im"][l], np.float32)
        ldt = np.asarray(inp["ssm_log_dt"][l], np.float32)
        put(l, "lre", lre.reshape(16, 128).T)
        put(l, "lim", lim.reshape(16, 128).T)
        put(l, "ldt", np.repeat(ldt, 64).reshape(16, 128).T)
        put(l, "dvec", np.asarray(inp["ssm_d"][l], np.float32).reshape(4, 128).T)
    return wst, small, craw


def make_consts():
    cst = np.zeros((128, 128 * 3 + TC), np.float32)
    cst[:, 0:128] = np.eye(128, dtype=np.float32)
    cst[:, 128:256] = np.triu(np.ones((128, 128), np.float32))
    cst[:, 256:384] = 1.0
    cst[:, 384:] = np.arange(TC, dtype=np.float32)[None, :]
    return cst


def pack_tokens(x, meta, nch):
    seq = np.zeros((NCH * TC, D), np.float32)
    seq[PADF:PADF + NMETA] = meta
    seq[PADF + NMETA:] = x
    seq = seq[:nch * TC]
    xT = np.ascontiguousarray(seq.reshape(nch, TC, KD, 128).transpose(0, 3, 2, 1))
    tok = np.arange(nch * TC).reshape(nch * NSUB, 128).T
    padb = np.where(tok < PADF, np.float32(-30000.0), np.float32(0.0)).astype(np.float32)
    return xT, np.ascontiguousarray(padb)


_CACHE = {}


def run(inputs, nch, dumps=()):
    key = (nch, tuple(dumps))
    if key not in _CACHE:
        _CACHE[key] = build_program(nch, dumps)
    nc = _CACHE[key]
    wst, small, craw = pack_weights(inputs)
    xT, padb = pack_tokens(np.asarray(inputs["x"], np.float32)[0], np.asarray(inputs["meta_tokens"], np.float32), nch)
    in_map = {"xT": xT, "wst": wst, "small": small, "craw": craw, "consts": make_consts(), "padb": padb}
    res = run_bass_kernel_spmd(nc, [in_map], core_ids=[0])
    return res.results[0]


def kernel(**inputs):
    r = run(inputs, NCH)
    yT = r["yT"]
    seq = yT.transpose(0, 3, 2, 1).reshape(NCH * TC, D)
    out = seq[PADF + NMETA:].reshape(1, SEQ, D)
    return np.ascontiguousarray(out.astype(np.float32))
```

```python
import math
from contextlib import ExitStack
import numpy as np
import concourse.bass as bass
import concourse.mybir as mybir
from concourse.bass_utils import run_bass_kernel_spmd

F32 = mybir.dt.float32
BF16 = mybir.dt.bfloat16
I32 = mybir.dt.int32
AF = mybir.ActivationFunctionType
ALU = mybir.AluOpType

D = 1024
KD = 8
TC = 512
NSUB = 4
DEPTH = 4
NMETA = 16
SEQ = 16384
NCH = 33
PADF = NCH * TC - SEQ - NMETA
FFN = 2816
NFT = 22
NBLK = 36
NBUF = 4
EPS = 1e-6
LN_SCALE = math.log(128.0 ** -0.5)
TWO_PI = 2.0 * math.pi
CW1 = 6.28125
CW2 = TWO_PI - CW1
SEM_LIMIT = 30000
NO_SELF_WAIT = ("pe",)

_off = {}
_o = 0
for _n, _w in [("g_pre", 8), ("g_post", 8), ("gf_pre", 8), ("gf_post", 8), ("wgate", 64), ("bgate", 8),
               ("wqk", 32), ("bqk", 8), ("ghead", 4), ("wfc", 66), ("bfc", 22), ("lre", 16), ("lim", 16),
               ("ldt", 16), ("dvec", 4)]:
    _off[_n] = (_o, _w)
    _o += _w
NSMALL = _o


class Reg:
    __slots__ = ("w", "r", "name")

    def __init__(self, name=""):
        self.w = None
        self.r = []
        self.name = name


class Prog:
    ENGS = ("pe", "act", "dve", "pool", "sp")

    def __init__(self, nc, stack):
        self.nc = nc
        self.stack = stack
        self.q = {e: [] for e in self.ENGS}
        self.cnt = {e: 0 for e in self.ENGS}
        self.sems = {e: [stack.enter_context(nc.semaphore(f"s_{e}_0"))] for e in self.ENGS}
        self.waited = {e: {} for e in self.ENGS}
        self.dma_sems = [stack.enter_context(nc.semaphore(f"s_dma_{i}")) for i in range(8 + NBUF)]
        self.dma_cnt = [0] * (8 + NBUF)
        self.dma_rr = 0
        self.regs = {}
        self.nops = 0
        self.no_self_wait = set(NO_SELF_WAIT)
        self.own_sem_ids = {e: {id(self.sems[e][0])} for e in self.ENGS}

    def R(self, x):
        if isinstance(x, Reg):
            return x
        if x not in self.regs:
            self.regs[x] = Reg(x)
        return self.regs[x]

    def _collect(self, eng, R, W, extra=()):
        evs = list(extra)
        for r in R:
            if r.w is not None:
                evs.append(r.w)
        for w in W:
            if w.w is not None:
                evs.append(w.w)
            evs.extend(w.r)
        need = {}
        own = self.own_sem_ids[eng] if eng in self.no_self_wait else ()
        for (s, v) in evs:
            k = id(s)
            if k in own:
                continue
            if self.waited[eng].get(k, 0) >= v:
                continue
            if k not in need or need[k][1] < v:
                need[k] = (s, v)
        for k, (s, v) in need.items():
            self.waited[eng][k] = v
        return list(need.values())

    def _mark(self, ev, R, W):
        for r in R:
            r.r.append(ev)
        for w in W:
            w.w = ev
            w.r = []

    def op(self, eng, fn, R=(), W=()):
        R = [self.R(x) for x in R]
        W = [self.R(x) for x in W]
        waits = self._collect(eng, R, W)
        if self.cnt[eng] >= SEM_LIMIT:
            self.sems[eng].append(self.stack.enter_context(self.nc.semaphore(f"s_{eng}_{len(self.sems[eng])}")))
            self.own_sem_ids[eng].add(id(self.sems[eng][-1]))
            self.cnt[eng] = 0
        self.cnt[eng] += 1
        sem = self.sems[eng][-1]
        ev = (sem, self.cnt[eng])
        self.q[eng].append((waits, fn, (sem, 1)))
        self._mark(ev, R, W)
        self.nops += 1
        return ev

    def dma(self, eng, out, in_, R=(), W=(), sem_idx=None):
        R = [self.R(x) for x in R]
        W = [self.R(x) for x in W]
        if sem_idx is None:
            sem_idx = self.dma_rr
            self.dma_rr = (self.dma_rr + 1) % 8
        s = self.dma_sems[sem_idx]
        extra = [(s, self.dma_cnt[sem_idx])] if self.dma_cnt[sem_idx] else []
        waits = self._collect(eng, R, W, extra)
        self.dma_cnt[sem_idx] += 16
        ev = (s, self.dma_cnt[sem_idx])
        self.q[eng].append((waits, (lambda e, o=out, i=in_: e.dma_start(out=o, in_=i)), (s, 16)))
        self._mark(ev, R, W)
        return ev

    def final_wait(self, eng, evs):
        self.q[eng].append((list(evs), None, None))

    def emit(self):
        nc = self.nc
        with nc.Block() as block:
            def mk(name):
                def body(e):
                    for waits, fn, inc in self.q[name]:
                        for (s, v) in waits:
                            e.wait_ge(s, v)
                        if fn is not None:
                            ins = fn(e)
                            ins.then_inc(inc[0], inc[1])
                return body
            block.tensor(mk("pe"))
            block.scalar(mk("act"))
            block.vector(mk("dve"))
            block.gpsimd(mk("pool"))
            block.sync(mk("sp"))


def build_program(nch_run, dumps=()):
    nc = bass.Bass("TRN2", target_bir_lowering=False)
    xT = nc.dram_tensor("xT", [nch_run, 128, KD, TC], F32, kind="ExternalInput").ap()
    yT = nc.dram_tensor("yT", [nch_run, 128, KD, TC], F32, kind="ExternalOutput").ap()
    wst = nc.dram_tensor("wst", [DEPTH, NBLK, 128, 4096], F32, kind="ExternalInput").ap()
    small = nc.dram_tensor("small", [128, DEPTH, NSMALL], F32, kind="ExternalInput").ap()
    craw = nc.dram_tensor("craw", [DEPTH, 128, 16, 256], F32, kind="ExternalInput").ap()
    consts = nc.dram_tensor("consts", [128, 128 * 3 + TC], F32, kind="ExternalInput").ap()
    padb = nc.dram_tensor("padb", [128, nch_run * NSUB], F32, kind="ExternalInput").ap()
    tabs = nc.dram_tensor("tabs", [DEPTH, 16, 128, 2 * TC], F32).ap()
    cz = nc.dram_tensor("cz", [DEPTH, 128, 4096], F32).ap()
    czn = nc.dram_tensor("czn", [DEPTH, 128, 4096], F32).ap()
    dump_out = {}
    dump_c = 1 if nch_run > 1 else 0

    st = ExitStack()
    P = Prog(nc, st)

    def sb(name, shape, dt=F32):
        return st.enter_context(nc.sbuf_tensor(name, shape, dt))

    X = sb("X", [128, KD, TC])
    HT = sb("HT", [128, KD, TC], BF16)
    SQ = sb("SQ", [128, 2, TC], BF16)
    RSTD = sb("RSTD", [128, TC])
    OT = sb("OT", [128, KD, TC])
    WB = [sb(f"WB{i}", [128, 4096], BF16) for i in range(NBUF)]
    SM = sb("SM", [128, DEPTH, NSMALL])
    CONS = sb("CONS", [128, 128 * 3 + TC])
    PADB = sb("PADB", [128, nch_run * NSUB])
    IDB = sb("IDB", [128, 128], BF16)
    ONESB = sb("ONESB", [128, 128], BF16)
    WGB = sb("WGB", [128, DEPTH, 64], BF16)
    ONE1 = sb("ONE1", [128, 1])
    LNS = sb("LNS", [128, 1])
    EPS1 = sb("EPS1", [128, 1])
    UT = sb("UT", [128, 4, TC], BF16)
    QKPH = sb("QKPH", [128, DEPTH, 8, 3])
    QKP1 = sb("QKP1", [128, 2, 3 + TC])
    QKA = sb("QKA", [128, 2, TC])
    QKT = sb("QKT", [128, 8, TC], BF16)
    V = sb("V", [128, NSUB, 4, 129], BF16)
    SIGO = sb("SIGO", [128, 4, TC], BF16)
    BIG = sb("BIG", [128, 3 * KD, TC], BF16)
    G = sb("G", [128, NSUB, 8])
    YA = sb("YA", [128, 4, TC], BF16)
    YG = sb("YG", [128, 4, TC], BF16)
    YB = sb("YB", [128, 4, TC], BF16)
    S5DEC = sb("S5DEC", [128, DEPTH, 16])
    S5ROT = sb("S5ROT", [128, DEPTH, 2, 16])
    S5ST = sb("S5ST", [128, DEPTH, 2, 16])
    S5E = sb("S5E", [128, 2, 16])
    TAB = [sb(f"TAB{i}", [128, 2 * TC]) for i in range(2)]
    BB = sb("BB", [128, 2, TC])
    SS = sb("SS", [128, 2, TC])
    T = [sb(f"T{i}", [128, TC]) for i in range(4)]
    XR = [sb(f"XR{i}", [128, 2, TC], BF16) for i in range(2)]
    YS = sb("YS", [128, TC])
    CT = sb("CT", [128, DEPTH, 4, 129])
    CTB = sb("CTB", [128, 4, 128], BF16)
    NB = sb("NB", [128, 4, 128], BF16)
    SP_ = sb("SP_", [128, 4])
    LI = sb("LI", [128, 4])
    SPB = sb("SPB", [128, 4, 128])
    BD = sb("BD", [128, 4])
    TW = sb("TW", [128, 4])
    WEX = sb("WEX", [128, 4])
    DEC = sb("DEC", [128, 4])
    DT_ = sb("DT_", [128, TC])
    DTM = sb("DTM", [128, TC])
    EF = sb("EF", [128, TC])
    QP = sb("QP", [128, TC], BF16)
    WT = sb("WT", [128, TC], BF16)
    KW = sb("KW", [128, TC], BF16)
    SQH = sb("SQH", [128, TC], BF16)
    FH = sb("FH", [128, DEPTH, NFT, 2])
    GE = sb("GE", [128, TC])
    PRK = GE[:].bitcast(I32)
    SCN = ("dt", "th", "ar", "c", "s", "abr", "abi", "nr", "den", "t1", "t2", "zre", "zim", "a16", "c2", "s2", "nzim")
    SCT = sb("SCT", [128, len(SCN), 16])
    SC = {n: SCT[:, i, :] for i, n in enumerate(SCN)}
    SIGA = [BIG[:, t, :] for t in range(KD)]
    SIGB = [BIG[:, KD + t, :] for t in range(KD)]
    MRG = [BIG[:, 2 * KD + t, :] for t in range(KD)]
    ACTT = [BIG[:, j, :] for j in range(NFT)]
    HTF = HT[:].rearrange("p a t -> p (a t)").bitcast(F32)
    AD, HH, RS = HTF[:, 0:TC], HTF[:, TC:2 * TC], HTF[:, 2 * TC:3 * TC]
    ADR, HHR, RSR = ["HT0", "HT1"], ["HT2", "HT3"], ["HT4", "HT5"]
    ACC = T[3]
    GP = BB[:].rearrange("p a t -> p (a t)")

    PS = [st.enter_context(nc.psum_tensor(f"PS{i}", [128, TC], F32)) for i in range(7)]
    PST = st.enter_context(nc.psum_tensor("PST", [128, TC], BF16))
    PSR = [Reg(f"ps{i}") for i in range(7)]
    bank_rr = [0]

    def bank():
        i = bank_rr[0]
        bank_rr[0] = (i + 1) % 6
        return PS[i], PSR[i]

    def ACT(out, in_, func, R, W, **kw):
        P.op("act", lambda e: e.activation(out=out, in_=in_, func=func, **kw), R, W)

    def TT(eng, out, in0, in1, op, R, W):
        P.op(eng, lambda e: e.tensor_tensor(out=out, in0=in0, in1=in1, op=op), R, W)

    def TS(eng, out, in0, s1, s2, op0, op1, R, W):
        if s2 is None:
            P.op(eng, lambda e: e.tensor_scalar(out=out, in0=in0, scalar1=s1, scalar2=None, op0=op0), R, W)
        else:
            P.op(eng, lambda e: e.tensor_scalar(out=out, in0=in0, scalar1=s1, scalar2=s2, op0=op0, op1=op1), R, W)

    def STT(eng, out, in0, scalar, in1, op0, op1, R, W):
        P.op(eng, lambda e: e.scalar_tensor_tensor(out=out, in0=in0, scalar=scalar, in1=in1, op0=op0, op1=op1), R, W)

    def CP(eng, out, in_, R, W):
        P.op(eng, lambda e: e.tensor_copy(out=out, in_=in_), R, W)

    def RECIP(out, in_, R, W):
        P.op("dve", lambda e: e.reciprocal(out=out, in_=in_), R, W)

    def MM(out, lhsT, rhs, R, W, start=True, stop=True):
        P.op("pe", lambda e: e.matmul(out, lhsT=lhsT, rhs=rhs, start=start, stop=stop), R, W)

    def MMG(out, pairs, R, W):
        pairs = list(pairs)

        def fn(e):
            ins = None
            n = len(pairs)
            for i, (a, b) in enumerate(pairs):
                ins = e.matmul(out, lhsT=a, rhs=b, start=(i == 0), stop=(i == n - 1))
            return ins
        P.op("pe", fn, R, W)

    def dump(name, ap, shape, R):
        if name in dumps:
            t = nc.dram_tensor("dbg_" + name, list(shape), F32 if ap.dtype == F32 else BF16, kind="ExternalOutput").ap()
            dump_out[name] = P.dma("sp", t, ap, R=R, W=["dbg_" + name])

    P.dma("sp", SM[:], small[:, :, :], W=["SM"])
    P.dma("sp", CONS[:], consts[:, :], W=["CONS"])
    P.dma("sp", PADB[:], padb[:, :], W=["PADB"])
    IDF = CONS[:, 0:128]
    TRIU = CONS[:, 128:256]
    ONESF = CONS[:, 256:384]
    IOTA = CONS[:, 384:384 + TC]
    CP("dve", IDB[:], IDF, ["CONS"], ["IDB"])
    CP("dve", ONESB[:], ONESF, ["CONS"], ["ONESB"])
    for l in range(DEPTH):
        o = _off["wgate"][0]
        CP("dve", WGB[:, l, :], SM[:, l, o:o + 64], ["SM"], ["WGB"])
    P.op("pool", lambda e: e.memset(V[:], 1.0), W=["V0", "V1", "V2", "V3"])
    P.op("pool", lambda e: e.memset(CT[:], 0.0), W=["CT"])
    P.op("pool", lambda e: e.memset(S5ST[:], 0.0), W=["S5ST"])
    P.op("pool", lambda e: e.memset(QKPH[:], 0.0), W=["QKPH"])
    P.op("pool", lambda e: e.memset(FH[:], 0.0), W=["FH"])
    P.op("pool", lambda e: e.memset(ONE1[:], 1.0), W=["ONE1"])
    P.op("pool", lambda e: e.memset(LNS[:], LN_SCALE), W=["LNS"])
    P.op("pool", lambda e: e.memset(EPS1[:], EPS), W=["EPS1"])

    def sm(l, name, j=None):
        o, w = _off[name]
        if j is None:
            return SM[:, l, o:o + w]
        return SM[:, l, o + j:o + j + 1]

    PRC = OT[:].rearrange("p a t -> p (a t)")
    PRZ = X[:].rearrange("p a t -> p (a t)")
    PRA, PRF, PRT0, PRT1 = T[0], T[1], T[2], T[3]

    OTR = [f"OT{t}" for t in range(KD)]
    XR_ALL = [f"X{t}" for t in range(KD)]

    def prologue_layer(l):
        s = SC
        lre, lim, ldt = sm(l, "lre"), sm(l, "lim"), sm(l, "ldt")
        dec = S5DEC[:, l, :]
        def exp_taylor(dst, dreg, src, sreg):
            TS("dve", dst, src, 1.0 / 5040, 1.0 / 720, ALU.mult, ALU.add, [sreg], [dreg])
            for cf in (1.0 / 120, 1.0 / 24, 1.0 / 6, 0.5, 1.0, 1.0):
                TT("dve", dst, dst, src, ALU.mult, [sreg], [dreg])
                TS("dve", dst, dst, cf, None, ALU.add, None, [], [dreg])

        def sin_reduced(dst, dreg, ang, areg, shift):
            TS("dve", s["t1"], ang, shift, 1.0 / TWO_PI, ALU.add, ALU.mult, [areg], ["sc_t1"])
            CP("dve", PRK[:, 0:16], s["t1"], ["sc_t1"], ["PRK"])
            CP("dve", s["t1"], PRK[:, 0:16], ["PRK"], ["sc_t1"])
            STT("dve", s["t2"], s["t1"], -CW1, ang, ALU.mult, ALU.add, ["sc_t1", areg], ["sc_t2"])
            STT("dve", s["t2"], s["t1"], -CW2, s["t2"], ALU.mult, ALU.add, ["sc_t1"], ["sc_t2"])
            TS("dve", s["t2"], s["t2"], shift, -math.pi, ALU.add, ALU.max, [], ["sc_t2"])
            TS("dve", s["t2"], s["t2"], math.pi, None, ALU.min, None, [], ["sc_t2"])
            ACT(dst, s["t2"], AF.Sin, ["sc_t2"], [dreg])

        TS("dve", s["a16"], ldt, 1.0 / 16, None, ALU.mult, None, ["SM"], ["sc_a16"])
        exp_taylor(s["dt"], "sc_dt", s["a16"], "sc_a16")
        for _ in range(4):
            TT("dve", s["dt"], s["dt"], s["dt"], ALU.mult, [], ["sc_dt"])
        TT("dve", s["ar"], lre, s["dt"], ALU.mult, ["SM", "sc_dt"], ["sc_ar"])
        TT("dve", s["th"], lim, s["dt"], ALU.mult, ["SM", "sc_dt"], ["sc_th"])
        exp_taylor(dec, "S5DEC", s["ar"], "sc_ar")
        sin_reduced(s["s"], "sc_s", s["th"], "sc_th", 0.0)
        sin_reduced(s["c"], "sc_c", s["th"], "sc_th", math.pi / 2)
        TT("dve", s["abr"], dec, s["c"], ALU.mult, ["S5DEC", "sc_c"], ["sc_abr"])
        TT("dve", s["abi"], dec, s["s"], ALU.mult, ["S5DEC", "sc_s"], ["sc_abi"])
        TS("dve", s["nr"], s["abr"], -1.0, None, ALU.add, None, ["sc_abr"], ["sc_nr"])
        TT("dve", s["t1"], lre, lre, ALU.mult, ["SM"], ["sc_t1"])
        TT("dve", s["t2"], lim, lim, ALU.mult, ["SM"], ["sc_t2"])
        TT("dve", s["den"], s["t1"], s["t2"], ALU.add, ["sc_t1", "sc_t2"], ["sc_den"])
        RECIP(s["den"], s["den"], ["sc_den"], ["sc_den"])
        TT("dve", s["t1"], s["nr"], lre, ALU.mult, ["sc_nr", "SM"], ["sc_t1"])
        TT("dve", s["t2"], s["abi"], lim, ALU.mult, ["sc_abi", "SM"], ["sc_t2"])
        TT("dve", s["t1"], s["t1"], s["t2"], ALU.add, ["sc_t1", "sc_t2"], ["sc_t1"])
        TT("dve", s["zre"], s["t1"], s["den"], ALU.mult, ["sc_t1", "sc_den"], ["sc_zre"])
        TT("dve", s["t1"], s["abi"], lre, ALU.mult, ["sc_abi", "SM"], ["sc_t1"])
        TT("dve", s["t2"], s["nr"], lim, ALU.mult, ["sc_nr", "SM"], ["sc_t2"])
        TT("dve", s["t1"], s["t1"], s["t2"], ALU.subtract, ["sc_t1", "sc_t2"], ["sc_t1"])
        TT("dve", s["zim"], s["t1"], s["den"], ALU.mult, ["sc_t1", "sc_den"], ["sc_zim"])
        TS("dve", s["nzim"], s["zim"], -1.0, None, ALU.mult, None, ["sc_zim"], ["sc_nzim"])
        P.dma("sp", PRC, craw[l].rearrange("p a t -> p (a t)"), W=OTR)
        for k in range(16):
            cre, cim = PRC[:, k * 256:k * 256 + 128], PRC[:, k * 256 + 128:k * 256 + 256]
            o1, o2 = PRZ[:, k * 256:k * 256 + 128], PRZ[:, k * 256 + 128:k * 256 + 256]
            TS("dve", o1, cim, s["nzim"][:, k:k + 1], None, ALU.mult, None, OTR + ["sc_nzim"], XR_ALL)
            STT("dve", o1, cre, s["zre"][:, k:k + 1], o1, ALU.mult, ALU.add, OTR + ["sc_zre"], XR_ALL)
            TS("dve", o2, cim, s["zre"][:, k:k + 1], -1.0, ALU.mult, ALU.mult, OTR + ["sc_zre"], XR_ALL)
            STT("dve", o2, cre, s["nzim"][:, k:k + 1], o2, ALU.mult, ALU.add, OTR + ["sc_nzim"], XR_ALL)
        P.dma("sp", cz[l], PRZ, R=XR_ALL, W=[f"cz{l}"])
        for k in range(16):
            TS("dve", PRC[:, k * 128:(k + 1) * 128], PRZ[:, k * 256:k * 256 + 128], -1.0, None, ALU.mult, None, XR_ALL, OTR)
        P.dma("sp", czn[l], PRC, R=OTR, W=[f"czn{l}"] + OTR)
        for k in range(16):
            th = s["th"][:, k:k + 1]
            TS("dve", PRA[:], IOTA, th, None, ALU.mult, None, ["CONS", "sc_th"], ["T0"])
            tb = TAB[k % 2]
            for half, shift in ((1, 0.0), (0, math.pi / 2)):
                TS("dve", PRF[:], PRA[:], shift, 1.0 / TWO_PI, ALU.add, ALU.mult, ["T0"], ["T1"])
                CP("dve", PRK[:], PRF[:], ["T1"], ["PRK"])
                CP("dve", PRF[:], PRK[:], ["PRK"], ["T1"])
                STT("dve", PRT0[:], PRF[:], -CW1, PRA[:], ALU.mult, ALU.add, ["T1", "T0"], ["T2"])
                STT("dve", PRT0[:], PRF[:], -CW2, PRT0[:], ALU.mult, ALU.add, ["T1", "T2"], ["T2"])
                TS("dve", PRT0[:], PRT0[:], shift, -math.pi, ALU.add, ALU.max, ["T2"], ["T2"])
                TS("dve", PRT0[:], PRT0[:], math.pi, None, ALU.min, None, ["T2"], ["T2"])
                ACT(tb[:, half * TC:(half + 1) * TC], PRT0[:], AF.Sin, ["T2"], [f"TAB{k % 2}"])
            c1, s1 = s["c"][:, k:k + 1], s["s"][:, k:k + 1]
            c511, s511 = tb[:, TC - 1:TC], tb[:, 2 * TC - 1:2 * TC]
            tr = f"TAB{k % 2}"
            TT("dve", s["c2"][:, k:k + 1], c511, c1, ALU.mult, [tr, "sc_c"], ["sc_c2"])
            TT("dve", s["s2"][:, k:k + 1], s511, s1, ALU.mult, [tr, "sc_s"], ["sc_s2"])
            TT("dve", S5ROT[:, l, 0, k:k + 1], s["c2"][:, k:k + 1], s["s2"][:, k:k + 1], ALU.subtract, ["sc_c2", "sc_s2"], ["S5ROT"])
            TT("dve", s["c2"][:, k:k + 1], s511, c1, ALU.mult, [tr, "sc_c"], ["sc_c2"])
            TT("dve", s["s2"][:, k:k + 1], c511, s1, ALU.mult, [tr, "sc_s"], ["sc_s2"])
            TT("dve", S5ROT[:, l, 1, k:k + 1], s["c2"][:, k:k + 1], s["s2"][:, k:k + 1], ALU.add, ["sc_c2", "sc_s2"], ["S5ROT"])
            P.dma("sp", tabs[l, k], tb[:], R=[tr], W=[f"tab{l}_{k}", tr])

    for l in range(DEPTH):
        prologue_layer(l)

    border = list(range(11)) + [36] + list(range(11, NBLK))
    wq = [(l, b) for c in range(nch_run) for l in range(DEPTH) for b in border]
    wstate = {"next": 0, "use": 0}
    WBR = [Reg(f"wb{i}") for i in range(NBUF)]

    def w_issue():
        i = wstate["next"]
        l, b = wq[i]
        buf = i % NBUF
        if b == 10:
            P.dma("pool", WB[buf][:], cz[l], R=[f"cz{l}"], W=[WBR[buf]], sem_idx=8 + buf)
        elif b == 36:
            P.dma("pool", WB[buf][:], czn[l], R=[f"czn{l}"], W=[WBR[buf]], sem_idx=8 + buf)
        else:
            P.dma("pool", WB[buf][:], wst[l, b], W=[WBR[buf]], sem_idx=8 + buf)
        wstate["next"] += 1

    def w_get(l, b, extra_hold=0):
        i = wstate["use"]
        assert wq[i] == (l, b), (wq[i], l, b)
        wstate["use"] += 1
        while wstate["next"] < min(len(wq), i + NBUF - 1 - extra_hold):
            w_issue()
        return WB[i % NBUF], WBR[i % NBUF]

    HTR = [f"HT{kt}" for kt in range(KD)]

    def rmsnorm_stats(src_tile, src_reg):
        ps, pr = bank()
        for kt in range(KD):
            ACT(SQ[:, kt % 2, :], src_tile(kt), AF.Square, [src_reg(kt)], [f"SQ{kt % 2}"])
            MM(ps[:, :], ONESB[:], SQ[:, kt % 2, :], ["ONESB", f"SQ{kt % 2}"], [pr], start=(kt == 0), stop=(kt == KD - 1))
        ACT(RSTD[:], ps[:, :], AF.Ln, [pr, "EPS1"], ["RSTD"], scale=1.0 / D, bias=EPS1[:, 0:1])
        ACT(RSTD[:], RSTD[:], AF.Exp, [], ["RSTD"], scale=-0.5)

    def pre_norm(l, gname):
        rmsnorm_stats(lambda kt: X[:, kt, :], lambda kt: f"X{kt}")
        for kt in range(KD):
            STT("dve", HT[:, kt, :], X[:, kt, :], sm(l, gname, kt), RSTD[:], ALU.mult, ALU.mult, [f"X{kt}", "RSTD", "SM"], [f"HT{kt}"])

    def stats_tile(t, ps, pr):
        ACT(SQ[:, t % 2, :], ps[:, :], AF.Square, [pr], [f"SQ{t % 2}"])
        MM(PS[6][:, :], ONESB[:], SQ[:, t % 2, :], ["ONESB", f"SQ{t % 2}"], [PSR[6]], start=(t == 0), stop=(t == KD - 1))

    def post_norm_residual(l, gname):
        rmsnorm_stats(lambda kt: OT[:, kt, :], lambda kt: f"OT{kt}")
        for kt in range(KD):
            STT("dve", OT[:, kt, :], OT[:, kt, :], sm(l, gname, kt), RSTD[:], ALU.mult, ALU.mult, ["RSTD", "SM"], [f"OT{kt}"])
            TT("pool", X[:, kt, :], X[:, kt, :], OT[:, kt, :], ALU.add, [f"OT{kt}"], [f"X{kt}"])

    def proj_tile(wb, wr, mm, ncols=512):
        ps, pr = bank()
        MMG(ps[:, :], [(wb[:, kc * ncols + mm * 128: kc * ncols + (mm + 1) * 128], HT[:, kc, :]) for kc in range(KD)], [wr] + HTR, [pr])
        return ps, pr

    def refresh_state(l):
        ACT(CTB[:], CT[:, l, :, 0:128], AF.Copy, ["CT"], ["CTB"])
        for h in range(4):
            TS("pool", NB[:, h, :], ONESB[:], CT[:, l, h, 128:129], None, ALU.mult, None, ["ONESB", "CT"], ["NB"])

    def mixer(c, l):
        pre_norm(l, "g_pre")
        if c == dump_c and l == 0:
            dump("ht", HT[:], [128, KD, TC], HTR)
        wb, wr = w_get(l, 0)
        for mm in range(4):
            ps, pr = proj_tile(wb, wr, mm)
            CP("dve", UT[:, mm, :], ps[:, :], [pr], [f"UT{mm}"])
        for blk, base in ((1, 0), (2, 4)):
            wb, wr = w_get(l, blk)
            for mm in range(4):
                t = base + mm
                pb = t % 2
                ps, pr = proj_tile(wb, wr, mm)
                rq = f"QKP1_{pb}"
                ACT(QKP1[:, pb, 3:3 + TC], ps[:, :], AF.Copy, [pr], [rq])
                CP("pool", QKP1[:, pb, 0:3], QKPH[:, l, t, :], ["QKPH"], [rq])
                o = _off["wqk"][0] + t * 4
                TS("dve", QKA[:, pb, :], QKP1[:, pb, 0:TC], SM[:, l, o:o + 1], sm(l, "bqk", t), ALU.mult, ALU.add, [rq, "SM"], [f"QKA{pb}"])
                for j in (1, 2, 3):
                    STT("dve", QKA[:, pb, :], QKP1[:, pb, j:j + TC], SM[:, l, o + j:o + j + 1], QKA[:, pb, :], ALU.mult, ALU.add, [rq, "SM"], [f"QKA{pb}"])
                ACT(QKT[:, t, :], QKA[:, pb, :], AF.Silu, [f"QKA{pb}"], [f"QKT{t}"])
                CP("pool", QKPH[:, l, t, :], QKP1[:, pb, TC:TC + 3], [rq], ["QKPH"])
        wb, wr = w_get(l, 3)
        for sc in range(NSUB):
            ps, pr = bank()
            MMG(ps[:, :], [(HT[:, kc, sc * 128:(sc + 1) * 128], wb[:, kc * 512:(kc + 1) * 512]) for kc in range(KD)], [wr] + HTR, [pr])
            CP("dve", V[:, sc, :, 0:128], ps[:, :].rearrange("p (h t) -> p h t", h=4), [pr], [f"V{sc}"])
        for sc in range(NSUB):
            ps, pr = bank()
            MMG(ps[:, 0:8], [(HT[:, kc, sc * 128:(sc + 1) * 128], WGB[:, l, kc * 8:kc * 8 + 8]) for kc in range(KD)], ["WGB"] + HTR, [pr])
            TT("dve", G[:, sc, :], ps[:, 0:8], sm(l, "bgate"), ALU.add, [pr, "SM"], [f"G{sc}"])
        wb, wr = w_get(l, 4)
        for mm in range(4):
            ps, pr = proj_tile(wb, wr, mm)
            ACT(SIGO[:, mm, :], ps[:, :], AF.Sigmoid, [pr], [f"SIGO{mm}"])
        for dst, nm, blks in ((SIGA, "SIGA", (5, 6)), (SIGB, "SIGB", (7, 8))):
            for bi, blk in enumerate(blks):
                wb, wr = w_get(l, blk)
                for mm in range(4):
                    t = bi * 4 + mm
                    ps, pr = proj_tile(wb, wr, mm)
                    ACT(dst[t], ps[:, :], AF.Sigmoid, [pr], [f"{nm}{t}"])
        if c == dump_c and l == 0:
            dump("ut", UT[:], [128, 4, TC], [f"UT{m}" for m in range(4)])
            dump("qkt", QKT[:], [128, 8, TC], [f"QKT{m}" for m in range(8)])
        wbB, wrB = w_get(l, 9)
        wbC, wrC = w_get(l, 10)
        wbN, wrN = w_get(l, 36, extra_hold=1)
        ys_ps, ys_pr = PS[6], PSR[6]

        def s5_front(k):
            q4 = k // 4
            tb, tbr = TAB[k % 2], f"TAB{k % 2}"
            P.dma("sp", tb[:], tabs[l, k], R=[f"tab{l}_{k}"], W=[tbr])
            cs, sn = tb[:, 0:TC], tb[:, TC:2 * TC]
            psr_, prr = bank()
            psi_, pri = bank()
            MM(psr_[:, :], wbB[:, k * 256:k * 256 + 128], UT[:, q4, :], [wrB, f"UT{q4}"], [prr])
            MM(psi_[:, :], wbB[:, k * 256 + 128:k * 256 + 256], UT[:, q4, :], [wrB, f"UT{q4}"], [pri])
            TA, TAR = HTF[:, 3 * TC:4 * TC], ["HT6", "HT7"]
            TT("dve", T[0][:], psr_[:, :], cs, ALU.mult, [prr, tbr], ["T0"])
            TT("dve", T[1][:], psi_[:, :], sn, ALU.mult, [pri, tbr], ["T1"])
            TT("dve", TA, psi_[:, :], cs, ALU.mult, [pri, tbr], TAR)
            TT("dve", GE[:], psr_[:, :], sn, ALU.mult, [prr, tbr], ["GE"])
            TT("dve", BB[:, 0, :], T[0][:], T[1][:], ALU.add, [], ["BB0", "T0", "T1"])
            TT("dve", BB[:, 1, :], TA, GE[:], ALU.subtract, [], ["BB1", "GE"] + TAR)
            ssb = SS if k % 2 == 0 else QKA
            ssr = ("SS0", "SS1") if k % 2 == 0 else ("QKA0", "QKA1")
            for ri in range(2):
                dec_b = S5DEC[:, l, k:k + 1].to_broadcast([128, TC])
                P.op("dve", (lambda e, ri=ri, dec_b=dec_b, init=S5ST[:, l, ri, k:k + 1], o=ssb[:, ri, :]:
                             e.tensor_tensor_scan(out=o, data0=dec_b, data1=BB[:, ri, :], initial=init, op0=ALU.mult, op1=ALU.add)),
                     ["S5DEC", "S5ST", f"BB{ri}"], [ssr[ri]])
                ACT(S5E[:, ri, k:k + 1], ssb[:, ri, TC - 1:TC], AF.Copy, [ssr[ri]], ["S5E"])

        def s5_back(k):
            q4 = k // 4
            tb, tbr = TAB[k % 2], f"TAB{k % 2}"
            cs, sn = tb[:, 0:TC], tb[:, TC:2 * TC]
            ssb = SS if k % 2 == 0 else QKA
            ssr = ("SS0", "SS1") if k % 2 == 0 else ("QKA0", "QKA1")
            xr, xrr = XR[k % 2], f"XR{k % 2}"
            xq = T[2 + k % 2][:].bitcast(BF16).rearrange("p (a t) -> p a t", a=2)
            xqr = f"T{2 + k % 2}"
            TT("pool", xr[:, 0, :], ssb[:, 0, :], cs, ALU.mult, [ssr[0], tbr], [xrr + "a"])
            TT("pool", xr[:, 1, :], ssb[:, 1, :], sn, ALU.mult, [ssr[1], tbr], [xrr + "b"])
            link = [xqr] if k < 2 else []
            TT("pool", xq[:, 0, :], ssb[:, 0, :], sn, ALU.mult, [ssr[0], tbr], [xqr + "a"] + link)
            TT("pool", xq[:, 1, :], ssb[:, 1, :], cs, ALU.mult, [ssr[1], tbr], [xqr + "b"] + link)
            cre = wbC[:, k * 256:k * 256 + 128]
            cimn = wbC[:, k * 256 + 128:k * 256 + 256]
            cren = wbN[:, k * 128:(k + 1) * 128]
            MM(ys_ps[:, :], cre, xr[:, 0, :], [wrC, xrr + "a"], [ys_pr], start=(k % 4 == 0), stop=False)
            MM(ys_ps[:, :], cren, xr[:, 1, :], [wrN, xrr + "b"], [ys_pr], start=False, stop=False)
            MM(ys_ps[:, :], cimn, xq[:, 0, :], [wrC, xqr + "a"], [ys_pr], start=False, stop=False)
            MM(ys_ps[:, :], cimn, xq[:, 1, :], [wrC, xqr + "b"], [ys_pr], start=False, stop=(k % 4 == 3))
            if k % 4 == 3:
                STT("dve", YG[:, q4, :], UT[:, q4, :], sm(l, "dvec", q4), ys_ps[:, :], ALU.mult, ALU.add, [f"UT{q4}", "SM", ys_pr], [f"YG{q4}"])

        def mlstm_gen():
            refresh_state(l)
            yield
            for sc in range(NSUB):
                tsl = slice(sc * 128, (sc + 1) * 128)
                gcol = c * NSUB + sc
                ACT(SP_[:], G[:, sc, 4:8], AF.Exp, [f"G{sc}"], ["SP_"], scale=-1.0)
                ACT(SP_[:], SP_[:], AF.Ln, ["ONE1"], ["SP_"], bias=ONE1[:, 0:1])
                TS("dve", LI[:], G[:, sc, 0:4], PADB[:, gcol:gcol + 1], None, ALU.add, None, [f"G{sc}", "PADB"], ["LI"])
                yield
                for h in range(4):
                    TS("dve", SPB[:, h, :], ONESF, SP_[:, h:h + 1], None, ALU.mult, None, ["CONS", "SP_"], ["SPB"])
                psF, prF = bank()
                MM(psF[:, 0:4], TRIU, SP_[:], ["CONS", "SP_"], [prF])
                MM(psF[:, 4:8], ONESF, SP_[:], ["CONS", "SP_"], [prF])
                yield
                psFr, prFr = bank()
                for h in range(4):
                    MM(psFr[:, h * 128:(h + 1) * 128], SPB[:, h, :], TRIU, ["SPB", "CONS"], [prFr])
                TT("dve", BD[:], psF[:, 0:4], LI[:], ALU.add, [prF, "LI"], ["BD"])
                TT("dve", TW[:], BD[:], psF[:, 4:8], ALU.subtract, [prF, "BD"], ["TW"])
                ACT(WEX[:], TW[:], AF.Exp, ["TW"], ["WEX"])
                ACT(DEC[:], psF[:, 4:8], AF.Exp, [prF], ["DEC"], scale=-1.0)
                TS("dve", BD[:], BD[:], LN_SCALE, None, ALU.add, None, ["TW"], ["BD"])
                yield
                for h in range(4):
                    hs = slice(h * 128, (h + 1) * 128)
                    ACT(DT_[:, hs], psFr[:, hs], AF.Exp, [prFr, "BD"], ["DT_"], scale=-1.0, bias=BD[:, h:h + 1])
                    TT("pool", DTM[:, hs], DT_[:, hs], TRIU, ALU.mult, ["DT_", "CONS"], ["DTM"])
                ACT(EF[:], psFr[:, :], AF.Exp, [prFr, "LNS"], ["EF"], scale=-1.0, bias=LNS[:, 0:1])
                psS, prS = bank()
                for h in range(4):
                    MM(psS[:, h * 128:(h + 1) * 128], QKT[:, 4 + h, tsl], QKT[:, h, tsl], [f"QKT{4 + h}", f"QKT{h}"], [prS])
                for h in range(4):
                    P.op("pe", (lambda e, o=PST[:, h * 128:(h + 1) * 128], i=QKT[:, 4 + h, tsl]: e.transpose(o, i, IDB[:])),
                         [f"QKT{4 + h}", "IDB"], ["PST"])
                yield
                TT("dve", QP[:].rearrange("p (h t) -> p h t", h=4), QKT[:, 0:4, tsl], EF[:].rearrange("p (h t) -> p h t", h=4), ALU.mult,
                   [f"QKT{h}" for h in range(4)] + ["EF"], ["QP"])
                TT("dve", WT[:], psS[:, :], DTM[:], ALU.mult, [prS, "DTM"], ["WT"])
                for h in range(4):
                    hs = slice(h * 128, (h + 1) * 128)
                    TS("dve", KW[:, hs], PST[:, hs], WEX[:, h:h + 1], None, ALU.mult, None, ["PST", "WEX"], ["KW"])
                yield
                psN, prN = bank()
                psD, prD = bank()
                for h in range(4):
                    hs = slice(h * 128, (h + 1) * 128)
                    MM(psN[:, hs], V[:, sc, h, 0:128], WT[:, hs], [f"V{sc}", "WT"], [prN], start=True, stop=False)
                    MM(psN[:, hs], CTB[:, h, :], QP[:, hs], ["CTB", "QP"], [prN], start=False, stop=True)
                    MM(psD[:, hs], ONESB[:], WT[:, hs], ["ONESB", "WT"], [prD], start=True, stop=False)
                    MM(psD[:, hs], NB[:, h, :], QP[:, hs], ["NB", "QP"], [prD], start=False, stop=True)
                psU = []
                for half in range(2):
                    pu, pru = bank()
                    psU.append((pu, pru))
                    for hh in range(2):
                        h = half * 2 + hh
                        MM(pu[:, hh * 129:(hh + 1) * 129], KW[:, h * 128:(h + 1) * 128], V[:, sc, h, :], ["KW", f"V{sc}"], [pru])
                yield
                TS("dve", AD, psD[:, :], -1.0, 1.0, ALU.mult, ALU.max, [prD], ADR)
                TT("dve", AD, AD, psD[:, :], ALU.max, [prD], ADR)
                RECIP(AD, AD, [], ADR)
                TT("dve", HH, psN[:, :], AD, ALU.mult, [prN] + ADR, HHR)
                for half in range(2):
                    pu, pru = psU[half]
                    for hh in range(2):
                        h = half * 2 + hh
                        STT("dve", CT[:, l, h, :], CT[:, l, h, :], DEC[:, h:h + 1], pu[:, hh * 129:(hh + 1) * 129], ALU.mult, ALU.add, [pru, "DEC"], ["CT"])
                yield
                ACT(SQH[:], HH, AF.Square, HHR, ["SQH"])
                if sc < NSUB - 1:
                    refresh_state(l)
                psH, prH = bank()
                for h in range(4):
                    hs = slice(h * 128, (h + 1) * 128)
                    MM(psH[:, hs], ONESB[:], SQH[:, hs], ["ONESB", "SQH"], [prH])
                yield
                ACT(RS, psH[:, :], AF.Ln, [prH, "EPS1"], RSR, scale=1.0 / 128, bias=EPS1[:, 0:1])
                ACT(RS, RS, AF.Exp, [], RSR, scale=-0.5)
                yield
                for h in range(4):
                    hs = slice(h * 128, (h + 1) * 128)
                    STT("dve", HH[:, hs], HH[:, hs], sm(l, "ghead", h), RS[:, hs], ALU.mult, ALU.mult, RSR + ["SM"], HHR)
                TT("pool", YB[:, :, tsl], HH.rearrange("p (h t) -> p h t", h=4), SIGO[:, :, tsl], ALU.mult,
                   HHR + [f"SIGO{h}" for h in range(4)], [f"YB{h}" for h in range(4)])
                yield

        mg = mlstm_gen()

        def pull(n):
            for _ in range(n):
                try:
                    next(mg)
                except StopIteration:
                    return
        s5_front(0)
        pull(3)
        for k in range(1, 16):
            s5_front(k)
            s5_back(k - 1)
            pull(3)
        s5_back(15)
        for _ in mg:
            pass
        c5, s5 = S5ROT[:, l, 0, :], S5ROT[:, l, 1, :]
        TT("dve", SC["t1"], S5E[:, 0, :], c5, ALU.mult, ["S5E", "S5ROT"], ["sc_t1"])
        TT("dve", SC["t2"], S5E[:, 1, :], s5, ALU.mult, ["S5E", "S5ROT"], ["sc_t2"])
        TT("dve", S5ST[:, l, 0, :], SC["t1"], SC["t2"], ALU.subtract, ["sc_t1", "sc_t2"], ["S5ST"])
        TT("dve", SC["t1"], S5E[:, 0, :], s5, ALU.mult, ["S5E", "S5ROT"], ["sc_t1"])
        TT("dve", SC["t2"], S5E[:, 1, :], c5, ALU.mult, ["S5E", "S5ROT"], ["sc_t2"])
        TT("dve", S5ST[:, l, 1, :], SC["t1"], SC["t2"], ALU.add, ["sc_t1", "sc_t2"], ["S5ST"])
        for q4 in range(4):
            ACT(YG[:, q4, :], YG[:, q4, :], AF.Gelu_apprx_tanh, [], [f"YG{q4}"])
        wb, wr = w_get(l, 11)
        YGR = [f"YG{q}" for q in range(4)]
        for mm in range(4):
            ps, pr = bank()
            MMG(ps[:, :], [(wb[:, kc * 512 + mm * 128:kc * 512 + (mm + 1) * 128], YG[:, kc, :]) for kc in range(4)], [wr] + YGR, [pr])
            ACT(GE[:], ps[:, :], AF.Sigmoid, [pr], ["GE"])
            TT("dve", YA[:, mm, :], YG[:, mm, :], GE[:], ALU.mult, ["GE", f"YG{mm}"], [f"YA{mm}"])
        if c == dump_c and l == 0:
            dump("ya", YA[:], [128, 4, TC], [f"YA{m}" for m in range(4)])
        if c == dump_c and l == 0:
            dump("yb", YB[:], [128, 4, TC], [f"YB{m}" for m in range(4)])
        wbA, wrA = w_get(l, 12)
        wbBm, wrBm = w_get(l, 13)
        YAR = [f"YA{q}" for q in range(4)]
        YBR = [f"YB{q}" for q in range(4)]
        for mm in range(KD):
            psa, pra = bank()
            psb, prb = bank()
            MMG(psa[:, :], [(wbA[:, kc * 1024 + mm * 128:kc * 1024 + (mm + 1) * 128], YA[:, kc, :]) for kc in range(4)], [wrA] + YAR, [pra])
            MMG(psb[:, :], [(wbBm[:, kc * 1024 + mm * 128:kc * 1024 + (mm + 1) * 128], YB[:, kc, :]) for kc in range(4)], [wrBm] + YBR, [prb])
            TT("dve", T[0][:], psa[:, :], SIGA[mm], ALU.mult, [pra, f"SIGA{mm}"], ["T0"])
            TT("dve", T[1][:], psb[:, :], SIGB[mm], ALU.mult, [prb, f"SIGB{mm}"], ["T1"])
            TT("pool", MRG[mm], T[0][:], T[1][:], ALU.add, ["T0", "T1"], [f"MRG{mm}"])
        MR = [f"MRG{q}" for q in range(KD)]
        if c == dump_c and l == 0:
            dump("mrg", BIG[:, 2 * KD:3 * KD, :], [128, KD, TC], MR)
        for bi in range(2):
            wb, wr = w_get(l, 14 + bi)
            for mm in range(4):
                t = bi * 4 + mm
                ps, pr = bank()
                MMG(ps[:, :], [(wb[:, kc * 512 + mm * 128:kc * 512 + (mm + 1) * 128], MRG[kc]) for kc in range(KD)], [wr] + MR, [pr])
                ACT(OT[:, t, :], ps[:, :], AF.Copy, [pr], [f"OT{t}"])
        if c == dump_c and l == 0:
            dump("ot", OT[:], [128, KD, TC], [f"OT{t}" for t in range(KD)])
        post_norm_residual(l, "g_post")
        if c == dump_c and l == 0:
            dump("xmid", X[:], [128, KD, TC], XR_ALL)

    def ffn(c, l):
        pre_norm(l, "gf_pre")
        alias = [f"SIGA{t}" for t in range(KD)] + [f"SIGB{t}" for t in range(KD)] + [f"MRG{t}" for t in range(KD)]
        for bi in range(6):
            wbg, wrg = w_get(l, 16 + 2 * bi)
            wbu, wru = w_get(l, 17 + 2 * bi)
            ncols = 512 if bi < 5 else 256
            for mm in range(ncols // 128):
                j = bi * 4 + mm
                psg, prg = proj_tile(wbg, wrg, mm, ncols)
                psu, pru = proj_tile(wbu, wru, mm, ncols)
                o = _off["wfc"][0] + j * 3
                ACT(GP[:, 2:2 + TC], psg[:, :], AF.Copy, [prg], ["BB0", "BB1"])
                CP("pool", GP[:, 0:2], FH[:, l, j, :], ["FH"], ["BB0", "BB1"])
                ACT(ACC[:], GP[:, 0:TC], AF.Identity, ["BB0", "BB1", "SM"], ["T3"] + (["T3a", "T3b"] if j == 0 else []),
                    scale=SM[:, l, o:o + 1], bias=sm(l, "bfc", j))
                STT("dve", ACC[:], GP[:, 1:1 + TC], SM[:, l, o + 1:o + 2], ACC[:], ALU.mult, ALU.add, ["BB0", "BB1", "SM"], ["T3"])
                STT("dve", ACC[:], GP[:, 2:2 + TC], SM[:, l, o + 2:o + 3], ACC[:], ALU.mult, ALU.add, ["BB0", "BB1", "SM"], ["T3"])
                CP("pool", FH[:, l, j, :], GP[:, TC:TC + 2], ["BB0", "BB1"], ["FH"])
                ACT(GE[:], ACC[:], AF.Gelu_apprx_tanh, ["T3"], ["GE"])
                TT("dve", ACTT[j], psu[:, :], GE[:], ALU.mult, [pru, "GE"], [f"ACTT{j}"] + alias)
        AR = [f"ACTT{j}" for j in range(NFT)]
        for mm in range(KD):
            wb, wr = w_get(l, 28 + mm)
            ps, pr = bank()
            MMG(ps[:, :], [(wb[:, j * 128:(j + 1) * 128], ACTT[j]) for j in range(NFT)], [wr] + AR, [pr])
            ACT(OT[:, mm, :], ps[:, :], AF.Copy, [pr], [f"OT{mm}"])
        post_norm_residual(l, "gf_post")
        P.op("act", lambda e: e.activation(out=GE[:, 0:1], in_=GE[:, 0:1], func=AF.Copy), [], AR + alias + ["GE"])

    for c in range(nch_run):
        P.dma("sp", X[:], xT[c], W=XR_ALL)
        for l in range(DEPTH):
            mixer(c, l)
            ffn(c, l)
        P.dma("sp", yT[c], X[:], R=XR_ALL, W=[f"yT{c}"])
    evs = [P.R(f"yT{c}").w for c in range(nch_run)] + list(dump_out.values())
    P.final_wait("sp", evs)
    P.emit()
    st.close()
    return nc


def _kblock(W, n):
    K = W.shape[0]
    kc = K // 128
    a = W.reshape(kc, 128, n).transpose(1, 0, 2).reshape(128, kc * n)
    out = np.zeros((128, 4096), np.float32)
    out[:, :kc * n] = a
    return out


def pack_weights(inp):
    wst = np.zeros((DEPTH, NBLK, 128, 4096), np.float32)
    small = np.zeros((128, DEPTH, NSMALL), np.float32)
    craw = np.zeros((DEPTH, 128, 16, 256), np.float32)

    def put(l, name, arr):
        o, w = _off[name]
        small[:, l, o:o + w] = arr

    for l in range(DEPTH):
        w_in = np.asarray(inp["w_in"][l], np.float32)
        for b, c0 in enumerate((0, 512, 1024, 1536, 2048)):
            wst[l, b] = _kblock(w_in[:, c0:c0 + 512], 512)
        for b, c0 in ((5, 2568), (6, 3080), (7, 3592), (8, 4104)):
            wst[l, b] = _kblock(w_in[:, c0:c0 + 512], 512)
        bre = np.asarray(inp["ssm_b_re"][l], np.float32)
        bim = np.asarray(inp["ssm_b_im"][l], np.float32)
        cre = np.asarray(inp["ssm_c_re"][l], np.float32)
        cim = np.asarray(inp["ssm_c_im"][l], np.float32)
        blkB = np.zeros((128, 16, 256), np.float32)
        for k in range(16):
            for gi in range(2):
                g = 2 * k + gi
                gg = g % 8
                blkB[gg * 16:(gg + 1) * 16, k, gi * 64:(gi + 1) * 64] = bre[g].T
                blkB[gg * 16:(gg + 1) * 16, k, 128 + gi * 64:128 + (gi + 1) * 64] = bim[g].T
                craw[l, gi * 64:(gi + 1) * 64, k, gg * 16:(gg + 1) * 16] = cre[g].T
                craw[l, gi * 64:(gi + 1) * 64, k, 128 + gg * 16:128 + (gg + 1) * 16] = cim[g].T
        wst[l, 9] = blkB.reshape(128, 4096)
        wst[l, 11] = _kblock(np.asarray(inp["w_ssm_glu"][l], np.float32), 512)
        wst[l, 12] = _kblock(np.asarray(inp["w_branch_ssm"][l], np.float32), 1024)
        wst[l, 13] = _kblock(np.asarray(inp["w_branch_mlstm"][l], np.float32), 1024)
        w_out = np.asarray(inp["w_out"][l], np.float32)
        wst[l, 14] = _kblock(w_out[:, 0:512], 512)
        wst[l, 15] = _kblock(w_out[:, 512:1024], 512)
        wg = np.asarray(inp["w_ffn_gate"][l], np.float32)
        wu = np.asarray(inp["w_ffn_up"][l], np.float32)
        for bi in range(6):
            n = 512 if bi < 5 else 256
            wst[l, 16 + 2 * bi] = _kblock(wg[:, bi * 512:bi * 512 + n], n)
            wst[l, 17 + 2 * bi] = _kblock(wu[:, bi * 512:bi * 512 + n], n)
        wd = np.asarray(inp["w_ffn_down"][l], np.float32)
        for m in range(8):
            wst[l, 28 + m] = _kblock(wd[:, m * 128:(m + 1) * 128], 128)
        put(l, "g_pre", np.asarray(inp["g_mix_pre"][l]).reshape(8, 128).T)
        put(l, "g_post", np.asarray(inp["g_mix_post"][l]).reshape(8, 128).T)
        put(l, "gf_pre", np.asarray(inp["g_ffn_pre"][l]).reshape(8, 128).T)
        put(l, "gf_post", np.asarray(inp["g_ffn_post"][l]).reshape(8, 128).T)
        put(l, "wgate", w_in[:, 2560:2568].reshape(8, 128, 8).transpose(1, 0, 2).reshape(128, 64))
        put(l, "bgate", np.broadcast_to(np.asarray(inp["b_gates"][l], np.float32)[None, :], (128, 8)))
        wqk = np.asarray(inp["w_qk_conv"][l], np.float32)
        put(l, "wqk", wqk.reshape(4, 8, 128).transpose(2, 1, 0).reshape(128, 32))
        put(l, "bqk", np.asarray(inp["b_qk_conv"][l]).reshape(8, 128).T)
        put(l, "ghead", np.asarray(inp["g_head_norm"][l]).reshape(4, 128).T)
        wfc = np.asarray(inp["w_ffn_conv"][l], np.float32)
        put(l, "wfc", wfc.reshape(3, 22, 128).transpose(2, 1, 0).reshape(128, 66))
        put(l, "bfc", np.asarray(inp["b_ffn_conv"][l]).reshape(22, 128).T)
        lre = np.asarray(inp["ssm_lambda_re"][l], np.float32)
        lim = np.asarray(inp["ssm_lambda_im"][l], np.float32)
        ldt = np.asarray(inp["ssm_log_dt"][l], np.float32)
        put(l, "lre", lre.reshape(16, 128).T)
        put(l, "lim", lim.reshape(16, 128).T)
        put(l, "ldt", np.repeat(ldt, 64).reshape(16, 128).T)
        put(l, "dvec", np.asarray(inp["ssm_d"][l], np.float32).reshape(4, 128).T)
    return wst, small, craw


def make_consts():
    cst = np.zeros((128, 128 * 3 + TC), np.float32)
    cst[:, 0:128] = np.eye(128, dtype=np.float32)
    cst[:, 128:256] = np.triu(np.ones((128, 128), np.float32))
    cst[:, 256:384] = 1.0
    cst[:, 384:] = np.arange(TC, dtype=np.float32)[None, :]
    return cst


def pack_tokens(x, meta, nch):
    seq = np.zeros((NCH * TC, D), np.float32)
    seq[PADF:PADF + NMETA] = meta
    seq[PADF + NMETA:] = x
    seq = seq[:nch * TC]
    xT = np.ascontiguousarray(seq.reshape(nch, TC, KD, 128).transpose(0, 3, 2, 1))
    tok = np.arange(nch * TC).reshape(nch * NSUB, 128).T
    padb = np.where(tok < PADF, np.float32(-30000.0), np.float32(0.0)).astype(np.float32)
    return xT, np.ascontiguousarray(padb)


_CACHE = {}


def run(inputs, nch, dumps=()):
    key = (nch, tuple(dumps))
    if key not in _CACHE:
        _CACHE[key] = build_program(nch, dumps)
    nc = _CACHE[key]
    wst, small, craw = pack_weights(inputs)
    xT, padb = pack_tokens(np.asarray(inputs["x"], np.float32)[0], np.asarray(inputs["meta_tokens"], np.float32), nch)
    in_map = {"xT": xT, "wst": wst, "small": small, "craw": craw, "consts": make_consts(), "padb": padb}
    res = run_bass_kernel_spmd(nc, [in_map], core_ids=[0])
    return res.results[0]


def kernel(**inputs):
    r = run(inputs, NCH)
    yT = r["yT"]
    seq = yT.transpose(0, 3, 2, 1).reshape(NCH * TC, D)
    out = seq[PADF + NMETA:].reshape(1, SEQ, D)
    return np.ascontiguousarray(out.astype(np.float32))
```
